# Optimizing a Trainium2 kernel written in Bass

```python
import math
import jax
import jax.numpy as jnp
from jax import lax
import numpy as np

D_MODEL = 1024
BATCH = 16
SEQ = 2048
DEPTH = 1

CTX_LEN = 256
GRID_W = 64

N_HEADS = 8
N_KV_HEADS = 2
GQA_GROUP = N_HEADS // N_KV_HEADS
HEAD_DIM = 64
WINDOW = 128
ATTN_BLOCK = 128
ROPE_BASE = 10000.0

HY_WIDTH = 512
HY_ORDER = 2
HY_SHORT_CONV = 3
HY_EMB_DIM = 33
HY_FILTER_HIDDEN = 64
HY_FILTER_OUT_SCALE = 0.05
HY_DECAY_TARGET = 1e-2
HY_FAST_DECAY_PCT = 0.3
HY_SLOW_DECAY_PCT = 1.5

N_BRANCHES = 2

N_EXPERTS = 32
TOP_K = 4
D_FF = 1024
SWIGLU_LIMIT = 7.0
SWIGLU_ALPHA = 1.702
EXPERT_BLOCK = 128

LN_EPS = 1e-5

Q_W = N_HEADS * HEAD_DIM
KV_W = N_KV_HEADS * HEAD_DIM
K_OFF = Q_W
V_OFF = K_OFF + KV_W
HY_OFF = V_OFF + KV_W
GATE_OFF = HY_OFF + (HY_ORDER + 1) * HY_WIDTH
IN_W = GATE_OFF + N_BRANCHES * D_MODEL

DEEPNORM_ALPHA = (2 * DEPTH) ** 0.25
DEEPNORM_BETA = (8 * DEPTH) ** -0.25

kernel_name = 'hybrid_swa_hyena_moe_diffusion_block'


def _layer_norm(x, g=None, b=None):
    xf = x.astype(jnp.float32)
    mu = jnp.mean(xf, axis=-1, keepdims=True)
    var = jnp.mean(jnp.square(xf - mu), axis=-1, keepdims=True)
    y = (xf - mu) * lax.rsqrt(var + LN_EPS)
    if g is not None:
        y = y * g.astype(jnp.float32) + b.astype(jnp.float32)
    return y.astype(x.dtype)


def _modulation(cond, w, b):
    return jax.nn.silu(cond) @ w + b


def _modulate(x, shift, scale):
    return _layer_norm(x) * (1 + scale) + shift


def _axial_rope_tables(rows):
    row = jnp.repeat(jnp.arange(rows, dtype=jnp.float32), GRID_W)
    col = jnp.tile(jnp.arange(GRID_W, dtype=jnp.float32), rows)
    n_freq = HEAD_DIM // 4
    inv_freq = ROPE_BASE ** (-jnp.arange(n_freq, dtype=jnp.float32) / n_freq)
    ang = jnp.stack([row[:, None] * inv_freq, col[:, None] * inv_freq], axis=1)
    return jnp.cos(ang), jnp.sin(ang)


def _apply_axial_rope(x, cos, sin):
    B_, L, H, _ = x.shape
    xr = x.astype(jnp.float32).reshape(B_, L, H, 2, 2, HEAD_DIM // 4)
    x1, x2 = xr[..., 0, :], xr[..., 1, :]
    c = cos[None, :, None]
    s = sin[None, :, None]
    out = jnp.stack([x1 * c - x2 * s, x2 * c + x1 * s], axis=-2)
    return out.reshape(x.shape).astype(x.dtype)


def _split_proj(p):
    return (p[..., :K_OFF], p[..., K_OFF:V_OFF], p[..., V_OFF:HY_OFF],
            p[..., HY_OFF:GATE_OFF], p[..., GATE_OFF:])


def _latent_window_attention(q, k, v, k_ctx, v_ctx, sink):
    B_, L = q.shape[:2]
    n_ctx = k_ctx.shape[1]
    n_blocks = L // ATTN_BLOCK
    span = ATTN_BLOCK + 2 * WINDOW
    pad = ((0, 0), (WINDOW, WINDOW), (0, 0), (0, 0))
    k_pad, v_pad = jnp.pad(k, pad), jnp.pad(v, pad)
    q_blocks = jnp.moveaxis(
        q.reshape(B_, n_blocks, ATTN_BLOCK, N_KV_HEADS, GQA_GROUP, HEAD_DIM), 1, 0)
    qi = jnp.arange(ATTN_BLOCK)[:, None]
    kj = jnp.arange(span)[None, :]
    band = jnp.abs(kj - WINDOW - qi) <= WINDOW
    sink_logit = sink.astype(jnp.float32).reshape(1, N_KV_HEADS, GQA_GROUP, 1, 1)
    scale = HEAD_DIM ** -0.5

    def one_block(args):
        b_idx, q_blk = args
        start = b_idx * ATTN_BLOCK
        k_blk = lax.dynamic_slice_in_dim(k_pad, start, span, axis=1)
        v_blk = lax.dynamic_slice_in_dim(v_pad, start, span, axis=1)
        key_pos = start - WINDOW + kj
        valid = band & (key_pos >= 0) & (key_pos < L)
        s_loc = jnp.einsum('bqhgd,bshd->bhgqs', q_blk, k_blk).astype(jnp.float32) * scale
        s_loc = jnp.where(valid, s_loc, -jnp.inf)
        s_c = jnp.einsum('bqhgd,bchd->bhgqc', q_blk, k_ctx).astype(jnp.float32) * scale
        s_sink = jnp.broadcast_to(sink_logit, s_loc.shape[:-1] + (1,))
        p = jax.nn.softmax(jnp.concatenate([s_loc, s_c, s_sink], axis=-1), axis=-1).astype(v.dtype)
        o = (jnp.einsum('bhgqs,bshd->bqhgd', p[..., :span], v_blk)
             + jnp.einsum('bhgqc,bchd->bqhgd', p[..., span:span + n_ctx], v_ctx))
        return o

    out = lax.map(one_block, (jnp.arange(n_blocks), q_blocks))
    return jnp.moveaxis(out, 0, 1).reshape(B_, L, Q_W)


def _context_attention(q, k, v, sink):
    B_, C = q.shape[:2]
    s = jnp.einsum('bqhgd,bshd->bhgqs', q, k).astype(jnp.float32) * HEAD_DIM ** -0.5
    sink_logit = sink.astype(jnp.float32).reshape(1, N_KV_HEADS, GQA_GROUP, 1, 1)
    s_sink = jnp.broadcast_to(sink_logit, s.shape[:-1] + (1,))
    p = jax.nn.softmax(jnp.concatenate([s, s_sink], axis=-1), axis=-1)[..., :-1].astype(v.dtype)
    return jnp.einsum('bhgqs,bshd->bqhgd', p, v).reshape(B_, C, Q_W)


def _short_conv(u, w, b):
    L = u.shape[1]
    half = HY_SHORT_CONV // 2
    up = jnp.pad(u, ((0, 0), (half, half), (0, 0)))
    out = b
    for i in range(HY_SHORT_CONV):
        out = out + up[:, i:i + L] * w[i]
    return out


def _hyena_filters(L, w1, b1, w2, b2, w3):
    bands = (HY_EMB_DIM - 1) // 2
    t = jnp.linspace(0.0, 1.0, L, dtype=jnp.float32)[:, None]
    omega = 2.0 * math.pi * jnp.arange(L, dtype=jnp.float32)[:, None] / L
    f = jnp.linspace(1e-4, bands - 1, bands, dtype=jnp.float32)[None, :]
    z = jnp.concatenate([t, jnp.cos(f * omega), -jnp.sin(f * omega)], axis=-1)
    h = jnp.sin(z @ w1 + b1)
    h = jnp.sin(h @ w2 + b2)
    h = (h @ w3).astype(jnp.float32).reshape(L, HY_ORDER, 2, HY_WIDTH)
    min_decay = math.log(HY_DECAY_TARGET) / HY_FAST_DECAY_PCT
    max_decay = math.log(HY_DECAY_TARGET) / HY_SLOW_DECAY_PCT
    deltas = jnp.abs(jnp.linspace(min_decay, max_decay, HY_WIDTH, dtype=jnp.float32))
    h = h * jnp.exp(-t * deltas)[:, None, None, :]
    fwd, bwd = h[:, :, 0], h[:, :, 1]
    return jnp.concatenate([fwd, jnp.zeros_like(fwd[:1]), bwd[:0:-1]], axis=0)


def _hyena(u, conv_w, conv_b, fw1, fb1, fw2, fb2, fw3, skip):
    L = u.shape[1]
    n_fft = 2 * L
    streams = jnp.split(_short_conv(u, conv_w, conv_b).astype(jnp.float32), HY_ORDER + 1, axis=-1)
    filt_f = jnp.fft.rfft(_hyena_filters(L, fw1, fb1, fw2, fb2, fw3), axis=0)
    z = streams[-1]
    for o in range(HY_ORDER):
        conv = jnp.fft.irfft(jnp.fft.rfft(z, n=n_fft, axis=1) * filt_f[None, :, o],
                             n=n_fft, axis=1)[:, :L]
        z = streams[o] * (conv + skip[o].astype(jnp.float32) * z)
    return z.astype(u.dtype)


def _merge_branches(att, hy, gate_pre, w_ba, w_bh, w_o):
    g_att, g_hy = jnp.split(jax.nn.sigmoid(gate_pre), N_BRANCHES, axis=-1)
    return (g_att * (att @ w_ba) + g_hy * (hy @ w_bh)) @ w_o


def _moe(h, router_w, router_b, w1, b1, w2, b2):
    B_, L, D = h.shape
    T = B_ * L
    xt = h.reshape(T, D)
    logits = (xt @ router_w + router_b).astype(jnp.float32)
    top_vals, top_idx = lax.top_k(logits, TOP_K)
    gate = jax.nn.softmax(top_vals, axis=-1)
    A = T * TOP_K
    expert_ids = top_idx.reshape(A).astype(jnp.int32)
    token_ids = (jnp.arange(A, dtype=jnp.int32) // TOP_K)
    weights = gate.reshape(A)
    order = jnp.argsort(expert_ids)
    e_sorted = expert_ids[order]
    counts = jnp.bincount(expert_ids, length=N_EXPERTS).astype(jnp.int32)
    starts = jnp.cumsum(counts) - counts
    padded = (counts + EXPERT_BLOCK - 1) // EXPERT_BLOCK * EXPERT_BLOCK
    pad_end = jnp.cumsum(padded)
    pad_start = pad_end - padded
    dest = pad_start[e_sorted] + (jnp.arange(A, dtype=jnp.int32) - starts[e_sorted])
    n_blocks = A // EXPERT_BLOCK + N_EXPERTS
    n_slots = n_blocks * EXPERT_BLOCK
    slot_tok = jnp.full((n_slots,), T, jnp.int32).at[dest].set(token_ids[order])
    slot_w = jnp.zeros((n_slots,), jnp.float32).at[dest].set(weights[order])
    block_start = jnp.arange(n_blocks, dtype=jnp.int32) * EXPERT_BLOCK
    block_exp = jnp.minimum(jnp.searchsorted(pad_end, block_start, side='right'), N_EXPERTS - 1)
    x_pad = jnp.concatenate([xt, jnp.zeros((1, D), xt.dtype)], axis=0)

    def step(acc, blk):
        tok, wt, e = blk
        hb = x_pad[tok] @ w1[e] + b1[e]
        glu, lin = jnp.split(hb.astype(jnp.float32), 2, axis=-1)
        glu = jnp.minimum(glu, SWIGLU_LIMIT)
        lin = jnp.clip(lin, -SWIGLU_LIMIT, SWIGLU_LIMIT)
        act = (glu * jax.nn.sigmoid(SWIGLU_ALPHA * glu) * (lin + 1.0)).astype(h.dtype)
        yb = (act @ w2[e] + b2[e]).astype(jnp.float32)
        return acc.at[tok].add(yb * wt[:, None]), None

    acc0 = jnp.zeros((T + 1, D), jnp.float32)
    acc, _ = lax.scan(step, acc0, (slot_tok.reshape(n_blocks, EXPERT_BLOCK),
                                   slot_w.reshape(n_blocks, EXPERT_BLOCK), block_exp))
    return acc[:T].reshape(B_, L, D).astype(h.dtype)


def setup_inputs(seed: int = 0) -> dict:
    key = jax.random.key(seed)
    ks = jax.random.split(key, 32)
    D = D_MODEL
    hy_in = (HY_ORDER + 1) * HY_WIDTH
    filt_out = HY_ORDER * 2 * HY_WIDTH

    def nrm(k, shape, s):
        return jax.random.normal(k, shape, jnp.float32) * s

    return {
        'x': nrm(ks[0], (BATCH, SEQ, D), 1.0),
        'c': nrm(ks[1], (BATCH, D), 1.0),
        'ctx': nrm(ks[2], (BATCH, CTX_LEN, D), 1.0),
        'c_ctx': nrm(ks[3], (D,), 1.0),
        'w_mod': nrm(ks[4], (DEPTH, D, 6 * D), D ** -0.5),
        'b_mod': nrm(ks[5], (DEPTH, 6 * D), 0.02),
        'w_in': nrm(ks[6], (DEPTH, D, IN_W), D ** -0.5),
        'attn_sink': nrm(ks[7], (DEPTH, N_HEADS), 0.5),
        'hy_conv_w': nrm(ks[8], (DEPTH, HY_SHORT_CONV, hy_in), HY_SHORT_CONV ** -0.5),
        'hy_conv_b': nrm(ks[9], (DEPTH, hy_in), 0.02),
        'hy_filt_w1': nrm(ks[10], (DEPTH, HY_EMB_DIM, HY_FILTER_HIDDEN), HY_EMB_DIM ** -0.5),
        'hy_filt_b1': nrm(ks[11], (DEPTH, HY_FILTER_HIDDEN), 0.1),
        'hy_filt_w2': nrm(ks[12], (DEPTH, HY_FILTER_HIDDEN, HY_FILTER_HIDDEN), HY_FILTER_HIDDEN ** -0.5),
        'hy_filt_b2': nrm(ks[13], (DEPTH, HY_FILTER_HIDDEN), 0.1),
        'hy_filt_w3': nrm(ks[14], (DEPTH, HY_FILTER_HIDDEN, filt_out),
                          HY_FILTER_HIDDEN ** -0.5 * HY_FILTER_OUT_SCALE),
        'hy_skip': nrm(ks[15], (DEPTH, HY_ORDER, HY_WIDTH), 0.5),
        'w_branch_attn': nrm(ks[16], (DEPTH, Q_W, D), Q_W ** -0.5 * DEEPNORM_BETA),
        'w_branch_hyena': nrm(ks[17], (DEPTH, HY_WIDTH, D), HY_WIDTH ** -0.5 * DEEPNORM_BETA),
        'w_out': nrm(ks[18], (DEPTH, D, D), D ** -0.5 * DEEPNORM_BETA),
        'ln1_g': 1.0 + nrm(ks[19], (DEPTH, D), 0.02),
        'ln1_b': nrm(ks[20], (DEPTH, D), 0.02),
        'router_w': nrm(ks[21], (DEPTH, D, N_EXPERTS), D ** -0.5),
        'router_b': nrm(ks[22], (DEPTH, N_EXPERTS), 0.01),
        'exp_w1': nrm(ks[23], (DEPTH, N_EXPERTS, D, 2 * D_FF), D ** -0.5),
        'exp_b1': nrm(ks[24], (DEPTH, N_EXPERTS, 2 * D_FF), 0.02),
        'exp_w2': nrm(ks[25], (DEPTH, N_EXPERTS, D_FF, D), D_FF ** -0.5 * DEEPNORM_BETA),
        'exp_b2': nrm(ks[26], (DEPTH, N_EXPERTS, D), 0.02),
        'ln2_g': 1.0 + nrm(ks[27], (DEPTH, D), 0.02),
        'ln2_b': nrm(ks[28], (DEPTH, D), 0.02),
    }


def reference(x, c, ctx, c_ctx, w_mod, b_mod, w_in, attn_sink, hy_conv_w, hy_conv_b,
              hy_filt_w1, hy_filt_b1, hy_filt_w2, hy_filt_b2, hy_filt_w3, hy_skip,
              w_branch_attn, w_branch_hyena, w_out, ln1_g, ln1_b,
              router_w, router_b, exp_w1, exp_b1, exp_w2, exp_b2, ln2_g, ln2_b):
    B_, L, _ = x.shape
    C = ctx.shape[1]
    rows = L // GRID_W
    cos, sin = _axial_rope_tables(rows)
    s_ctx = ctx
    for l in range(DEPTH):
        w_in_l = w_in[l]
        filt = (hy_filt_w1[l], hy_filt_b1[l], hy_filt_w2[l], hy_filt_b2[l], hy_filt_w3[l])
        mod_x = _modulation(c, w_mod[l], b_mod[l])[:, None, :]
        mod_c = _modulation(c_ctx[None], w_mod[l], b_mod[l])[:, None, :]
        sh1, sc1, g1, sh2, sc2, g2 = jnp.split(mod_x, 6, axis=-1)
        csh1, csc1, cg1, csh2, csc2, cg2 = jnp.split(mod_c, 6, axis=-1)
        last = l + 1 == DEPTH

        hc = _modulate(s_ctx, csh1, csc1)
        if last:
            kv_c = hc @ w_in_l[:, K_OFF:HY_OFF]
            k_c, v_c = kv_c[..., :KV_W], kv_c[..., KV_W:]
        else:
            q_c, k_c, v_c, u_hy_c, gate_c = _split_proj(hc @ w_in_l)
        k_c = k_c.reshape(B_, C, N_KV_HEADS, HEAD_DIM)
        v_c = v_c.reshape(B_, C, N_KV_HEADS, HEAD_DIM)

        hx = _modulate(x, sh1, sc1)
        q, k, v, u_hy, gate_x = _split_proj(hx @ w_in_l)
        q = _apply_axial_rope(q.reshape(B_, L, N_HEADS, HEAD_DIM), cos, sin)
        q = q.reshape(B_, L, N_KV_HEADS, GQA_GROUP, HEAD_DIM)
        k = _apply_axial_rope(k.reshape(B_, L, N_KV_HEADS, HEAD_DIM), cos, sin)
        v = v.reshape(B_, L, N_KV_HEADS, HEAD_DIM)
        att = _latent_window_attention(q, k, v, k_c, v_c, attn_sink[l])
        hy = _hyena(u_hy, hy_conv_w[l], hy_conv_b[l], *filt, hy_skip[l])
        y = _merge_branches(att, hy, gate_x, w_branch_attn[l], w_branch_hyena[l], w_out[l])
        x_mid = _layer_norm(DEEPNORM_ALPHA * x + g1 * y, ln1_g[l], ln1_b[l])

        if not last:
            q_c = q_c.reshape(B_, C, N_KV_HEADS, GQA_GROUP, HEAD_DIM)
            att_c = _context_attention(q_c, k_c, v_c, attn_sink[l])
            hy_c = _hyena(u_hy_c, hy_conv_w[l], hy_conv_b[l], *filt, hy_skip[l])
            y_c = _merge_branches(att_c, hy_c, gate_c, w_branch_attn[l], w_branch_hyena[l], w_out[l])
            c_mid = _layer_norm(DEEPNORM_ALPHA * s_ctx + cg1 * y_c, ln1_g[l], ln1_b[l])
            f_c = _moe(_modulate(c_mid, csh2, csc2), router_w[l], router_b[l],
                       exp_w1[l], exp_b1[l], exp_w2[l], exp_b2[l])
            s_ctx = _layer_norm(DEEPNORM_ALPHA * c_mid + cg2 * f_c, ln2_g[l], ln2_b[l])

        f_x = _moe(_modulate(x_mid, sh2, sc2), router_w[l], router_b[l],
                   exp_w1[l], exp_b1[l], exp_w2[l], exp_b2[l])
        x = _layer_norm(DEEPNORM_ALPHA * x_mid + g2 * f_x, ln2_g[l], ln2_b[l])
    return x
```

```python
import contextlib
import math
import numpy as np
import ml_dtypes
import concourse.bass as bass
import concourse.mybir as mybir
from concourse.bass_utils import run_bass_kernel_spmd

F32 = mybir.dt.float32
BF16 = mybir.dt.bfloat16
I32 = mybir.dt.int32
AF = mybir.ActivationFunctionType
ALU = mybir.AluOpType

ENGS = ["tensor", "vector", "scalar", "gpsimd", "sync"]
NCORES = 8
T = 2048
NT = 16
D = 1024
KC = 8
NE = 32
EXTW = 4992
GATE_BASE = 2944
HY_BASE = 1408
NBLK = 63
BLK = 512
JT = BLK // 128
NTT = 2 * NT
TT = 2 * T
NSLOT = NBLK * BLK
ALPHA = 2 ** 0.25
PI = math.pi
DEBUG = {}


def C(method, *a, **k):
    return lambda e: getattr(e, method)(*a, **k)


class Buf:
    __slots__ = ("ap", "name", "last_w", "readers", "off", "size")

    def __init__(self, ap, name=""):
        self.ap = ap
        self.name = name
        self.last_w = None
        self.readers = []

    def __getitem__(self, k):
        return self.ap[k]


class Op:
    __slots__ = ("id", "eng", "fn", "waits", "is_dma", "dsem", "dval", "needed", "val", "idx")


class Prog:
    NDSEM = 40

    def __init__(self, nc):
        self.nc = nc
        self.ops = []
        self.q = {e: [] for e in ENGS}
        self.dma_rr = 0
        self.dsem_last = [None] * self.NDSEM
        self.dsem_uses = [0] * self.NDSEM
        self.dma_since_barrier = []

    def _mk(self, eng, fn, reads, writes, is_dma):
        op = Op()
        op.id = len(self.ops)
        op.eng = eng
        op.fn = fn
        op.is_dma = is_dma
        op.needed = False
        op.val = None
        op.dsem = None
        op.dval = None
        deps = set()
        for b in reads:
            if b.last_w is not None:
                deps.add(b.last_w)
        for b in writes:
            if b.last_w is not None:
                deps.add(b.last_w)
            for r in b.readers:
                deps.add(r)
        if is_dma:
            k = self.dma_rr % self.NDSEM
            self.dma_rr += 1
            if self.dsem_last[k] is not None:
                deps.add(self.dsem_last[k])
            self.dsem_last[k] = op.id
            self.dsem_uses[k] += 1
            op.dsem = k
            op.dval = 16 * self.dsem_uses[k]
            self.dma_since_barrier.append(op.id)
        op.waits = deps
        self.ops.append(op)
        op.idx = len(self.q[eng])
        self.q[eng].append(op)
        for b in reads:
            b.readers.append(op.id)
        for b in writes:
            b.last_w = op.id
            b.readers = []
        return op

    def op(self, eng, fn, reads=(), writes=()):
        return self._mk(eng, fn, reads, writes, False)

    def dma(self, eng, out, in_, reads=(), writes=(), **kw):
        return self._mk(eng, C("dma_start", out=out, in_=in_, **kw), reads, writes, True)

    def barrier(self):
        last = [self.q[e][-1].id for e in ENGS if self.q[e] and not self.q[e][-1].is_dma]
        last = []
        for e in ENGS:
            for o in reversed(self.q[e]):
                if not o.is_dma and o.fn is not None:
                    last.append(o.id)
                    break
        dmas = list(self.dma_since_barrier)
        self.dma_since_barrier = []
        for e in ENGS:
            o = self._mk(e, None, (), (), False)
            o.waits = set(last) | set(dmas)

    def finish(self, out_ops):
        o = self._mk("sync", None, (), (), False)
        o.waits = set(x.id for x in out_ops)

    def emit(self):
        nc = self.nc
        ops = self.ops
        final_waits = {}
        for e in ENGS:
            seen = {}
            seen_d = {}
            for op in self.q[e]:
                wl = []
                for d in sorted(op.waits):
                    t = ops[d]
                    if t.is_dma:
                        if seen_d.get(t.dsem, 0) >= t.dval:
                            continue
                        seen_d[t.dsem] = t.dval
                        wl.append(d)
                    else:
                        if t.fn is None:
                            continue
                        if t.eng == e and e == "tensor":
                            continue
                        if seen.get(t.eng, -1) >= t.idx:
                            continue
                        seen[t.eng] = t.idx
                        wl.append(d)
                        t.needed = True
                final_waits[op.id] = wl
        for e in ENGS:
            c = 0
            for op in self.q[e]:
                if op.is_dma:
                    continue
                if op.needed:
                    c += 1
                    op.val = c
        with contextlib.ExitStack() as st:
            esem = {e: st.enter_context(nc.semaphore("s_" + e)) for e in ENGS}
            dsem = [st.enter_context(nc.semaphore("d%d" % i)) for i in range(self.NDSEM)]
            block = st.enter_context(nc.Block())

            def run(e, eng):
                for op in self.q[e]:
                    for d in final_waits[op.id]:
                        t = ops[d]
                        if t.is_dma:
                            eng.wait_ge(dsem[t.dsem], t.dval)
                        else:
                            eng.wait_ge(esem[t.eng], t.val)
                    if op.fn is None:
                        continue
                    ins = op.fn(eng)
                    if op.is_dma:
                        ins.then_inc(dsem[op.dsem], 16)
                    elif op.needed:
                        ins.then_inc(esem[e], 1)

            @block.tensor
            def _(eng):
                run("tensor", eng)

            @block.vector
            def _(eng):
                run("vector", eng)

            @block.scalar
            def _(eng):
                run("scalar", eng)

            @block.gpsimd
            def _(eng):
                run("gpsimd", eng)

            @block.sync
            def _(eng):
                run("sync", eng)


class Arena:
    def __init__(self, big_ap, nbytes):
        self.big = big_ap
        self.free = [(0, nbytes)]
        self.pending = []
        self.live = []

    def alloc(self, name, parts, shape, dtype):
        esz = 4 if dtype in (F32, I32) else 2
        n = int(np.prod(shape))
        size = (n * esz + 63) // 64 * 64
        for i, (o, s) in enumerate(self.free):
            if s >= size:
                if s == size:
                    self.free.pop(i)
                else:
                    self.free[i] = (o + size, s - size)
                ap = self.big[0:parts, o // 2:(o + n * esz) // 2]
                if dtype != BF16:
                    ap = ap.bitcast(dtype)
                if len(shape) > 1:
                    names = " ".join("a%d" % i for i in range(len(shape)))
                    kw = {"a%d" % i: shape[i] for i in range(1, len(shape))}
                    ap = ap.rearrange("p (%s) -> p %s" % (names, names), **kw)
                b = Buf(ap, name)
                b.off = o
                b.size = size
                for (lo, ls, ln) in self.live:
                    if o < lo + ls and lo < o + size:
                        raise RuntimeError("OVERLAP %s with %s" % (name, ln))
                self.live.append((o, size, name))
                return b
        raise RuntimeError("arena OOM for %s (%d bytes); free=%s" % (name, size, self.free))

    def release(self, *bufs):
        for b in bufs:
            self.pending.append((b.off, b.size))
            k = [x for x in self.live if x[0] == b.off and x[1] == b.size]
            if len(k) != 1:
                raise RuntimeError("bad release %s %s" % (b.name, k))
            self.live.remove(k[0])

    def commit(self):
        fl = self.free + self.pending
        self.pending = []
        fl.sort()
        out = []
        for o, s in fl:
            if out and out[-1][0] + out[-1][1] == o:
                out[-1] = (out[-1][0], out[-1][1] + s)
            else:
                out.append((o, s))
        self.free = out


_CONST_CACHE = {}


def _consts():
    if _CONST_CACHE:
        return _CONST_CACHE
    L = T
    t = np.arange(L)
    row = (t // 64).astype(np.float32)
    col = (t % 64).astype(np.float32)
    inv = (np.float32(10000.0) ** (-np.arange(16, dtype=np.float32) / np.float32(16))).astype(np.float32)
    d = np.arange(64)
    a = d // 32
    half = (d % 32) // 16
    f = d % 16
    pos = np.where(a[:, None] == 0, row[None, :], col[None, :]).astype(np.float32)
    ang = (pos * inv[f][:, None]).astype(np.float32).astype(np.float64)
    C = np.cos(ang)
    S = np.sin(ang) * np.where(half == 0, -1.0, 1.0)[:, None]
    ropeC = np.tile(C, (2, 1)).astype(np.float32)
    ropeS = np.tile(S, (2, 1)).astype(np.float32)
    s_i = np.arange(128)[:, None]
    q_i = np.arange(128)[None, :]
    maskP = (q_i <= s_i).astype(np.float32)
    maskN = (s_i <= q_i).astype(np.float32)
    masks = np.stack([maskP, maskN], axis=1).astype(ml_dtypes.bfloat16)
    tl = np.linspace(0.0, 1.0, L, dtype=np.float32)
    omega = (np.float32(2.0 * math.pi) * np.arange(L, dtype=np.float32) / np.float32(L)).astype(np.float32)
    fb = np.linspace(1e-4, 15, 16, dtype=np.float32)
    fo = (fb[None, :] * omega[:, None]).astype(np.float32).astype(np.float64)
    z = np.concatenate([tl[:, None].astype(np.float64), np.cos(fo), -np.sin(fo)], axis=-1)
    zT = np.ascontiguousarray(z.T).astype(np.float32)
    min_decay = math.log(1e-2) / 0.3
    max_decay = math.log(1e-2) / 1.5
    deltas = np.abs(np.linspace(min_decay, max_decay, 512, dtype=np.float32)).astype(np.float64)
    decay = np.exp(-tl[:, None].astype(np.float64) * deltas[None, :]).astype(np.float32)
    decayb = decay.copy()
    decayb[0] = 0.0
    N = 2 * L
    tt = np.arange(L, dtype=np.float64)
    ff = np.arange(L, dtype=np.float64) + 0.5
    th = 2.0 * math.pi * np.outer(tt, ff) / N
    Cf = np.cos(th)
    Sf = np.sin(th)
    bf = ml_dtypes.bfloat16

    def tile_fwd(M):
        return np.ascontiguousarray(M.reshape(16, 128, 16, 128).transpose(2, 1, 0, 3)).astype(bf).reshape(16, 128, 2048)

    def tile_inv(M):
        return np.ascontiguousarray(M.reshape(16, 128, 4, 512).transpose(2, 1, 0, 3)).astype(bf).reshape(4, 128, 8192)

    pp_ = np.arange(128)
    ltri = (pp_[:, None] < pp_[None, :]).astype(np.float32)
    kk512 = np.tile(np.repeat(np.arange(NBLK, dtype=np.float32) * BLK, NE)[None, :], (128, 1))
    fcp = (np.arange(8)[None, :] * 128 + pp_[:, None]).astype(np.float32)
    ecol = pp_[:, None].astype(np.float32)
    tokid = (np.arange(NTT)[None, :] * 128 + pp_[:, None]).astype(np.float32)
    j4 = np.tile(np.tile(np.arange(4, dtype=np.float32), NTT)[None, :], (128, 1))
    sl = np.arange(NSLOT)
    slot_init = np.stack([TT + sl % 128, 4 * TT + sl % 128, np.zeros(NSLOT), np.zeros(NSLOT)], axis=1).astype(np.float32)
    _CONST_CACHE.update(dict(
        ltri=ltri, kk512=np.ascontiguousarray(kk512), fcp=fcp, ecol=ecol, tokid=tokid, j4=np.ascontiguousarray(j4), slot_init=slot_init,
        ropeC=ropeC, ropeS=ropeS, masks=masks, zT=zT, decay=decay, decayb=decayb,
        cft=tile_fwd(Cf), sft=tile_fwd(Sf),
        cit=tile_inv(Cf.T * (2.0 / N)), sit=tile_inv(Sf.T * (2.0 / N)),
    ))
    return _CONST_CACHE


def _ext_cols():
    q_cols, qp_cols = [], []
    for c in range(4):
        for hf in range(2):
            h = c + 4 * hf
            for dd in range(64):
                q_cols.append(h * 64 + dd)
                qp_cols.append(h * 64 + (dd ^ 16))
    k_cols = [512 + i for i in range(128)]
    kp_cols = [512 + (i // 64) * 64 + ((i % 64) ^ 16) for i in range(128)]
    v_cols = [640 + i for i in range(128)]
    rest = list(range(768, 4352))
    return np.array(q_cols + qp_cols + k_cols + kp_cols + v_cols + rest)


def build(stop_after=None):
    nc = bass.Bass("TRN2", target_bir_lowering=False)

    def din(name, shape, dt=F32):
        return nc.dram_tensor(name, list(shape), dt, kind="ExternalInput").ap()

    x_d = din("x", [2, T, D])
    c3_d = din("c3", [3, D])
    ctx_d = din("ctx", [2, 256, D])
    wmod_d = din("w_mod", [D, 6144])
    bmod_d = din("b_mod", [1, 6144])
    win_d = din("w_in", [D, EXTW])
    sink_d = din("attn_sink", [1, 8])
    convp_d = din("convp", [128, 48])
    skip_d = din("skipp", [128, 8])
    fw1_d = din("fw1", [33, 64])
    fb1_d = din("fb1", [64, 1])
    fw2_d = din("fw2", [64, 64])
    fb2_d = din("fb2", [64, 1])
    fw3_d = din("fw3", [64, 2048])
    wba_d = din("w_ba", [512, D])
    wbh_d = din("w_bh", [512, D])
    wo_d = din("w_o", [D, D])
    ln_d = din("lnp", [4, D])
    rw_d = din("router_w", [D, NE])
    rb_d = din("router_b", [1, NE])
    w1_d = din("w1r", [NE * 8 * 128, 2048])
    b1_d = din("b1r", [128, NE * 16])
    w2_d = din("w2", [NE * D, D])
    b2_d = din("b2", [NE, D])
    ropeC_d = din("ropeC", [128, T])
    ropeS_d = din("ropeS", [128, T])
    masks_d = din("masks", [128, 256], BF16)
    zT_d = din("zT", [33, T])
    decay_d = din("decay", [T, 512])
    decayb_d = din("decayb", [T, 512])
    cft_d = din("cft", [16, 128, 2048], BF16)
    sft_d = din("sft", [16, 128, 2048], BF16)
    cit_d = din("cit", [4, 128, 8192], BF16)
    sit_d = din("sit", [4, 128, 8192], BF16)
    ltri_d = din("ltri", [128, 128])
    kk512_d = din("kk512", [128, NBLK * NE])
    fcp_d = din("fcp", [128, 8])
    ecol_d = din("ecol", [128, 1])
    tokid_d = din("tokid", [128, NTT])
    j4_d = din("j4", [128, NTT * 4])
    slotinit_d = din("slot_init", [NSLOT, 4])
    b1n_d = din("b1n", [NE, 2048])
    out_d = nc.dram_tensor("out", [2, T, D], F32, kind="ExternalOutput").ap()
    dbg_d = {k: nc.dram_tensor("dbg_" + k, list(s), F32, kind="ExternalOutput").ap() for k, s in DEBUG.items()}

    modrows_d = nc.dram_tensor("modrows", [3, 6144], F32).ap()
    xmid_d = nc.dram_tensor("xmid", [2 * T, D], F32).ap()
    hr_d = nc.dram_tensor("hr_s", [2, T, 512], BF16).ap()
    hs_d = nc.dram_tensor("hs_s", [2, T, 512], BF16).ap()
    hxs_d = nc.dram_tensor("hx_s", [128, KC * T], BF16).ap()
    atts_d = nc.dram_tensor("att_s", [64, 8 * T], BF16).ap()
    h2d_d = nc.dram_tensor("h2_rows", [TT + 128, D], BF16).ap()
    slot_d = nc.dram_tensor("slot_tab", [NSLOT, 4], F32).ap()
    out4_d = nc.dram_tensor("out4", [4 * TT + 128, D], F32).ap()
    h2rows = Buf(h2d_d, "h2rows")
    slottab = Buf(slot_d, "slottab")
    modrows = Buf(modrows_d, "modrows")
    xmid = [Buf(xmid_d, "xmid0"), Buf(xmid_d, "xmid1")]
    hspec = Buf(hr_d, "hspec")

    ARENA_BYTES = 206 * 1024
    st = contextlib.ExitStack()
    with st:
        P = Prog(nc)
        big = st.enter_context(nc.sbuf_tensor("arena", [128, ARENA_BYTES // 2], BF16))
        A = Arena(big, ARENA_BYTES)
        psb = [Buf(st.enter_context(nc.psum_tensor("ps%d" % i, [128, 512], F32)), "ps%d" % i) for i in range(8)]
        out_ops = []

        def phase_end(*bufs):
            A.release(*bufs)
            P.barrier()
            A.commit()

        cpy_rr = [0]

        def evac(out_ap, in_ap, reads, writes, eng=None):
            if eng is None:
                eng = ("scalar", "vector")[cpy_rr[0] % 2]
                cpy_rr[0] += 1
            if eng == "scalar":
                P.op("scalar", C("activation", out=out_ap, in_=in_ap, func=AF.Copy), reads=reads, writes=writes)
            else:
                P.op(eng, C("tensor_copy", out=out_ap, in_=in_ap), reads=reads, writes=writes)

        def mm(out_ap, lhsT, rhs, start, stop, reads, writes):
            P.op("tensor", C("matmul", out_ap, lhsT=lhsT, rhs=rhs, start=start, stop=stop), reads=reads, writes=writes)

        ident_f = A.alloc("ident_f", 128, [128], F32)
        ident_b = A.alloc("ident_b", 128, [128], BF16)
        ones_b = A.alloc("ones_b", 128, [64], BF16)
        ones_f = A.alloc("ones_f", 128, [128], F32)
        lnst_l = [A.alloc("lnst%d" % i, 128, [2, 6], F32) for i in range(4)]
        lnmv_l = [A.alloc("lnmv%d" % i, 128, [2], F32) for i in range(4)]
        lnrs_l = [A.alloc("lnrs%d" % i, 128, [1], F32) for i in range(4)]
        ln_rr = [0]
        P.op("gpsimd", C("memset", ident_f[:], 1.0), writes=[ident_f])
        P.op("gpsimd", C("affine_select", out=ident_f[:], in_=ident_f[:], pattern=[[-1, 128]], compare_op=ALU.is_equal,
                                                  fill=0.0, base=0, channel_multiplier=1), reads=[ident_f], writes=[ident_f])
        P.op("vector", C("tensor_copy", out=ident_b[:], in_=ident_f[:]), reads=[ident_f], writes=[ident_b])
        P.op("gpsimd", C("memset", ones_b[:], 1.0), writes=[ones_b])
        P.op("gpsimd", C("memset", ones_f[:], 1.0), writes=[ones_f])
        ones_sq = ones_f
        zrow = A.alloc("zrow", 128, [D], BF16)
        P.op("gpsimd", C("memset", zrow[:], 0.0), writes=[zrow])
        P.dma("sync", h2d_d[TT:TT + 128, :], zrow[:], reads=[zrow])

        def layer_norm(src, dst, reads_extra=()):
            lnst, lnmv, lnrs = lnst_l[ln_rr[0] % 4], lnmv_l[ln_rr[0] % 4], lnrs_l[ln_rr[0] % 4]
            ln_rr[0] += 1
            for i in range(2):
                P.op("vector", C("bn_stats", out=lnst[:, i, :], in_=src[:, i * 512:(i + 1) * 512]), reads=[src], writes=[lnst])
            P.op("vector", C("bn_aggr", out=lnmv[:], in_=lnst[:]), reads=[lnst], writes=[lnmv])
            P.op("scalar", C("activation", out=lnrs[:], in_=lnmv[:, 1:2], func=AF.Sqrt, bias=1e-5), reads=[lnmv], writes=[lnrs])
            P.op("vector", C("reciprocal", out=lnrs[:], in_=lnrs[:]), reads=[lnrs], writes=[lnrs])
            P.op("vector", C("tensor_scalar", out=dst[:], in0=src[:], scalar1=lnmv[:, 0:1], scalar2=lnrs[:, 0:1],
                                                     op0=ALU.subtract, op1=ALU.mult), reads=[src, lnmv, lnrs], writes=[dst])

        def layer_norm_g(src, dst):
            lnst, lnmv, lnrs = lnst_l[ln_rr[0] % 4], lnmv_l[ln_rr[0] % 4], lnrs_l[ln_rr[0] % 4]
            ln_rr[0] += 1
            for i in range(2):
                P.op("vector", C("bn_stats", out=lnst[:, i, :], in_=src[:, i * 512:(i + 1) * 512]), reads=[src], writes=[lnst])
            yield
            P.op("vector", C("bn_aggr", out=lnmv[:], in_=lnst[:]), reads=[lnst], writes=[lnmv])
            yield
            P.op("scalar", C("activation", out=lnrs[:], in_=lnmv[:, 1:2], func=AF.Sqrt, bias=1e-5), reads=[lnmv], writes=[lnrs])
            yield
            P.op("vector", C("reciprocal", out=lnrs[:], in_=lnrs[:]), reads=[lnrs], writes=[lnrs])
            yield
            P.op("vector", C("tensor_scalar", out=dst[:], in0=src[:], scalar1=lnmv[:, 0:1], scalar2=lnrs[:, 0:1],
                             op0=ALU.subtract, op1=ALU.mult), reads=[src, lnmv, lnrs], writes=[dst])
            yield

        def modulate_g(t, scrow, shrow):
            P.op("gpsimd", C("tensor_tensor", out=t[:], in0=t[:], in1=scrow[:], op=ALU.mult), reads=[t, scrow], writes=[t])
            yield
            P.op("vector", C("tensor_tensor", out=t[:], in0=t[:], in1=shrow[:], op=ALU.add), reads=[t, shrow], writes=[t])
            yield

        def run_interleaved(gens):
            active = list(gens)
            while active:
                nxt = []
                for g_ in active:
                    try:
                        next(g_)
                        nxt.append(g_)
                    except StopIteration:
                        pass
                active = nxt

        def load_row(buf, dram_row_ap, reads=(), plus1=False):
            P.dma("sync", buf[:], dram_row_ap.partition_broadcast(128), reads=reads, writes=[buf])
            if plus1:
                P.op("gpsimd", C("tensor_scalar", out=buf[:], in0=buf[:], scalar1=1.0, scalar2=None, op0=ALU.add), reads=[buf], writes=[buf])

        def modulate(t, scrow, shrow):
            P.op("gpsimd", C("tensor_tensor", out=t[:], in0=t[:], in1=scrow[:], op=ALU.mult), reads=[t, scrow], writes=[t])
            P.op("vector", C("tensor_tensor", out=t[:], in0=t[:], in1=shrow[:], op=ALU.add), reads=[t, shrow], writes=[t])

        def transpose_to(dst_ap, src_bf, pbank, reads, writes):
            pv = pbank.ap.bitcast(BF16)
            for kc in range(KC):
                P.op("tensor", C("transpose", out=pv[:, kc * 128:(kc + 1) * 128], in_=src_bf[:, kc * 128:(kc + 1) * 128],
                                                            identity=ident_b[:]), reads=[src_bf, ident_b], writes=[pbank])
            evac(dst_ap, pv.rearrange("p (k t) -> p k t", k=KC), reads=[pbank] + list(reads), writes=writes)

        def dbg(name, sb_ap, buf, dst=None):
            if name in dbg_d:
                o = P.dma("sync", dbg_d[name] if dst is None else dst, sb_ap, reads=[buf])
                out_ops.append(o)

        c3 = A.alloc("c3", 128, [D], F32)
        sT = A.alloc("sT", 128, [KC, 3], F32)
        bmr = A.alloc("bmr", 128, [6144], F32)
        mrow = A.alloc("mrow", 128, [6144], F32)
        wm = [A.alloc("wm%d" % i, 128, [KC, 512], F32) for i in range(2)]
        P.dma("sync", c3[0:3, :], c3_d[:, :], writes=[c3])
        P.dma("sync", bmr[0:1, :], bmod_d[:, :], writes=[bmr])
        for kc in range(KC):
            P.op("tensor", C("transpose", out=psb[0][:, kc * 4:kc * 4 + 3], in_=c3[0:3, kc * 128:(kc + 1) * 128],
                                                        identity=ident_f[0:3, 0:3]), reads=[c3, ident_f], writes=[psb[0]])
        P.op("scalar", C("activation", out=sT[:], in_=psb[0][:, 0:32].rearrange("p (k j) -> p k j", j=4)[:, :, 0:3], func=AF.Silu),
             reads=[psb[0]], writes=[sT])
        for n in range(12):
            w = wm[n % 2]
            P.dma("sync", w[:], wmod_d[:, n * 512:(n + 1) * 512].rearrange("(k p) n -> p k n", p=128), writes=[w])
            pb = psb[1 + n % 2]
            for kc in range(KC):
                mm(pb[0:3, :], sT[:, kc, :], w[:, kc, :], kc == 0, False, [sT, w], [pb])
            mm(pb[0:3, :], ones_f[0:1, 0:3], bmr[0:1, n * 512:(n + 1) * 512], False, True, [ones_f, bmr], [pb])
            evac(mrow[0:3, n * 512:(n + 1) * 512], pb[0:3, :], [pb], [mrow])
        P.dma("sync", modrows_d[:, :], mrow[0:3, :], reads=[mrow], writes=[modrows])
        dbg("mod", mrow[0:3, :], mrow)
        phase_end(c3, sT, bmr, mrow, wm[0], wm[1])
        if stop_after == 0:
            P.finish(out_ops)
            P.emit()
            return nc

        zT = A.alloc("zT", 128, [T], F32)
        fw1 = A.alloc("fw1", 128, [64], F32)
        fw2 = A.alloc("fw2", 128, [64], F32)
        fw3 = A.alloc("fw3", 128, [2048], F32)
        fb = A.alloc("fb", 128, [2], F32)
        h1T = A.alloc("h1T", 128, [T], F32)
        h2T_f = A.alloc("h2T_f", 128, [T], F32)
        su = A.alloc("su", 128, [512], F32)
        sk = A.alloc("sk", 128, [512], I32)
        skf = A.alloc("skf", 128, [512], F32)
        sfx = A.alloc("sfx", 128, [512], F32)
        P.dma("sync", zT[0:33, :], zT_d[:, :], writes=[zT])
        P.dma("sync", fw1[0:33, :], fw1_d[:, :], writes=[fw1])
        P.dma("sync", fw2[0:64, :], fw2_d[:, :], writes=[fw2])
        P.dma("sync", fw3[0:64, :], fw3_d[:, :], writes=[fw3])
        P.dma("sync", fb[0:64, 0:1], fb1_d[:, :], writes=[fb])
        P.dma("sync", fb[0:64, 1:2], fb2_d[:, :], reads=[], writes=[fb])
        P.op("vector", C("tensor_scalar", out=fb[0:64, :], in0=fb[0:64, :], scalar1=9.0 * PI, scalar2=None, op0=ALU.add), reads=[fb], writes=[fb])

        def sin_layer(wbuf, kdim, src, dst, bcol):
            for tch in range(4):
                pb = psb[tch % 2]
                mm(pb[0:64, :], wbuf[0:kdim, 0:64], src[0:kdim, tch * 512:(tch + 1) * 512], True, True, [wbuf, src], [pb])
                P.op("vector", C("tensor_scalar", out=su[0:64, :], in0=pb[0:64, :], scalar1=fb[0:64, bcol:bcol + 1], scalar2=None, op0=ALU.add),
                     reads=[pb, fb], writes=[su])
                P.op("vector", C("tensor_scalar", out=sk[0:64, :], in0=su[0:64, :], scalar1=1.0 / (2 * PI), scalar2=None, op0=ALU.mult), reads=[su], writes=[sk])
                P.op("vector", C("tensor_copy", out=skf[0:64, :], in_=sk[0:64, :]), reads=[sk], writes=[skf])
                P.op("vector", C("scalar_tensor_tensor", out=su[0:64, :], in0=skf[0:64, :], scalar=-2.0 * PI, in1=su[0:64, :], op0=ALU.mult, op1=ALU.add),
                     reads=[skf, su], writes=[su])
                P.op("vector", C("tensor_scalar", out=sfx[0:64, :], in0=su[0:64, :], scalar1=0.0, scalar2=2.0 * PI, op0=ALU.is_lt, op1=ALU.mult), reads=[su], writes=[sfx])
                P.op("vector", C("tensor_tensor", out=su[0:64, :], in0=su[0:64, :], in1=sfx[0:64, :], op=ALU.add), reads=[su, sfx], writes=[su])
                P.op("vector", C("tensor_scalar", out=su[0:64, :], in0=su[0:64, :], scalar1=-PI, scalar2=PI, op0=ALU.add, op1=ALU.min), reads=[su], writes=[su])
                P.op("vector", C("tensor_scalar", out=su[0:64, :], in0=su[0:64, :], scalar1=-PI, scalar2=None, op0=ALU.max), reads=[su], writes=[su])
                P.op("scalar", C("activation", out=dst[0:64, tch * 512:(tch + 1) * 512], in_=su[0:64, :], func=AF.Sin), reads=[su], writes=[dst])

        sin_layer(fw1, 33, zT, h1T, 0)
        sin_layer(fw2, 64, h1T, h2T_f, 1)
        dbg("h2f", h2T_f[0:64, :], h2T_f)
        Pall = A.alloc("Pall", 128, [2, NT, 512], BF16)
        Mall = A.alloc("Mall", 128, [2, NT, 512], BF16)
        dec = [A.alloc("dec%d" % i, 128, [2, 512], F32) for i in range(2)]
        ta_l = [A.alloc("ta%d" % i, 128, [512], F32) for i in range(2)]
        tb_l = [A.alloc("tb%d" % i, 128, [512], F32) for i in range(2)]
        for tt in range(NT):
            dc = dec[tt % 2]
            P.dma("sync", dc[:, 0, :], decay_d[tt * 128:(tt + 1) * 128, :], writes=[dc])
            P.dma("sync", dc[:, 1, :], decayb_d[tt * 128:(tt + 1) * 128, :], writes=[dc])
            for o in range(2):
                pf = psb[2 + (o * 2) % 4]
                pbk = psb[3 + (o * 2) % 4]
                ta, tb = ta_l[o], tb_l[o]
                mm(pf[:, :], h2T_f[0:64, tt * 128:(tt + 1) * 128], fw3[0:64, (o * 2) * 512:(o * 2 + 1) * 512], True, True, [h2T_f, fw3], [pf])
                mm(pbk[:, :], h2T_f[0:64, tt * 128:(tt + 1) * 128], fw3[0:64, (o * 2 + 1) * 512:(o * 2 + 2) * 512], True, True, [h2T_f, fw3], [pbk])
                P.op("vector", C("tensor_tensor", out=ta[:], in0=pf[:, :], in1=dc[:, 0, :], op=ALU.mult), reads=[pf, dc], writes=[ta])
                P.op("vector", C("tensor_tensor", out=tb[:], in0=pbk[:, :], in1=dc[:, 1, :], op=ALU.mult), reads=[pbk, dc], writes=[tb])
                P.op("gpsimd", C("tensor_tensor", out=Pall[:, o, tt, :], in0=ta[:], in1=tb[:], op=ALU.add), reads=[ta, tb], writes=[Pall])
                P.op("gpsimd", C("tensor_tensor", out=Mall[:, o, tt, :], in0=ta[:], in1=tb[:], op=ALU.subtract), reads=[ta, tb], writes=[Mall])
        ftab = [[A.alloc("ftab%d%d" % (i, j), 128, [NT, 128], BF16) for j in range(2)] for i in range(2)]
        hout = [[A.alloc("hout%d%d" % (i, j), 128, [512], BF16) for j in range(2)] for i in range(2)]
        it = 0
        for fch in range(16):
            ct, stb = ftab[fch % 2]
            P.dma("sync", ct[:], cft_d[fch].rearrange("p (a b) -> p a b", a=NT), writes=[ct])
            P.dma("sync", stb[:], sft_d[fch].rearrange("p (a b) -> p a b", a=NT), writes=[stb])
            for o in range(2):
                pr = psb[(it * 2) % 8]
                pi_ = psb[(it * 2 + 1) % 8]
                hr_t, hs_t = hout[it % 2]
                it += 1
                for tc in range(NT):
                    mm(pr[:, :], ct[:, tc, :], Pall[:, o, tc, :], tc == 0, tc == NT - 1, [ct, Pall], [pr])
                for tc in range(NT):
                    mm(pi_[:, :], stb[:, tc, :], Mall[:, o, tc, :], tc == 0, tc == NT - 1, [stb, Mall], [pi_])
                evac(hr_t[:], pr[:, :], [pr], [hr_t])
                evac(hs_t[:], pi_[:, :], [pi_], [hs_t])
                P.dma("sync", hr_d[o, fch * 128:(fch + 1) * 128, :], hr_t[:], reads=[hr_t])
                P.dma("sync", hs_d[o, fch * 128:(fch + 1) * 128, :], hs_t[:], reads=[hs_t])
        phase_end(zT, fw1, fw2, fw3, fb, h1T, h2T_f, su, sk, skf, sfx, Pall, Mall, dec[0], dec[1], *ta_l, *tb_l,
                  ftab[0][0], ftab[0][1], ftab[1][0], ftab[1][1], hout[0][0], hout[0][1], hout[1][0], hout[1][1])
        if stop_after == 1:
            P.finish(out_ops)
            P.emit()
            return nc

        masks = A.alloc("masks", 128, [2, 128], BF16)
        esink = A.alloc("esink", 128, [8], F32)
        convp = A.alloc("convp", 128, [4, 12], F32)
        skipp = A.alloc("skipp", 128, [2, 4], F32)
        P.dma("sync", masks[:], masks_d.rearrange("p (a b) -> p a b", a=2), writes=[masks])
        P.dma("sync", esink[0:64, :], sink_d[0, :].partition_broadcast(64), writes=[esink])
        P.op("scalar", C("activation", out=esink[0:64, :], in_=esink[0:64, :], func=AF.Exp), reads=[esink], writes=[esink])
        P.dma("sync", convp[:], convp_d.rearrange("p (a b) -> p a b", a=4), writes=[convp])
        P.dma("sync", skipp[:], skip_d.rearrange("p (a b) -> p a b", a=2), writes=[skipp])
        kcT = [A.alloc("kcT%d" % b, 128, [256], BF16) for b in range(2)]
        vc = [A.alloc("vc%d" % b, 128, [2, 128], BF16) for b in range(2)]

        chk_n = [0]

        def chk(tag):
            if "chk" not in dbg_d:
                return
            i = chk_n[0]
            chk_n[0] += 1
            tb_ = A.alloc("chk%d" % i, 128, [256], F32)
            P.op("vector", C("tensor_copy", out=tb_[:], in_=kcT[0][:]), reads=[kcT[0]], writes=[tb_])
            out_ops.append(P.dma("sync", dbg_d["chk"][i], tb_[:], reads=[tb_]))
            print("chk", i, tag)

        def wchunk_load(buf, col0, ncols=128):
            P.dma("gpsimd", buf[:], win_d[:, col0:col0 + ncols].rearrange("(k p) n -> p k n", p=128), writes=[buf])

        rows = [A.alloc("row%d" % i, 128, [D], F32) for i in range(2)]
        xin = [A.alloc("xin%d" % i, 128, [D], F32) for i in range(2)]
        xn = A.alloc("xn", 128, [D], F32)
        xb = A.alloc("xb", 128, [D], BF16)
        hcT = A.alloc("hcT", 128, [KC, 256], BF16)
        wk = A.alloc("wk", 128, [KC, 128], BF16)
        wv = A.alloc("wv", 128, [KC, 128], BF16)
        wchunk_load(wk, 1024)
        wchunk_load(wv, 1280)
        load_row(rows[0], modrows_d[2, D:2 * D], reads=[modrows], plus1=True)
        load_row(rows[1], modrows_d[2, 0:D], reads=[modrows])
        for b in range(2):
            for tl in range(2):
                xi = xin[tl % 2]
                P.dma("sync", xi[:], ctx_d[b, tl * 128:(tl + 1) * 128, :], writes=[xi])
                layer_norm(xi, xn)
                modulate(xn, rows[0], rows[1])
                P.op("vector", C("tensor_copy", out=xb[:], in_=xn[:]), reads=[xn], writes=[xb])
                transpose_to(hcT[:, :, tl * 128:(tl + 1) * 128], xb, psb[tl % 2], [], [hcT])
            pk = psb[2 + b]
            for kc in range(KC):
                mm(pk[:, 0:256], wk[:, kc, :], hcT[:, kc, :], kc == 0, kc == KC - 1, [wk, hcT], [pk])
            evac(kcT[b][:], pk[:, 0:256], [pk], [kcT[b]])
            for tl in range(2):
                pvv = psb[4 + tl]
                for kc in range(KC):
                    mm(pvv[:, 0:128], hcT[:, kc, tl * 128:(tl + 1) * 128], wv[:, kc, :], kc == 0, kc == KC - 1, [hcT, wv], [pvv])
                evac(vc[b][:, tl, :], pvv[:, 0:128], [pvv], [vc[b]])
        if "a_xn" in dbg_d:
            dbg("a_xn", xn[:], xn)
            dbg("a_row0", rows[0][:], rows[0])
            tmpb = A.alloc("dbgA", 128, [2048], F32)
            P.op("vector", C("tensor_copy", out=tmpb[:], in_=hcT[:].rearrange("p a b -> p (a b)")), reads=[hcT], writes=[tmpb])
            dbg("a_hcT", tmpb[:], tmpb)
            tmpb2 = A.alloc("dbgA2", 128, [1024], F32)
            P.op("vector", C("tensor_copy", out=tmpb2[:], in_=wk[:].rearrange("p a b -> p (a b)")), reads=[wk], writes=[tmpb2])
            dbg("a_wk", tmpb2[:], tmpb2)
            tmpb3 = A.alloc("dbgA3", 128, [512], F32)
            P.op("vector", C("tensor_copy", out=tmpb3[:, 0:256], in_=kcT[0][:]), reads=[kcT[0]], writes=[tmpb3])
            P.op("vector", C("tensor_copy", out=tmpb3[:, 256:512], in_=kcT[1][:]), reads=[kcT[1]], writes=[tmpb3])
            dbg("a_kcT", tmpb3[:], tmpb3)
            tmpb4 = A.alloc("dbgA4", 128, [512], F32)
            P.op("vector", C("tensor_copy", out=tmpb4[:], in_=psb[2][:, :]), reads=[psb[2]], writes=[tmpb4])
            dbg("a_pk", tmpb4[:], tmpb4)
        chk("end of A before phase_end")
        phase_end(hcT, wk, xin[0], xin[1], xn, xb, rows[0], rows[1])
        chk("after A phase_end")
        if stop_after == 2:
            P.finish(out_ops)
            P.emit()
            return nc

        gates = A.alloc("gates", 128, [NTT, NE], F32)
        maskall = A.alloc("maskall", 128, [NTT, NE], F32)
        for b in range(2):
            hxT = A.alloc("hxT", 128, [KC, T], BF16)
            rows = [A.alloc("row%d" % i, 128, [D], F32) for i in range(5)]
            xin = [A.alloc("xin%d" % i, 128, [D], F32) for i in range(4)]
            xn_l = [A.alloc("xn%d" % i, 128, [D], F32) for i in range(2)]
            xb_l = [A.alloc("xb%d" % i, 128, [D], BF16) for i in range(2)]
            if b == 0:
                chk("B after allocs")
            load_row(rows[0], modrows_d[b, D:2 * D], reads=[modrows], plus1=True)
            load_row(rows[1], modrows_d[b, 0:D], reads=[modrows])
            if b == 0:
                chk("B after load_rows")
            def tileB(tl):
                xi = xin[tl % 4]
                P.dma("sync", xi[:], x_d[b, tl * 128:(tl + 1) * 128, :], writes=[xi])
                xn, xb = xn_l[tl % 2], xb_l[tl % 2]
                yield from layer_norm_g(xi, xn)
                yield from modulate_g(xn, rows[0], rows[1])
                P.op("vector", C("tensor_copy", out=xb[:], in_=xn[:]), reads=[xn], writes=[xb])
                yield
                transpose_to(hxT[:, :, tl * 128:(tl + 1) * 128], xb, psb[tl % 2], [], [hxT])

            for tl in range(0, NT, 2):
                run_interleaved([tileB(tl), tileB(tl + 1)])
            if b == 0:
                chk("after B loop")
            if stop_after == 2.5:
                P.finish(out_ops)
                P.emit()
                return nc
            phase_end(*xin, *xn_l, *xb_l, *rows)
            if b == 0:
                chk("after B phase_end")
            QT = A.alloc("QT", 128, [4, T], BF16)
            KT = A.alloc("KT", 128, [T], BF16)
            Vt = A.alloc("Vt", 128, [NT, 128], BF16)
            sTm = A.alloc("sTm", 128, [12, T], BF16)
            ropeC = A.alloc("ropeC", 128, [T], F32)
            ropeS = A.alloc("ropeS", 128, [T], F32)
            wq = [A.alloc("wq%d" % i, 128, [KC, 128], BF16) for i in range(4)]
            r1_l = [A.alloc("r1%d" % i, 128, [512], F32) for i in range(2)]
            r2_l = [A.alloc("r2%d" % i, 128, [512], F32) for i in range(2)]
            U = [A.alloc("U%d" % i, 128, [T + 2], F32) for i in range(2)]
            cs = A.alloc("cs", 128, [T], F32)
            P.dma("sync", ropeC[:], ropeC_d[:, :], writes=[ropeC])
            P.dma("sync", ropeS[:], ropeS_d[:, :], writes=[ropeS])
            for i in range(2):
                P.op("gpsimd", C("memset", U[i][:, 0:1], 0.0), writes=[U[i]])
                P.op("gpsimd", C("memset", U[i][:, T + 1:T + 2], 0.0), writes=[U[i]])
            pc = 0
            for c in range(5):
                wa, wb_ = wq[(2 * c) % 4], wq[(2 * c + 1) % 4]
                if c < 4:
                    wchunk_load(wa, c * 128)
                    wchunk_load(wb_, 512 + c * 128)
                else:
                    wchunk_load(wa, 1024)
                    wchunk_load(wb_, 1152)
                for tch in range(4):
                    pa = psb[pc % 8]
                    pb = psb[(pc + 1) % 8]
                    pc += 2
                    r1, r2 = r1_l[(pc // 2) % 2], r2_l[(pc // 2) % 2]
                    ts_ = slice(tch * 512, (tch + 1) * 512)
                    for kc in range(KC):
                        mm(pa[:, :], wa[:, kc, :], hxT[:, kc, ts_], kc == 0, kc == KC - 1, [wa, hxT], [pa])
                    for kc in range(KC):
                        mm(pb[:, :], wb_[:, kc, :], hxT[:, kc, ts_], kc == 0, kc == KC - 1, [wb_, hxT], [pb])
                    P.op("vector", C("tensor_tensor", out=r1[:], in0=pa[:, :], in1=ropeC[:, ts_], op=ALU.mult), reads=[pa, ropeC], writes=[r1])
                    P.op("vector", C("tensor_tensor", out=r2[:], in0=pb[:, :], in1=ropeS[:, ts_], op=ALU.mult), reads=[pb, ropeS], writes=[r2])
                    if c < 4:
                        P.op("gpsimd", C("tensor_tensor", out=QT[:, c, ts_], in0=r1[:], in1=r2[:], op=ALU.add), reads=[r1, r2], writes=[QT])
                    else:
                        P.op("gpsimd", C("tensor_tensor", out=KT[:, ts_], in0=r1[:], in1=r2[:], op=ALU.add), reads=[r1, r2], writes=[KT])
            if b == 0:
                chk("after qk")
            for tl in range(NT):
                pvv = psb[pc % 8]
                pc += 1
                for kc in range(KC):
                    mm(pvv[:, 0:128], hxT[:, kc, tl * 128:(tl + 1) * 128], wv[:, kc, :], kc == 0, kc == KC - 1, [hxT, wv], [pvv])
                evac(Vt[:, tl, :], pvv[:, 0:128], [pvv], [Vt])
            if b == 0:
                chk("after V")
            for j in range(12):
                wj = wq[j % 4]
                wchunk_load(wj, HY_BASE + j * 128)
                Uj = U[j % 2]
                for tch in range(4):
                    pu = psb[pc % 8]
                    pc += 1
                    for kc in range(KC):
                        mm(pu[:, :], wj[:, kc, :], hxT[:, kc, tch * 512:(tch + 1) * 512], kc == 0, kc == KC - 1, [wj, hxT], [pu])
                    evac(Uj[:, 1 + tch * 512:1 + (tch + 1) * 512], pu[:, :], [pu], [Uj])
                P.op("vector", C("tensor_scalar", out=cs[:], in0=Uj[:, 1:T + 1], scalar1=convp[:, 1, j:j + 1], scalar2=convp[:, 3, j:j + 1],
                                                                  op0=ALU.mult, op1=ALU.add), reads=[Uj, convp], writes=[cs])
                P.op("vector", C("scalar_tensor_tensor", out=cs[:], in0=Uj[:, 0:T], scalar=convp[:, 0, j:j + 1], in1=cs[:],
                                                                         op0=ALU.mult, op1=ALU.add), reads=[Uj, convp, cs], writes=[cs])
                P.op("vector", C("scalar_tensor_tensor", out=sTm[:, j, :], in0=Uj[:, 2:T + 2], scalar=convp[:, 2, j:j + 1], in1=cs[:],
                                                                         op0=ALU.mult, op1=ALU.add), reads=[Uj, convp, cs], writes=[sTm])
            if b == 0:
                chk("after hy")
            if "qT" in dbg_d and b == 0:
                for c in range(4):
                    tmpb = cs
                    P.op("vector", C("tensor_copy", out=tmpb[:], in_=QT[:, c, :]), reads=[QT], writes=[tmpb])
                    dbg("qT", tmpb[:], tmpb, dst=dbg_d["qT"][c])
            if "sT" in dbg_d and b == 0:
                for c in range(12):
                    tmpb = cs
                    P.op("vector", C("tensor_copy", out=tmpb[:], in_=sTm[:, c, :]), reads=[sTm], writes=[tmpb])
                    dbg("sT", tmpb[:], tmpb, dst=dbg_d["sT"][c])
            if "kchk" in dbg_d and b == 0:
                P.op("vector", C("tensor_copy", out=cs[:, 0:256], in_=kcT[0][:]), reads=[kcT[0]], writes=[cs])
                P.op("vector", C("tensor_copy", out=cs[:, 256:512], in_=vc[0][:].rearrange("p a b -> p (a b)")), reads=[vc[0]], writes=[cs])
                dbg("kchk", cs[:, 0:512], cs)
            P.dma("sync", hxs_d[:, :], hxT[:].rearrange("p k t -> p (k t)"), reads=[hxT])
            phase_end(ropeC, ropeS, wq[0], wq[1], wq[2], wq[3], *r1_l, *r2_l, U[0], U[1], cs, hxT)
            if stop_after == 3:
                P.finish(out_ops)
                P.emit()
                return nc

            attT = A.alloc("attT", 128, [8, T], BF16)
            PT = [A.alloc("PT%d" % i, 128, [4, 128], BF16) for i in range(3)]
            dn = A.alloc("dn", 128, [4, 128], F32)
            pidx = 0
            for qi in range(NT):
                for g in range(2):
                    gp = slice(g * 64, (g + 1) * 64)
                    keys = []
                    for j in (qi - 1, qi, qi + 1):
                        if 0 <= j < NT:
                            keys.append(("l", j))
                    keys += [("c", 0), ("c", 1)]
                    po = psb[4 + (qi * 2 + g) % 2]
                    pd = psb[6 + (qi * 2 + g) % 2]
                    for ki, (kind, j) in enumerate(keys):
                        pst = psb[pidx % 4]
                        ptb = PT[pidx % 3]
                        pidx += 1
                        if kind == "l":
                            lhs = KT[gp, j * 128:(j + 1) * 128]
                            lr = KT
                            vv = Vt[:, j, g * 64:(g + 1) * 64]
                            vr = Vt
                        else:
                            lhs = kcT[b][gp, j * 128:(j + 1) * 128]
                            lr = kcT[b]
                            vv = vc[b][:, j, g * 64:(g + 1) * 64]
                            vr = vc[b]
                        mm(pst[:, :], lhs, QT[gp, :, qi * 128:(qi + 1) * 128], True, True, [lr, QT], [pst])
                        P.op("scalar", C("activation", out=ptb[:], in_=pst[:, :].rearrange("p (h q) -> p h q", h=4), func=AF.Exp, scale=0.125),
                             reads=[pst], writes=[ptb])
                        if kind == "l" and j != qi:
                            mi = 0 if j < qi else 1
                            P.op("gpsimd", C("tensor_tensor", out=ptb[:], in0=ptb[:], in1=masks[:, mi:mi + 1, :].to_broadcast([128, 4, 128]), op=ALU.mult),
                                 reads=[ptb, masks], writes=[ptb])
                        mm(po[0:64, :], vv, ptb[:].rearrange("p h q -> p (h q)"), ki == 0, ki == len(keys) - 1, [vr, ptb], [po])
                        mm(pd[0:64, :], ones_b[:, 0:64], ptb[:].rearrange("p h q -> p (h q)"), ki == 0, ki == len(keys) - 1, [ones_b, ptb], [pd])
                    P.op("vector", C("tensor_tensor", out=dn[0:64], in0=pd[0:64, :].rearrange("p (h q) -> p h q", h=4),
                                                                          in1=esink[0:64, g * 4:(g + 1) * 4].unsqueeze(2).to_broadcast([64, 4, 128]), op=ALU.add),
                         reads=[pd, esink], writes=[dn])
                    P.op("vector", C("reciprocal", out=dn[0:64], in_=dn[0:64]), reads=[dn], writes=[dn])
                    P.op("vector", C("tensor_tensor", out=attT[0:64, g * 4:(g + 1) * 4, qi * 128:(qi + 1) * 128],
                                                                                 in0=po[0:64, :].rearrange("p (h q) -> p h q", h=4), in1=dn[0:64], op=ALU.mult),
                         reads=[po, dn], writes=[attT])
            if "attT" in dbg_d and b == 0:
                tmpb = A.alloc("dbga", 128, [T], F32)
                dbg("esink", esink[0:64, :], esink)
                P.op("vector", C("tensor_copy", out=tmpb[:, 0:256], in_=kcT[0][:]), reads=[kcT[0]], writes=[tmpb])
                dbg("kcT", tmpb[:, 0:256], tmpb)
                P.op("vector", C("tensor_copy", out=tmpb[:, 0:256], in_=vc[0][:].rearrange("p a b -> p (a b)")), reads=[vc[0]], writes=[tmpb])
                dbg("vc", tmpb[:, 0:256], tmpb)
                P.op("vector", C("tensor_copy", out=tmpb[:, 0:512], in_=PT[0][:].rearrange("p a b -> p (a b)")), reads=[PT[0]], writes=[tmpb])
                dbg("pt", tmpb[:, 0:512], tmpb)
                dbg("dn", dn[0:64].rearrange("p a b -> p (a b)"), dn)
                for c in range(8):
                    P.op("vector", C("tensor_copy", out=tmpb[0:64, :], in_=attT[0:64, c, :]), reads=[attT], writes=[tmpb])
                    dbg("attT", tmpb[0:64, :], tmpb, dst=dbg_d["attT"][c])
            P.dma("sync", atts_d[:, :], attT[0:64].rearrange("p k t -> p (k t)"), reads=[attT])
            phase_end(QT, KT, Vt, PT[0], PT[1], PT[2], dn, attT)
            if stop_after == 4:
                P.finish(out_ops)
                P.emit()
                return nc

            ztok = A.alloc("ztok", 128, [NT, 512], BF16)
            z1T = A.alloc("z1T", 128, [4, T], BF16)
            Yr = A.alloc("Yr", 128, [NT, 512], BF16)
            Ys = A.alloc("Ys", 128, [NT, 512], BF16)
            ftab = [[A.alloc("ftab%d%d" % (i, j), 128, [NT, 128], BF16) for j in range(2)] for i in range(2)]
            hin = [[A.alloc("hin%d%d" % (i, j), 128, [512], BF16) for j in range(2)] for i in range(2)]
            itab = [ztok, A.alloc("itab1", 128, [NT, 512], BF16)]
            hyT = A.alloc("hyT", 128, [4, T], BF16)
            e1_l = [A.alloc("e1%d" % i, 128, [512], F32) for i in range(2)]
            e2_l = [A.alloc("e2%d" % i, 128, [512], F32) for i in range(2)]
            e3_l = [A.alloc("e3%d" % i, 128, [512], F32) for i in range(2)]
            e4_l = [A.alloc("e4%d" % i, 128, [512], F32) for i in range(2)]
            for o in range(2):
                for tt in range(NT):
                    pz = psb[tt % 2]
                    pzv = pz.ap.bitcast(BF16)
                    for cc in range(4):
                        if o == 0:
                            src_ap, src_b = sTm[:, 8 + cc, tt * 128:(tt + 1) * 128], sTm
                        else:
                            src_ap, src_b = z1T[:, cc, tt * 128:(tt + 1) * 128], z1T
                        P.op("tensor", C("transpose", out=pzv[:, cc * 128:(cc + 1) * 128], in_=src_ap, identity=ident_b[:]),
                             reads=[src_b, ident_b], writes=[pz])
                    evac(ztok[:, tt, :], pzv[:, 0:512], [pz], [ztok])
                for fch in range(16):
                    ct, stb = ftab[fch % 2]
                    hr_t, hs_t = hin[fch % 2]
                    e1, e2, e3, e4 = e1_l[fch % 2], e2_l[fch % 2], e3_l[fch % 2], e4_l[fch % 2]
                    P.dma("sync", ct[:], cft_d[fch].rearrange("p (a b) -> p a b", a=NT), writes=[ct])
                    P.dma("sync", stb[:], sft_d[fch].rearrange("p (a b) -> p a b", a=NT), writes=[stb])
                    P.dma("sync", hr_t[:], hr_d[o, fch * 128:(fch + 1) * 128, :], reads=[hspec], writes=[hr_t])
                    P.dma("sync", hs_t[:], hs_d[o, fch * 128:(fch + 1) * 128, :], reads=[hspec], writes=[hs_t])
                    pr = psb[2 + (fch % 2) * 2]
                    pi_ = psb[3 + (fch % 2) * 2]
                    for tc in range(NT):
                        mm(pr[:, :], ct[:, tc, :], ztok[:, tc, :], tc == 0, tc == NT - 1, [ct, ztok], [pr])
                    for tc in range(NT):
                        mm(pi_[:, :], stb[:, tc, :], ztok[:, tc, :], tc == 0, tc == NT - 1, [stb, ztok], [pi_])
                    P.op("vector", C("tensor_tensor", out=e1[:], in0=pr[:, :], in1=hr_t[:], op=ALU.mult), reads=[pr, hr_t], writes=[e1])
                    P.op("vector", C("tensor_tensor", out=e2[:], in0=pi_[:, :], in1=hs_t[:], op=ALU.mult), reads=[pi_, hs_t], writes=[e2])
                    P.op("gpsimd", C("tensor_tensor", out=Yr[:, fch, :], in0=e1[:], in1=e2[:], op=ALU.subtract), reads=[e1, e2], writes=[Yr])
                    P.op("vector", C("tensor_tensor", out=e3[:], in0=pr[:, :], in1=hs_t[:], op=ALU.mult), reads=[pr, hs_t], writes=[e3])
                    P.op("vector", C("tensor_tensor", out=e4[:], in0=pi_[:, :], in1=hr_t[:], op=ALU.mult), reads=[pi_, hr_t], writes=[e4])
                    P.op("gpsimd", C("tensor_tensor", out=Ys[:, fch, :], in0=e3[:], in1=e4[:], op=ALU.add), reads=[e3, e4], writes=[Ys])
                for tch in range(4):
                    ci_, si_ = itab
                    P.dma("sync", ci_[:], cit_d[tch].rearrange("p (a b) -> p a b", a=NT), writes=[ci_])
                    P.dma("gpsimd", si_[:], sit_d[tch].rearrange("p (a b) -> p a b", a=NT), writes=[si_])
                    ts_ = slice(tch * 512, (tch + 1) * 512)
                    for cc in range(4):
                        pcv = psb[6 + cc % 2]
                        e1 = e1_l[cc % 2]
                        for fc in range(NT):
                            mm(pcv[:, :], Yr[:, fc, cc * 128:(cc + 1) * 128], ci_[:, fc, :], fc == 0, False, [Yr, ci_], [pcv])
                        for fc in range(NT):
                            mm(pcv[:, :], Ys[:, fc, cc * 128:(cc + 1) * 128], si_[:, fc, :], False, fc == NT - 1, [Ys, si_], [pcv])
                        if o == 0:
                            zin_ap, zin_b = sTm[:, 8 + cc, ts_], sTm
                            xo_ap = sTm[:, 0 + cc, ts_]
                            zo_ap, zo_b = z1T[:, cc, ts_], z1T
                        else:
                            zin_ap, zin_b = z1T[:, cc, ts_], z1T
                            xo_ap = sTm[:, 4 + cc, ts_]
                            zo_ap, zo_b = hyT[:, cc, ts_], hyT
                        P.op("vector", C("scalar_tensor_tensor", out=e1[:], in0=zin_ap, scalar=skipp[:, o, cc:cc + 1], in1=pcv[:, :],
                                                                                                          op0=ALU.mult, op1=ALU.add), reads=[pcv, zin_b, skipp], writes=[e1])
                        P.op("gpsimd", C("tensor_tensor", out=zo_ap, in0=e1[:], in1=xo_ap, op=ALU.mult), reads=[e1, sTm], writes=[zo_b])
            if "hyT" in dbg_d and b == 0:
                tmpb = A.alloc("dbgh", 128, [T], F32)
                for c in range(4):
                    P.op("vector", C("tensor_copy", out=tmpb[:], in_=hyT[:, c, :]), reads=[hyT], writes=[tmpb])
                    dbg("hyT", tmpb[:], tmpb, dst=dbg_d["hyT"][c])
            phase_end(ztok, z1T, Yr, Ys, ftab[0][0], ftab[0][1], ftab[1][0], ftab[1][1], hin[0][0], hin[0][1], hin[1][0], hin[1][1],
                      itab[1], *e1_l, *e2_l, *e3_l, *e4_l, sTm)
            if stop_after == 5:
                P.finish(out_ops)
                P.emit()
                return nc

            hxT = A.alloc("hxT", 128, [KC, T], BF16)
            attT = A.alloc("attT", 128, [8, T], BF16)
            P.dma("sync", hxT[:].rearrange("p k t -> p (k t)"), hxs_d[:, :], writes=[hxT])
            P.dma("sync", attT[0:64].rearrange("p k t -> p (k t)"), atts_d[:, :], writes=[attT])
            wba = A.alloc("wba", 128, [8, D], BF16)
            wbh = A.alloc("wbh", 128, [4, D], BF16)
            mT = A.alloc("mT", 128, [KC, T], BF16)
            wg = [A.alloc("wg%d" % i, 128, [KC, 128], BF16) for i in range(4)]
            ga_l = [A.alloc("ga%d" % i, 128, [512], F32) for i in range(2)]
            gh_l = [A.alloc("gh%d" % i, 128, [512], F32) for i in range(2)]
            m1_l = [A.alloc("m1%d" % i, 128, [512], F32) for i in range(2)]
            m2_l = [A.alloc("m2%d" % i, 128, [512], F32) for i in range(2)]
            P.dma("gpsimd", wba[0:64, :, :], wba_d.rearrange("(h d) n -> d h n", d=64), writes=[wba])
            P.dma("gpsimd", wbh[:], wbh_d.rearrange("(c p) n -> p c n", p=128), writes=[wbh])
            pc = 0
            for nch in range(8):
                wga, wgh = wg[(2 * nch) % 4], wg[(2 * nch + 1) % 4]
                wchunk_load(wga, GATE_BASE + nch * 128)
                wchunk_load(wgh, GATE_BASE + 1024 + nch * 128)
                ns = slice(nch * 128, (nch + 1) * 128)
                for tch in range(4):
                    ts_ = slice(tch * 512, (tch + 1) * 512)
                    p1, p2, p3, p4 = [psb[(pc + i) % 8] for i in range(4)]
                    pc += 4
                    ga, gh, m1, m2 = ga_l[(pc // 4) % 2], gh_l[(pc // 4) % 2], m1_l[(pc // 4) % 2], m2_l[(pc // 4) % 2]
                    for h in range(8):
                        mm(p1[:, :], wba[0:64, h, ns], attT[0:64, h, ts_], h == 0, h == 7, [wba, attT], [p1])
                    for cc in range(4):
                        mm(p2[:, :], wbh[:, cc, ns], hyT[:, cc, ts_], cc == 0, cc == 3, [wbh, hyT], [p2])
                    for kc in range(KC):
                        mm(p3[:, :], wga[:, kc, :], hxT[:, kc, ts_], kc == 0, kc == KC - 1, [wga, hxT], [p3])
                    for kc in range(KC):
                        mm(p4[:, :], wgh[:, kc, :], hxT[:, kc, ts_], kc == 0, kc == KC - 1, [wgh, hxT], [p4])
                    P.op("scalar", C("activation", out=ga[:], in_=p3[:, :], func=AF.Sigmoid), reads=[p3], writes=[ga])
                    P.op("scalar", C("activation", out=gh[:], in_=p4[:, :], func=AF.Sigmoid), reads=[p4], writes=[gh])
                    P.op("vector", C("tensor_tensor", out=m1[:], in0=p1[:, :], in1=ga[:], op=ALU.mult), reads=[p1, ga], writes=[m1])
                    P.op("vector", C("tensor_tensor", out=m2[:], in0=p2[:, :], in1=gh[:], op=ALU.mult), reads=[p2, gh], writes=[m2])
                    P.op("gpsimd", C("tensor_tensor", out=mT[:, nch, ts_], in0=m1[:], in1=m2[:], op=ALU.add), reads=[m1, m2], writes=[mT])
            phase_end(hxT, attT, hyT, wba, wbh, wg[0], wg[1], wg[2], wg[3], *ga_l, *gh_l, *m1_l, *m2_l)
            h2Tt = [A.alloc("h2Tt%d" % i, 128, [KC, 128], BF16) for i in range(2)]
            wo = A.alloc("wo", 128, [KC, D], BF16)
            P.dma("gpsimd", wo[:], wo_d.rearrange("(c p) n -> p c n", p=128), writes=[wo])
            rows = [A.alloc("row%d" % i, 128, [D], F32) for i in range(5)]
            xin = [A.alloc("xin%d" % i, 128, [D], F32) for i in range(4)]
            xn_l = [A.alloc("xn%d" % i, 128, [D], F32) for i in range(2)]
            xb_l = [A.alloc("xb%d" % i, 128, [D], BF16) for i in range(2)]
            xm_l = [A.alloc("xm%d" % i, 128, [D], F32) for i in range(2)]
            lg_l = [A.alloc("lg%d" % i, 128, [NE], F32) for i in range(2)]
            m8_l = [A.alloc("m8%d" % i, 128, [8], F32) for i in range(2)]
            msk_l = [A.alloc("msk%d" % i, 128, [NE], F32) for i in range(2)]
            ssum_l = [A.alloc("ssum%d" % i, 128, [1], F32) for i in range(2)]
            rw = A.alloc("rw", 128, [KC, NE], BF16)
            rbrow = A.alloc("rbrow", 128, [NE], F32)
            P.dma("gpsimd", rw[:], rw_d.rearrange("(c p) n -> p c n", p=128), writes=[rw])
            P.dma("sync", rbrow[:], rb_d[0, :].partition_broadcast(128), writes=[rbrow])
            load_row(rows[0], modrows_d[b, 2 * D:3 * D], reads=[modrows])
            load_row(rows[1], ln_d[0, :])
            load_row(rows[2], ln_d[1, :])
            load_row(rows[3], modrows_d[b, 4 * D:5 * D], reads=[modrows], plus1=True)
            load_row(rows[4], modrows_d[b, 3 * D:4 * D], reads=[modrows])
            def tileF2(tl):
                tsl = slice(tl * 128, (tl + 1) * 128)
                xn, xb, xm = xn_l[tl % 2], xb_l[tl % 2], xm_l[tl % 2]
                lg, m8, msk, ssum = lg_l[tl % 2], m8_l[tl % 2], msk_l[tl % 2], ssum_l[tl % 2]
                py = [psb[(tl % 2) * 2], psb[(tl % 2) * 2 + 1]]
                for n2 in range(2):
                    for kc in range(KC):
                        mm(py[n2][:, :], mT[:, kc, tsl], wo[:, kc, n2 * 512:(n2 + 1) * 512], kc == 0, kc == KC - 1, [mT, wo], [py[n2]])
                xi = xin[tl % 4]
                P.dma("sync", xi[:], x_d[b, tsl, :], writes=[xi])
                for n2 in range(2):
                    P.op("vector", C("tensor_tensor", out=xn[:, n2 * 512:(n2 + 1) * 512], in0=py[n2][:, :], in1=rows[0][:, n2 * 512:(n2 + 1) * 512], op=ALU.mult),
                         reads=[py[n2], rows[0]], writes=[xn])
                P.op("vector", C("scalar_tensor_tensor", out=xn[:], in0=xi[:], scalar=ALPHA, in1=xn[:], op0=ALU.mult, op1=ALU.add), reads=[xi, xn], writes=[xn])
                yield
                yield from layer_norm_g(xn, xm)
                yield from modulate_g(xm, rows[1], rows[2])
                P.dma("sync", xmid_d[b * T + tl * 128:b * T + (tl + 1) * 128, :], xm[:], reads=[xm])
                yield from layer_norm_g(xm, xn)
                yield from modulate_g(xn, rows[3], rows[4])
                P.op("vector", C("tensor_copy", out=xb[:], in_=xn[:]), reads=[xn], writes=[xb])
                yield
                P.dma("sync", h2d_d[b * T + tl * 128:b * T + (tl + 1) * 128, :], xb[:], reads=[xb])
                h2t = h2Tt[tl % 2]
                transpose_to(h2t[:], xb, psb[4 + tl % 2], [], [h2t])
                pl = psb[6 + tl % 2]
                for kc in range(KC):
                    mm(pl[:, 0:NE], h2t[:, kc, :], rw[:, kc, :], kc == 0, kc == KC - 1, [h2t, rw], [pl])
                P.op("vector", C("tensor_tensor", out=lg[:], in0=pl[:, 0:NE], in1=rbrow[:], op=ALU.add), reads=[pl, rbrow], writes=[lg])
                yield
                P.op("vector", C("max", out=m8[:], in_=lg[:]), reads=[lg], writes=[m8])
                yield
                P.op("vector", C("tensor_scalar", out=msk[:], in0=lg[:], scalar1=m8[:, 3:4], scalar2=None, op0=ALU.is_ge), reads=[lg, m8], writes=[msk])
                yield
                P.op("gpsimd", C("tensor_copy", out=maskall[:, b * NT + tl, :], in_=msk[:]), reads=[msk], writes=[maskall])
                yield
                P.op("vector", C("tensor_scalar", out=lg[:], in0=lg[:], scalar1=m8[:, 0:1], scalar2=None, op0=ALU.subtract), reads=[lg, m8], writes=[lg])
                yield
                P.op("scalar", C("activation", out=lg[:], in_=lg[:], func=AF.Exp), reads=[lg], writes=[lg])
                yield
                P.op("vector", C("tensor_tensor", out=lg[:], in0=lg[:], in1=msk[:], op=ALU.mult), reads=[lg, msk], writes=[lg])
                yield
                P.op("vector", C("reduce_sum", out=ssum[:], in_=lg[:], axis=mybir.AxisListType.X), reads=[lg], writes=[ssum])
                yield
                P.op("vector", C("reciprocal", out=ssum[:], in_=ssum[:]), reads=[ssum], writes=[ssum])
                yield
                P.op("vector", C("tensor_scalar", out=gates[:, b * NT + tl, :], in0=lg[:], scalar1=ssum[:, 0:1], scalar2=None, op0=ALU.mult), reads=[lg, ssum], writes=[gates])
                yield

            for tl in range(0, NT, 2):
                run_interleaved([tileF2(tl), tileF2(tl + 1)])
            if "gates" in dbg_d and b == 0:
                dbg("gates", gates[:], gates)
            phase_end(mT, wo, rw, rbrow, *xm_l, *lg_l, *m8_l, *msk_l, *ssum_l, *xin, *xn_l, *xb_l, *rows, *h2Tt)
            if stop_after == 6:
                P.finish(out_ops)
                P.emit()
                return nc

        ltri = A.alloc("ltri", 128, [128], F32)
        kk = A.alloc("kk", 128, [NBLK, NE], F32)
        tokid = A.alloc("tokid", 128, [NTT], F32)
        j4 = A.alloc("j4", 128, [NTT, 4], F32)
        Srun = A.alloc("Srun", 128, [NE], F32)
        rankall = A.alloc("rankall", 128, [NTT, NE], F32)
        cnt = A.alloc("cnt", 128, [NE], F32)
        ci = A.alloc("ci", 128, [NE], I32)
        cf = A.alloc("cf", 128, [NE], F32)
        cfx = A.alloc("cfx", 128, [NE], F32)
        padded = A.alloc("padded", 128, [NE], F32)
        cs_a = A.alloc("cs_a", 128, [NE], F32)
        cs_b = A.alloc("cs_b", 128, [NE], F32)
        pstart = A.alloc("pstart", 128, [NE], F32)
        destm = A.alloc("destm", 128, [NTT, NE], F32)
        d8 = A.alloc("d8", 128, [NTT, 8], F32)
        didx = A.alloc("didx", 128, [NTT, 4], I32)
        eqt = A.alloc("eqt", 128, [NTT, NE], F32)
        pay = A.alloc("pay", 128, [NTT, 4, 4], F32)
        cmpk = A.alloc("cmpk", 128, [NBLK, NE], F32)
        bexp = A.alloc("bexp", 128, [NBLK], F32)
        bexp1024 = A.alloc("bexp1024", 128, [NBLK], F32)
        P.dma("sync", ltri[:], ltri_d[:, :], writes=[ltri])
        P.dma("sync", kk[:], kk512_d.rearrange("p (k e) -> p k e", k=NBLK), writes=[kk])
        P.dma("sync", tokid[:], tokid_d[:, :], writes=[tokid])
        P.dma("sync", j4[:], j4_d.rearrange("p (a b) -> p a b", a=NTT), writes=[j4])
        P.dma("sync", slot_d[:, :], slotinit_d[:, :], writes=[slottab])
        P.op("vector", C("memset", Srun[:], 0.0), writes=[Srun])
        for tl in range(NTT):
            pr_ = psb[tl % 2]
            mm(pr_[:, 0:NE], ones_sq[:], Srun[:], True, False, [ones_sq, Srun], [pr_])
            mm(pr_[:, 0:NE], ltri[:], maskall[:, tl, :], False, True, [ltri, maskall], [pr_])
            evac(rankall[:, tl, :], pr_[:, 0:NE], [pr_], [rankall], eng="scalar")
            P.op("vector", C("tensor_tensor", out=Srun[:], in0=Srun[:], in1=maskall[:, tl, :], op=ALU.add), reads=[Srun, maskall], writes=[Srun])
        pcn = psb[2]
        mm(pcn[:, 0:NE], ones_sq[:], Srun[:], True, True, [ones_sq, Srun], [pcn])
        P.op("vector", C("tensor_copy", out=cnt[:], in_=pcn[:, 0:NE]), reads=[pcn], writes=[cnt])
        P.op("vector", C("tensor_scalar", out=cf[:], in0=cnt[:], scalar1=float(BLK - 1), scalar2=1.0 / BLK, op0=ALU.add, op1=ALU.mult), reads=[cnt], writes=[cf])
        P.op("vector", C("tensor_copy", out=ci[:], in_=cf[:]), reads=[cf], writes=[ci])
        P.op("vector", C("tensor_copy", out=cfx[:], in_=ci[:]), reads=[ci], writes=[cfx])
        P.op("vector", C("tensor_tensor", out=cf[:], in0=cfx[:], in1=cf[:], op=ALU.is_gt), reads=[cfx, cf], writes=[cf])
        P.op("vector", C("tensor_tensor", out=cfx[:], in0=cfx[:], in1=cf[:], op=ALU.subtract), reads=[cfx, cf], writes=[cfx])
        P.op("vector", C("tensor_scalar", out=padded[:], in0=cfx[:], scalar1=float(BLK), scalar2=None, op0=ALU.mult), reads=[cfx], writes=[padded])
        src_b, dst_b = padded, cs_a
        for sft in (1, 2, 4, 8, 16):
            P.op("vector", C("tensor_copy", out=dst_b[:, 0:sft], in_=src_b[:, 0:sft]), reads=[src_b], writes=[dst_b])
            P.op("vector", C("tensor_tensor", out=dst_b[:, sft:NE], in0=src_b[:, sft:NE], in1=src_b[:, 0:NE - sft], op=ALU.add), reads=[src_b], writes=[dst_b])
            src_b, dst_b = dst_b, (cs_b if dst_b is cs_a else cs_a)
        pend = src_b
        P.op("vector", C("tensor_tensor", out=pstart[:], in0=pend[:], in1=padded[:], op=ALU.subtract), reads=[pend, padded], writes=[pstart])
        P.op("vector", C("tensor_tensor", out=destm[:], in0=rankall[:], in1=pstart[:].unsqueeze(1).to_broadcast([128, NTT, NE]), op=ALU.add),
             reads=[rankall, pstart], writes=[destm])
        P.op("vector", C("scalar_tensor_tensor", out=destm[:], in0=destm[:], scalar=1.0, in1=maskall[:], op0=ALU.add, op1=ALU.mult), reads=[destm, maskall], writes=[destm])
        P.op("vector", C("tensor_scalar", out=destm[:], in0=destm[:], scalar1=-1.0, scalar2=None, op0=ALU.add), reads=[destm], writes=[destm])
        for tl in range(NTT):
            P.op("vector", C("max", out=d8[:, tl, :], in_=destm[:, tl, :]), reads=[destm], writes=[d8])
        P.op("vector", C("tensor_copy", out=didx[:], in_=d8[:, :, 0:4]), reads=[d8], writes=[didx])
        P.op("gpsimd", C("memset", pay[:], 0.0), writes=[pay])
        P.op("vector", C("tensor_copy", out=pay[:, :, :, 0], in_=tokid[:].unsqueeze(2).to_broadcast([128, NTT, 4])), reads=[tokid, pay], writes=[pay])
        P.op("vector", C("scalar_tensor_tensor", out=pay[:, :, :, 1], in0=tokid[:].unsqueeze(2).to_broadcast([128, NTT, 4]), scalar=4.0, in1=j4[:],
                         op0=ALU.mult, op1=ALU.add), reads=[tokid, j4, pay], writes=[pay])
        for j in range(4):
            P.op("vector", C("tensor_tensor", out=eqt[:], in0=destm[:], in1=d8[:, :, j:j + 1].to_broadcast([128, NTT, NE]), op=ALU.is_equal),
                 reads=[destm, d8], writes=[eqt])
            P.op("vector", C("tensor_tensor", out=eqt[:], in0=eqt[:], in1=gates[:], op=ALU.mult), reads=[eqt, gates], writes=[eqt])
            P.op("vector", C("tensor_reduce", out=pay[:, :, j, 2], in_=eqt[:], axis=mybir.AxisListType.X, op=ALU.add), reads=[eqt, pay], writes=[pay])
        P.op("vector", C("tensor_scalar", out=pay[:, :, :, 2], in0=pay[:, :, :, 2], scalar1=1.0 / 1.702, scalar2=None, op0=ALU.mult), reads=[pay], writes=[pay])
        for tl in range(NTT):
            for j in range(4):
                P._mk("gpsimd", C("indirect_dma_start", out=slot_d[:, :], out_offset=bass.IndirectOffsetOnAxis(ap=didx[:, tl, j:j + 1], axis=0),
                                   in_=pay[:, tl, j, :], in_offset=None), [didx, pay, slottab], [], True)
        P.op("vector", C("tensor_tensor", out=cmpk[:], in0=pend[:].unsqueeze(1).to_broadcast([128, NBLK, NE]), in1=kk[:], op=ALU.is_le), reads=[pend, kk], writes=[cmpk])
        P.op("vector", C("tensor_reduce", out=bexp[:], in_=cmpk[:], axis=mybir.AxisListType.X, op=ALU.add), reads=[cmpk], writes=[bexp])
        P.op("vector", C("tensor_scalar", out=bexp1024[:], in0=bexp[:], scalar1=1024.0, scalar2=None, op0=ALU.mult), reads=[bexp], writes=[bexp1024])
        if "slot" in dbg_d and b == 0:
            dbg("bexp", bexp[:], bexp)
            dbg("pend", pend[:], pend)
        phase_end(ltri, kk, tokid, j4, Srun, rankall, cnt, ci, cf, cfx, padded, cs_a, cs_b, pstart, destm, d8, didx, eqt, pay, cmpk, maskall, gates)
        if "slot" in dbg_d and b == 0:
            stmp = A.alloc("stmp", 128, [NSLOT // 128, 4], F32)
            P.dma("sync", stmp[:], slot_d.rearrange("(a p) c -> p a c", p=128), reads=[slottab], writes=[stmp])
            dbg("slot", stmp[:], stmp)
            A.release(stmp)
        if stop_after == 7:
            P.finish(out_ops)
            P.emit()
            return nc

        fcp = A.alloc("fcp", 128, [8], F32)
        ecol = A.alloc("ecol", 128, [1], F32)
        b1all = A.alloc("b1all", 128, [2048], BF16)
        b2all = A.alloc("b2all", 128, [D], BF16)
        b2f = A.alloc("b2f", 128, [D], F32)
        P.dma("sync", fcp[:], fcp_d[:, :], writes=[fcp])
        P.dma("sync", ecol[:], ecol_d[:, :], writes=[ecol])
        P.dma("gpsimd", b1all[0:NE, :], b1n_d[:, :], writes=[b1all])
        P.dma("sync", b2f[0:NE, :], b2_d[:, :], writes=[b2f])
        P.op("vector", C("tensor_scalar", out=b2all[0:NE, :], in0=b2f[0:NE, :], scalar1=1.702, scalar2=None, op0=ALU.mult), reads=[b2f], writes=[b2all])
        NU = 16
        w1u = [A.alloc("w1u%d" % i, 128, [KC, 2, 128], BF16) for i in range(NU)]
        w2b = [A.alloc("w2b%d" % i, 128, [8, D], BF16) for i in range(2)]
        xg = [A.alloc("xg%d" % i, 128, [JT, D], BF16) for i in range(2)]
        xgT = [A.alloc("xgT%d" % i, 128, [KC, BLK], BF16) for i in range(2)]
        actT = [A.alloc("actT%d" % i, 128, [8, BLK], BF16) for i in range(2)]
        ys = [A.alloc("ys%d" % i, 128, [D], F32) for i in range(4)]
        stb = [A.alloc("stb%d" % i, 128, [JT, 4], F32) for i in range(2)]
        gidx = [A.alloc("gidx%d" % i, 128, [JT], I32) for i in range(2)]
        sidx = [A.alloc("sidx%d" % i, 128, [JT], I32) for i in range(2)]
        widf_all = A.alloc("widf_all", 128, [NBLK, 8], F32)
        widx_all = A.alloc("widx_all", 128, [NBLK, 8], I32)
        ohb = [A.alloc("ohb%d" % i, 128, [BLK], BF16) for i in range(2)]
        tg = [A.alloc("tg%d" % i, 128, [512], F32) for i in range(2)]
        tsg = [A.alloc("tsg%d" % i, 128, [512], F32) for i in range(2)]
        tlin = [A.alloc("tlin%d" % i, 128, [512], F32) for i in range(2)]
        w1rows = w1_d[:, :]
        w2rows = w2_d[:, :]
        ucount = [0]
        blk_units = {}

        P.op("vector", C("tensor_tensor", out=widf_all[:], in0=fcp[:].unsqueeze(1).to_broadcast([128, NBLK, 8]),
                         in1=bexp1024[:].unsqueeze(2).to_broadcast([128, NBLK, 8]), op=ALU.add), reads=[fcp, bexp1024], writes=[widf_all])
        P.op("vector", C("tensor_copy", out=widx_all[:], in_=widf_all[:]), reads=[widf_all], writes=[widx_all])
        regcache = {}

        def wgather(out_ap, src_ap, idx_ap):
            def fn(e):
                if "bc" not in regcache:
                    regcache["bc"] = e.to_reg(NE * 1024 - 1)
                return e.indirect_dma_start(out=out_ap, out_offset=None, in_=src_ap, in_offset=bass.IndirectOffsetOnAxis(ap=idx_ap, axis=0),
                                            bounds_check=regcache["bc"], oob_is_err=False)
            return fn

        def issue_w2(k):
            pp = k % 2
            for fc in range(8):
                P._mk("gpsimd", wgather(w2b[pp][:, fc, :], w2rows, widx_all[:, ORDER[k], fc:fc + 1]), [widx_all], [w2b[pp]], True)

        def issue_unit(k, fc):
            pp = k % 2
            wu = w1u[ucount[0] % NU]
            ucount[0] += 1
            blk_units.setdefault(k, []).append(wu)
            P._mk("gpsimd", wgather(wu[:].rearrange("p k g f -> p (k g f)"), w1rows, widx_all[:, ORDER[k], fc:fc + 1]), [widx_all, wu], [wu], True)

        ORDER = [0, 1]
        lo_, hi_ = 2, NBLK - 1
        while lo_ <= hi_:
            ORDER.append(hi_)
            hi_ -= 1
            if lo_ <= hi_:
                ORDER.append(lo_)
                lo_ += 1
        assert sorted(ORDER) == list(range(NBLK))

        def moe_loads(k):
            pp = k % 2
            blk = ORDER[k]
            P.dma("sync", stb[pp][:], slot_d[blk * BLK:(blk + 1) * BLK, :].rearrange("(j p) c -> p j c", p=128), reads=[slottab], writes=[stb[pp]])
            P.op("gpsimd", C("tensor_copy", out=gidx[pp][:], in_=stb[pp][:, :, 0]), reads=[stb[pp]], writes=[gidx[pp]])
            P.op("gpsimd", C("tensor_copy", out=sidx[pp][:], in_=stb[pp][:, :, 1]), reads=[stb[pp]], writes=[sidx[pp]])
            P.op("gpsimd", C("tensor_scalar", out=ohb[pp][0:NE, :], in0=ecol[0:NE, 0:1].to_broadcast([NE, BLK]), scalar1=bexp[0:NE, blk:blk + 1], scalar2=None, op0=ALU.is_equal),
                 reads=[ecol, bexp], writes=[ohb[pp]])
            for j in range(JT):
                P._mk("gpsimd", C("indirect_dma_start", out=xg[pp][:, j, :], out_offset=None, in_=h2d_d[:, :],
                                   in_offset=bass.IndirectOffsetOnAxis(ap=gidx[pp][:, j:j + 1], axis=0)), [gidx[pp], h2rows], [xg[pp]], True)
            for fc in range(8):
                issue_unit(k, fc)
            issue_w2(k)

        pcs = [0]
        stp = [0]

        def moe_transposes(k):
            pp = k % 2
            for j in range(JT):
                pz = psb[j % 2]
                pzv = pz.ap.bitcast(BF16)
                for kc in range(KC):
                    P.op("tensor", C("transpose", out=pzv[:, kc * 128:(kc + 1) * 128], in_=xg[pp][:, j, kc * 128:(kc + 1) * 128], identity=ident_b[:]),
                         reads=[xg[pp], ident_b], writes=[pz])
                evac(xgT[pp][:, :, j * 128:(j + 1) * 128], pzv.rearrange("p (k t) -> p k t", k=KC), [pz], [xgT[pp]])

        def moe_compute(k):
            pp = k % 2
            us = blk_units[k]
            for fc in range(8):
                wu = us[fc]
                pg_, pl_ = psb[2 + (pcs[0] % 2) * 2], psb[3 + (pcs[0] % 2) * 2]
                pcs[0] += 1
                tg_, tsg_, tlin_ = tg[stp[0] % 2], tsg[stp[0] % 2], tlin[stp[0] % 2]
                stp[0] += 1
                for kc in range(KC):
                    mm(pg_[:, 0:BLK], wu[:, kc, 0, :], xgT[pp][:, kc, :], kc == 0, False, [wu, xgT[pp]], [pg_])
                mm(pg_[:, 0:BLK], b1all[0:NE, fc * 128:(fc + 1) * 128], ohb[pp][0:NE, :], False, True, [b1all, ohb[pp]], [pg_])
                for kc in range(KC):
                    mm(pl_[:, 0:BLK], wu[:, kc, 1, :], xgT[pp][:, kc, :], kc == 0, False, [wu, xgT[pp]], [pl_])
                mm(pl_[:, 0:BLK], b1all[0:NE, 1024 + fc * 128:1024 + (fc + 1) * 128], ohb[pp][0:NE, :], False, True, [b1all, ohb[pp]], [pl_])
                P.op("vector", C("tensor_scalar", out=tg_[:, 0:BLK], in0=pg_[:, 0:BLK], scalar1=7.0, scalar2=None, op0=ALU.min), reads=[pg_], writes=[tg_])
                if "g_tg" in dbg_d and b == 0 and k == 0 and fc == 0:
                    dbg("g_tg", tg_[:], tg_)
                    dq = A.alloc("dq", 128, [2048], F32)
                    P.op("vector", C("tensor_copy", out=dq[:], in_=wu[:].rearrange("p k g f -> p (k g f)")), reads=[wu], writes=[dq])
                    dbg("g_wu", dq[:], dq)
                    dq2 = A.alloc("dq2", 128, [512], F32)
                    P.op("vector", C("tensor_copy", out=dq2[:], in_=ohb[pp][:]), reads=[ohb[pp]], writes=[dq2])
                    dbg("g_ohb", dq2[:], dq2)
                    dq3 = A.alloc("dq3", 128, [8], F32)
                    P.op("vector", C("tensor_copy", out=dq3[:], in_=widx[pp][:]), reads=[widx[pp]], writes=[dq3])
                    dbg("g_widx", dq3[:], dq3)
                P.op("scalar", C("activation", out=tsg_[:, 0:BLK], in_=tg_[:, 0:BLK], func=AF.Silu, scale=1.702), reads=[tg_], writes=[tsg_])
                P.op("vector", C("tensor_scalar", out=tlin_[:, 0:BLK], in0=pl_[:, 0:BLK], scalar1=7.0, scalar2=-7.0, op0=ALU.min, op1=ALU.max), reads=[pl_], writes=[tlin_])
                P.op("vector", C("scalar_tensor_tensor", out=actT[pp][:, fc, :], in0=tlin_[:, 0:BLK], scalar=1.0, in1=tsg_[:, 0:BLK], op0=ALU.add, op1=ALU.mult),
                     reads=[tlin_, tsg_], writes=[actT[pp]])
            if "g_xgT" in dbg_d and b == 0 and k == 0:
                dtmp = A.alloc("dtmp", 128, [KC * BLK], F32)
                P.op("vector", C("tensor_copy", out=dtmp[:], in_=xgT[pp][:].rearrange("p a b -> p (a b)")), reads=[xgT[pp]], writes=[dtmp])
                dbg("g_xgT", dtmp[:], dtmp)
                dtmp2 = dtmp
                P.op("vector", C("tensor_copy", out=dtmp2[:], in_=actT[pp][:].rearrange("p a b -> p (a b)")), reads=[actT[pp]], writes=[dtmp2])
                dbg("g_actT", dtmp2[:], dtmp2)
            if k + 1 < NBLK:
                moe_transposes(k + 1)
            for j in range(JT):
                ysj = ys[j % 4]
                for n2 in range(2):
                    py_ = psb[(6, 7, 0, 1)[(j * 2 + n2) % 4]]
                    for fc in range(8):
                        mm(py_[:, :], actT[pp][:, fc, j * 128:(j + 1) * 128], w2b[pp][:, fc, n2 * 512:(n2 + 1) * 512], fc == 0, False, [actT[pp], w2b[pp]], [py_])
                    mm(py_[:, :], ohb[pp][0:NE, 0:128], b2all[0:NE, n2 * 512:(n2 + 1) * 512], False, True, [ohb[pp], b2all], [py_])
                    if n2 == 0:
                        P.op("vector", C("tensor_scalar", out=ysj[:, 0:512], in0=py_[:, :], scalar1=stb[pp][:, j, 2:3], scalar2=None, op0=ALU.mult),
                             reads=[py_, stb[pp]], writes=[ysj])
                    else:
                        P.op("vector", C("tensor_scalar", out=ysj[:, 512:1024], in0=py_[:, :], scalar1=stb[pp][:, j, 2:3], scalar2=None, op0=ALU.mult),
                             reads=[py_, stb[pp]], writes=[ysj])
                if "g_ys" in dbg_d and b == 0 and k == 0 and j == 0:
                    dbg("g_ys", ysj[:], ysj)
                P._mk("gpsimd", C("indirect_dma_start", out=out4_d[:, :], out_offset=bass.IndirectOffsetOnAxis(ap=sidx[pp][:, j:j + 1], axis=0),
                                   in_=ysj[:], in_offset=None), [sidx[pp], ysj], [], True)

        moe_loads(0)
        moe_transposes(0)
        for k in range(NBLK):
            if k + 1 < NBLK:
                moe_loads(k + 1)
            moe_compute(k)
        phase_end(fcp, ecol, b1all, b2all, b2f, *w1u, *w2b, *xg, *xgT, *actT, *ys, *stb, *gidx, *sidx, widf_all, widx_all, *ohb, *tg, *tsg, *tlin, bexp, bexp1024)
        if stop_after == 8:
            P.finish(out_ops)
            P.emit()
            return nc

        xo = [A.alloc("xo%d" % i, 128, [D], F32) for i in range(4)]
        rows = [A.alloc("row%d" % i, 128, [D], F32) for i in range(4)]
        xin = [A.alloc("xin%d" % i, 128, [D], F32) for i in range(4)]
        xn_l = [A.alloc("xn%d" % i, 128, [D], F32) for i in range(4)]
        o4 = [A.alloc("o4%d" % i, 128, [4, D], F32) for i in range(4)]
        load_row(rows[0], modrows_d[0, 5 * D:6 * D], reads=[modrows])
        load_row(rows[3], modrows_d[1, 5 * D:6 * D], reads=[modrows])
        load_row(rows[1], ln_d[2, :])
        load_row(rows[2], ln_d[3, :])
        def tileH(b, tl):
            g2row = rows[0] if b == 0 else rows[3]
            xi = xin[tl % 4]
            o4t = o4[tl % 4]
            r0_ = (b * T + tl * 128) * 4
            P.dma("sync", xi[:], xmid_d[b * T + tl * 128:b * T + (tl + 1) * 128, :], reads=[xmid[b]], writes=[xi])
            P.dma("sync", o4t[:], out4_d[r0_:r0_ + 512, :].rearrange("(p j) n -> p j n", j=4), writes=[o4t])
            xn = xn_l[tl % 4]
            P.op("vector", C("tensor_tensor", out=o4t[:, 0, :], in0=o4t[:, 0, :], in1=o4t[:, 1, :], op=ALU.add), reads=[o4t], writes=[o4t])
            yield
            P.op("vector", C("tensor_tensor", out=o4t[:, 2, :], in0=o4t[:, 2, :], in1=o4t[:, 3, :], op=ALU.add), reads=[o4t], writes=[o4t])
            yield
            P.op("vector", C("tensor_tensor", out=o4t[:, 0, :], in0=o4t[:, 0, :], in1=o4t[:, 2, :], op=ALU.add), reads=[o4t], writes=[o4t])
            yield
            if "fx" in dbg_d and b == 0:
                out_ops.append(P.dma("sync", dbg_d["fx"][tl * 128:(tl + 1) * 128, :], o4t[:, 0, :], reads=[o4t]))
            P.op("vector", C("tensor_tensor", out=xn[:], in0=o4t[:, 0, :], in1=g2row[:], op=ALU.mult), reads=[o4t, g2row], writes=[xn])
            yield
            P.op("vector", C("scalar_tensor_tensor", out=xn[:], in0=xi[:], scalar=ALPHA, in1=xn[:], op0=ALU.mult, op1=ALU.add), reads=[xi, xn], writes=[xn])
            yield
            xot = xo[tl % 4]
            yield from layer_norm_g(xn, xot)
            P.op("vector", C("tensor_tensor", out=xot[:], in0=xot[:], in1=rows[1][:], op=ALU.mult), reads=[xot, rows[1]], writes=[xot])
            yield
            P.op("vector", C("tensor_tensor", out=xot[:], in0=xot[:], in1=rows[2][:], op=ALU.add), reads=[xot, rows[2]], writes=[xot])
            yield
            o = P.dma("sync", out_d[b, tl * 128:(tl + 1) * 128, :], xot[:], reads=[xot])
            out_ops.append(o)

        for b in range(2):
            for tl in range(0, NT, 4):
                run_interleaved([tileH(b, tl + q) for q in range(4)])
        phase_end(*xo, *xin, *xn_l, *rows, *o4)
        P.finish(out_ops)
        P.emit()
    return nc


def _prep_shared(inp):
    f32 = np.float32
    cst = _consts()
    cols = _ext_cols()
    w_in_ext = np.ascontiguousarray(inp["w_in"][0][:, cols]).astype(f32)
    convw = inp["hy_conv_w"][0]
    convb = inp["hy_conv_b"][0]
    cp = np.concatenate([convw, convb[None]], axis=0).reshape(4, 12, 128).transpose(2, 0, 1)
    skipp = inp["hy_skip"][0].reshape(2, 4, 128).transpose(2, 0, 1)
    w1 = inp["exp_w1"][0]
    w1r = np.ascontiguousarray(w1.reshape(NE, 8, 128, 2, 8, 128).transpose(0, 4, 2, 1, 3, 5)).reshape(NE * 8 * 128, 2048)
    b1 = inp["exp_b1"][0]
    b1r = np.ascontiguousarray(b1.reshape(NE, 16, 128).transpose(2, 0, 1)).reshape(128, NE * 16)
    lnp = np.stack([inp["ln1_g"][0], inp["ln1_b"][0], inp["ln2_g"][0], inp["ln2_b"][0]], axis=0)
    sh = {
        "w_mod": inp["w_mod"][0], "b_mod": inp["b_mod"][0][None, :], "w_in": w_in_ext,
        "attn_sink": inp["attn_sink"][0][None, :],
        "convp": np.ascontiguousarray(cp).reshape(128, 48), "skipp": np.ascontiguousarray(skipp).reshape(128, 8),
        "fw1": inp["hy_filt_w1"][0], "fb1": inp["hy_filt_b1"][0][:, None], "fw2": inp["hy_filt_w2"][0],
        "fb2": inp["hy_filt_b2"][0][:, None], "fw3": inp["hy_filt_w3"][0],
        "w_ba": inp["w_branch_attn"][0], "w_bh": inp["w_branch_hyena"][0], "w_o": inp["w_out"][0],
        "lnp": lnp, "router_w": inp["router_w"][0], "router_b": inp["router_b"][0][None, :],
        "w1r": w1r, "b1r": b1r, "w2": inp["exp_w2"][0].reshape(NE * D, D), "b2": inp["exp_b2"][0],
        "ropeC": cst["ropeC"], "ropeS": cst["ropeS"], "masks": cst["masks"].reshape(128, 256), "zT": cst["zT"],
        "decay": cst["decay"], "decayb": cst["decayb"],
        "cft": cst["cft"], "sft": cst["sft"], "cit": cst["cit"], "sit": cst["sit"],
        "ltri": cst["ltri"], "kk512": cst["kk512"], "fcp": cst["fcp"], "ecol": cst["ecol"], "tokid": cst["tokid"], "j4": cst["j4"],
        "slot_init": cst["slot_init"], "b1n": inp["exp_b1"][0],
    }
    return {k: np.ascontiguousarray(v) for k, v in sh.items()}


def _run(inp, stop_after=None, cores=NCORES):
    inp = {k: np.asarray(v) for k, v in inp.items()}
    shared = _prep_shared(inp)
    nc = build(stop_after)
    in_maps = []
    for i in range(cores):
        m = dict(shared)
        m["x"] = np.ascontiguousarray(inp["x"][2 * i:2 * i + 2])
        m["ctx"] = np.ascontiguousarray(inp["ctx"][2 * i:2 * i + 2])
        m["c3"] = np.ascontiguousarray(np.concatenate([inp["c"][2 * i:2 * i + 2], inp["c_ctx"][None, :]], axis=0))
        in_maps.append(m)
    res = run_bass_kernel_spmd(nc, in_maps, core_ids=list(range(cores)))
    return res


def kernel(**inputs):
    res = _run(inputs)
    out = np.concatenate([r["out"] for r in res.results], axis=0)
    return out.astype(np.float32)
```

```python
import contextlib
import math
import numpy as np
import ml_dtypes
import concourse.bass as bass
import concourse.mybir as mybir
from concourse.bass_utils import run_bass_kernel_spmd

F32 = mybir.dt.float32
BF16 = mybir.dt.bfloat16
I32 = mybir.dt.int32
AF = mybir.ActivationFunctionType
ALU = mybir.AluOpType

ENGS = ["tensor", "vector", "scalar", "gpsimd", "sync"]
NCORES = 8
T = 2048
NT = 16
D = 1024
KC = 8
NE = 32
EXTW = 4992
GATE_BASE = 2944
HY_BASE = 1408
NBLK = 63
BLK = 512
JT = BLK // 128
NTT = 2 * NT
TT = 2 * T
NSLOT = NBLK * BLK
ALPHA = 2 ** 0.25
PI = math.pi
DEBUG = {}


def C(method, *a, **k):
    return lambda e: getattr(e, method)(*a, **k)


class Buf:
    __slots__ = ("ap", "name", "last_w", "readers", "off", "size")

    def __init__(self, ap, name=""):
        self.ap = ap
        self.name = name
        self.last_w = None
        self.readers = []

    def __getitem__(self, k):
        return self.ap[k]


class Op:
    __slots__ = ("id", "eng", "fn", "waits", "is_dma", "dsem", "dval", "needed", "val", "idx")


class Prog:
    NDSEM = 40

    def __init__(self, nc):
        self.nc = nc
        self.ops = []
        self.q = {e: [] for e in ENGS}
        self.dma_rr = 0
        self.dsem_last = [None] * self.NDSEM
        self.dsem_uses = [0] * self.NDSEM
        self.dma_since_barrier = []

    def _mk(self, eng, fn, reads, writes, is_dma):
        op = Op()
        op.id = len(self.ops)
        op.eng = eng
        op.fn = fn
        op.is_dma = is_dma
        op.needed = False
        op.val = None
        op.dsem = None
        op.dval = None
        deps = set()
        for b in reads:
            if b.last_w is not None:
                deps.add(b.last_w)
        for b in writes:
            if b.last_w is not None:
                deps.add(b.last_w)
            for r in b.readers:
                deps.add(r)
        if is_dma:
            k = self.dma_rr % self.NDSEM
            self.dma_rr += 1
            if self.dsem_last[k] is not None:
                deps.add(self.dsem_last[k])
            self.dsem_last[k] = op.id
            self.dsem_uses[k] += 1
            op.dsem = k
            op.dval = 16 * self.dsem_uses[k]
            self.dma_since_barrier.append(op.id)
        op.waits = deps
        self.ops.append(op)
        op.idx = len(self.q[eng])
        self.q[eng].append(op)
        for b in reads:
            b.readers.append(op.id)
        for b in writes:
            b.last_w = op.id
            b.readers = []
        return op

    def op(self, eng, fn, reads=(), writes=()):
        return self._mk(eng, fn, reads, writes, False)

    def dma(self, eng, out, in_, reads=(), writes=(), **kw):
        return self._mk(eng, C("dma_start", out=out, in_=in_, **kw), reads, writes, True)

    def barrier(self):
        last = [self.q[e][-1].id for e in ENGS if self.q[e] and not self.q[e][-1].is_dma]
        last = []
        for e in ENGS:
            for o in reversed(self.q[e]):
                if not o.is_dma and o.fn is not None:
                    last.append(o.id)
                    break
        dmas = list(self.dma_since_barrier)
        self.dma_since_barrier = []
        for e in ENGS:
            o = self._mk(e, None, (), (), False)
            o.waits = set(last) | set(dmas)

    def finish(self, out_ops):
        o = self._mk("sync", None, (), (), False)
        o.waits = set(x.id for x in out_ops)

    def emit(self):
        nc = self.nc
        ops = self.ops
        final_waits = {}
        for e in ENGS:
            seen = {}
            seen_d = {}
            for op in self.q[e]:
                wl = []
                for d in sorted(op.waits):
                    t = ops[d]
                    if t.is_dma:
                        if seen_d.get(t.dsem, 0) >= t.dval:
                            continue
                        seen_d[t.dsem] = t.dval
                        wl.append(d)
                    else:
                        if t.fn is None:
                            continue
                        if t.eng == e and e == "tensor":
                            continue
                        if seen.get(t.eng, -1) >= t.idx:
                            continue
                        seen[t.eng] = t.idx
                        wl.append(d)
                        t.needed = True
                final_waits[op.id] = wl
        for e in ENGS:
            c = 0
            for op in self.q[e]:
                if op.is_dma:
                    continue
                if op.needed:
                    c += 1
                    op.val = c
        with contextlib.ExitStack() as st:
            esem = {e: st.enter_context(nc.semaphore("s_" + e)) for e in ENGS}
            dsem = [st.enter_context(nc.semaphore("d%d" % i)) for i in range(self.NDSEM)]
            block = st.enter_context(nc.Block())

            def run(e, eng):
                for op in self.q[e]:
                    for d in final_waits[op.id]:
                        t = ops[d]
                        if t.is_dma:
                            eng.wait_ge(dsem[t.dsem], t.dval)
                        else:
                            eng.wait_ge(esem[t.eng], t.val)
                    if op.fn is None:
                        continue
                    ins = op.fn(eng)
                    if op.is_dma:
                        ins.then_inc(dsem[op.dsem], 16)
                    elif op.needed:
                        ins.then_inc(esem[e], 1)

            @block.tensor
            def _(eng):
                run("tensor", eng)

            @block.vector
            def _(eng):
                run("vector", eng)

            @block.scalar
            def _(eng):
                run("scalar", eng)

            @block.gpsimd
            def _(eng):
                run("gpsimd", eng)

            @block.sync
            def _(eng):
                run("sync", eng)


class Arena:
    def __init__(self, big_ap, nbytes):
        self.big = big_ap
        self.free = [(0, nbytes)]
        self.pending = []
        self.live = []

    def alloc(self, name, parts, shape, dtype):
        esz = 4 if dtype in (F32, I32) else 2
        n = int(np.prod(shape))
        size = (n * esz + 63) // 64 * 64
        for i, (o, s) in enumerate(self.free):
            if s >= size:
                if s == size:
                    self.free.pop(i)
                else:
                    self.free[i] = (o + size, s - size)
                ap = self.big[0:parts, o // 2:(o + n * esz) // 2]
                if dtype != BF16:
                    ap = ap.bitcast(dtype)
                if len(shape) > 1:
                    names = " ".join("a%d" % i for i in range(len(shape)))
                    kw = {"a%d" % i: shape[i] for i in range(1, len(shape))}
                    ap = ap.rearrange("p (%s) -> p %s" % (names, names), **kw)
                b = Buf(ap, name)
                b.off = o
                b.size = size
                for (lo, ls, ln) in self.live:
                    if o < lo + ls and lo < o + size:
                        raise RuntimeError("OVERLAP %s with %s" % (name, ln))
                self.live.append((o, size, name))
                return b
        raise RuntimeError("arena OOM for %s (%d bytes); free=%s" % (name, size, self.free))

    def release(self, *bufs):
        for b in bufs:
            self.pending.append((b.off, b.size))
            k = [x for x in self.live if x[0] == b.off and x[1] == b.size]
            if len(k) != 1:
                raise RuntimeError("bad release %s %s" % (b.name, k))
            self.live.remove(k[0])

    def commit(self):
        fl = self.free + self.pending
        self.pending = []
        fl.sort()
        out = []
        for o, s in fl:
            if out and out[-1][0] + out[-1][1] == o:
                out[-1] = (out[-1][0], out[-1][1] + s)
            else:
                out.append((o, s))
        self.free = out


_CONST_CACHE = {}


def _consts():
    if _CONST_CACHE:
        return _CONST_CACHE
    L = T
    t = np.arange(L)
    row = (t // 64).astype(np.float32)
    col = (t % 64).astype(np.float32)
    inv = (np.float32(10000.0) ** (-np.arange(16, dtype=np.float32) / np.float32(16))).astype(np.float32)
    d = np.arange(64)
    a = d // 32
    half = (d % 32) // 16
    f = d % 16
    pos = np.where(a[:, None] == 0, row[None, :], col[None, :]).astype(np.float32)
    ang = (pos * inv[f][:, None]).astype(np.float32).astype(np.float64)
    C = np.cos(ang)
    S = np.sin(ang) * np.where(half == 0, -1.0, 1.0)[:, None]
    ropeC = np.tile(C, (2, 1)).astype(np.float32)
    ropeS = np.tile(S, (2, 1)).astype(np.float32)
    s_i = np.arange(128)[:, None]
    q_i = np.arange(128)[None, :]
    maskP = (q_i <= s_i).astype(np.float32)
    maskN = (s_i <= q_i).astype(np.float32)
    masks = np.stack([maskP, maskN], axis=1).astype(ml_dtypes.bfloat16)
    tl = np.linspace(0.0, 1.0, L, dtype=np.float32)
    omega = (np.float32(2.0 * math.pi) * np.arange(L, dtype=np.float32) / np.float32(L)).astype(np.float32)
    fb = np.linspace(1e-4, 15, 16, dtype=np.float32)
    fo = (fb[None, :] * omega[:, None]).astype(np.float32).astype(np.float64)
    z = np.concatenate([tl[:, None].astype(np.float64), np.cos(fo), -np.sin(fo)], axis=-1)
    zT = np.ascontiguousarray(z.T).astype(np.float32)
    min_decay = math.log(1e-2) / 0.3
    max_decay = math.log(1e-2) / 1.5
    deltas = np.abs(np.linspace(min_decay, max_decay, 512, dtype=np.float32)).astype(np.float64)
    decay = np.exp(-tl[:, None].astype(np.float64) * deltas[None, :]).astype(np.float32)
    decayb = decay.copy()
    decayb[0] = 0.0
    N = 2 * L
    tt = np.arange(L, dtype=np.float64)
    ff = np.arange(L, dtype=np.float64) + 0.5
    th = 2.0 * math.pi * np.outer(tt, ff) / N
    Cf = np.cos(th)
    Sf = np.sin(th)
    bf = ml_dtypes.bfloat16

    def tile_fwd(M):
        return np.ascontiguousarray(M.reshape(16, 128, 16, 128).transpose(2, 1, 0, 3)).astype(bf).reshape(16, 128, 2048)

    def tile_inv(M):
        return np.ascontiguousarray(M.reshape(16, 128, 4, 512).transpose(2, 1, 0, 3)).astype(bf).reshape(4, 128, 8192)

    pp_ = np.arange(128)
    ltri = (pp_[:, None] < pp_[None, :]).astype(np.float32)
    kk512 = np.tile(np.repeat(np.arange(NBLK, dtype=np.float32) * BLK, NE)[None, :], (128, 1))
    fcp = (np.arange(8)[None, :] * 128 + pp_[:, None]).astype(np.float32)
    ecol = pp_[:, None].astype(np.float32)
    tokid = (np.arange(NTT)[None, :] * 128 + pp_[:, None]).astype(np.float32)
    j4 = np.tile(np.tile(np.arange(4, dtype=np.float32), NTT)[None, :], (128, 1))
    sl = np.arange(NSLOT)
    slot_init = np.stack([TT + sl % 128, 4 * TT + sl % 128, np.zeros(NSLOT), np.zeros(NSLOT)], axis=1).astype(np.float32)
    _CONST_CACHE.update(dict(
        ltri=ltri, kk512=np.ascontiguousarray(kk512), fcp=fcp, ecol=ecol, tokid=tokid, j4=np.ascontiguousarray(j4), slot_init=slot_init,
        ropeC=ropeC, ropeS=ropeS, masks=masks, zT=zT, decay=decay, decayb=decayb,
        cft=tile_fwd(Cf), sft=tile_fwd(Sf),
        cit=tile_inv(Cf.T * (2.0 / N)), sit=tile_inv(Sf.T * (2.0 / N)),
    ))
    return _CONST_CACHE


def _ext_cols():
    q_cols, qp_cols = [], []
    for c in range(4):
        for hf in range(2):
            h = c + 4 * hf
            for dd in range(64):
                q_cols.append(h * 64 + dd)
                qp_cols.append(h * 64 + (dd ^ 16))
    k_cols = [512 + i for i in range(128)]
    kp_cols = [512 + (i // 64) * 64 + ((i % 64) ^ 16) for i in range(128)]
    v_cols = [640 + i for i in range(128)]
    rest = list(range(768, 4352))
    return np.array(q_cols + qp_cols + k_cols + kp_cols + v_cols + rest)


def build(stop_after=None):
    nc = bass.Bass("TRN2", target_bir_lowering=False)

    def din(name, shape, dt=F32):
        return nc.dram_tensor(name, list(shape), dt, kind="ExternalInput").ap()

    x_d = din("x", [2, T, D])
    c3_d = din("c3", [3, D])
    ctx_d = din("ctx", [2, 256, D])
    wmod_d = din("w_mod", [D, 6144])
    bmod_d = din("b_mod", [1, 6144])
    win_d = din("w_in", [D, EXTW])
    sink_d = din("attn_sink", [1, 8])
    convp_d = din("convp", [128, 48])
    skip_d = din("skipp", [128, 8])
    fw1_d = din("fw1", [33, 64])
    fb1_d = din("fb1", [64, 1])
    fw2_d = din("fw2", [64, 64])
    fb2_d = din("fb2", [64, 1])
    fw3_d = din("fw3", [64, 2048])
    wba_d = din("w_ba", [512, D])
    wbh_d = din("w_bh", [512, D])
    wo_d = din("w_o", [D, D])
    ln_d = din("lnp", [4, D])
    rw_d = din("router_w", [D, NE])
    rb_d = din("router_b", [1, NE])
    w1_d = din("w1r", [NE * 8 * 128, 2048])
    b1_d = din("b1r", [128, NE * 16])
    w2_d = din("w2", [NE * D, D])
    b2_d = din("b2", [NE, D])
    ropeC_d = din("ropeC", [128, T])
    ropeS_d = din("ropeS", [128, T])
    masks_d = din("masks", [128, 256], BF16)
    zT_d = din("zT", [33, T])
    decay_d = din("decay", [T, 512])
    decayb_d = din("decayb", [T, 512])
    cft_d = din("cft", [16, 128, 2048], BF16)
    sft_d = din("sft", [16, 128, 2048], BF16)
    cit_d = din("cit", [4, 128, 8192], BF16)
    sit_d = din("sit", [4, 128, 8192], BF16)
    ltri_d = din("ltri", [128, 128])
    kk512_d = din("kk512", [128, NBLK * NE])
    fcp_d = din("fcp", [128, 8])
    ecol_d = din("ecol", [128, 1])
    tokid_d = din("tokid", [128, NTT])
    j4_d = din("j4", [128, NTT * 4])
    slotinit_d = din("slot_init", [NSLOT, 4])
    b1n_d = din("b1n", [NE, 2048])
    out_d = nc.dram_tensor("out", [2, T, D], F32, kind="ExternalOutput").ap()
    dbg_d = {k: nc.dram_tensor("dbg_" + k, list(s), F32, kind="ExternalOutput").ap() for k, s in DEBUG.items()}

    modrows_d = nc.dram_tensor("modrows", [3, 6144], F32).ap()
    xmid_d = nc.dram_tensor("xmid", [2 * T, D], F32).ap()
    hr_d = nc.dram_tensor("hr_s", [2, T, 512], BF16).ap()
    hs_d = nc.dram_tensor("hs_s", [2, T, 512], BF16).ap()
    hxs_d = nc.dram_tensor("hx_s", [128, KC * T], BF16).ap()
    atts_d = nc.dram_tensor("att_s", [64, 8 * T], BF16).ap()
    h2d_d = nc.dram_tensor("h2_rows", [TT + 128, D], BF16).ap()
    slot_d = nc.dram_tensor("slot_tab", [NSLOT, 4], F32).ap()
    out4_d = nc.dram_tensor("out4", [4 * TT + 128, D], F32).ap()
    h2rows = Buf(h2d_d, "h2rows")
    slottab = Buf(slot_d, "slottab")
    modrows = Buf(modrows_d, "modrows")
    xmid = [Buf(xmid_d, "xmid0"), Buf(xmid_d, "xmid1")]
    hspec = Buf(hr_d, "hspec")

    ARENA_BYTES = 206 * 1024
    st = contextlib.ExitStack()
    with st:
        P = Prog(nc)
        big = st.enter_context(nc.sbuf_tensor("arena", [128, ARENA_BYTES // 2], BF16))
        A = Arena(big, ARENA_BYTES)
        psb = [Buf(st.enter_context(nc.psum_tensor("ps%d" % i, [128, 512], F32)), "ps%d" % i) for i in range(8)]
        out_ops = []

        def phase_end(*bufs):
            A.release(*bufs)
            P.barrier()
            A.commit()

        cpy_rr = [0]

        def evac(out_ap, in_ap, reads, writes, eng=None):
            if eng is None:
                eng = ("scalar", "vector")[cpy_rr[0] % 2]
                cpy_rr[0] += 1
            if eng == "scalar":
                P.op("scalar", C("activation", out=out_ap, in_=in_ap, func=AF.Copy), reads=reads, writes=writes)
            else:
                P.op(eng, C("tensor_copy", out=out_ap, in_=in_ap), reads=reads, writes=writes)

        def mm(out_ap, lhsT, rhs, start, stop, reads, writes):
            P.op("tensor", C("matmul", out_ap, lhsT=lhsT, rhs=rhs, start=start, stop=stop), reads=reads, writes=writes)

        ident_f = A.alloc("ident_f", 128, [128], F32)
        ident_b = A.alloc("ident_b", 128, [128], BF16)
        ones_b = A.alloc("ones_b", 128, [64], BF16)
        ones_f = A.alloc("ones_f", 128, [128], F32)
        lnst_l = [A.alloc("lnst%d" % i, 128, [2, 6], F32) for i in range(4)]
        lnmv_l = [A.alloc("lnmv%d" % i, 128, [2], F32) for i in range(4)]
        lnrs_l = [A.alloc("lnrs%d" % i, 128, [1], F32) for i in range(4)]
        ln_rr = [0]
        P.op("gpsimd", C("memset", ident_f[:], 1.0), writes=[ident_f])
        P.op("gpsimd", C("affine_select", out=ident_f[:], in_=ident_f[:], pattern=[[-1, 128]], compare_op=ALU.is_equal,
                                                  fill=0.0, base=0, channel_multiplier=1), reads=[ident_f], writes=[ident_f])
        P.op("vector", C("tensor_copy", out=ident_b[:], in_=ident_f[:]), reads=[ident_f], writes=[ident_b])
        P.op("gpsimd", C("memset", ones_b[:], 1.0), writes=[ones_b])
        P.op("gpsimd", C("memset", ones_f[:], 1.0), writes=[ones_f])
        ones_sq = ones_f
        zrow = A.alloc("zrow", 128, [D], BF16)
        P.op("gpsimd", C("memset", zrow[:], 0.0), writes=[zrow])
        P.dma("sync", h2d_d[TT:TT + 128, :], zrow[:], reads=[zrow])

        def layer_norm(src, dst, reads_extra=()):
            lnst, lnmv, lnrs = lnst_l[ln_rr[0] % 4], lnmv_l[ln_rr[0] % 4], lnrs_l[ln_rr[0] % 4]
            ln_rr[0] += 1
            for i in range(2):
                P.op("vector", C("bn_stats", out=lnst[:, i, :], in_=src[:, i * 512:(i + 1) * 512]), reads=[src], writes=[lnst])
            P.op("vector", C("bn_aggr", out=lnmv[:], in_=lnst[:]), reads=[lnst], writes=[lnmv])
            P.op("scalar", C("activation", out=lnrs[:], in_=lnmv[:, 1:2], func=AF.Sqrt, bias=1e-5), reads=[lnmv], writes=[lnrs])
            P.op("vector", C("reciprocal", out=lnrs[:], in_=lnrs[:]), reads=[lnrs], writes=[lnrs])
            P.op("vector", C("tensor_scalar", out=dst[:], in0=src[:], scalar1=lnmv[:, 0:1], scalar2=lnrs[:, 0:1],
                                                     op0=ALU.subtract, op1=ALU.mult), reads=[src, lnmv, lnrs], writes=[dst])

        def layer_norm_g(src, dst):
            lnst, lnmv, lnrs = lnst_l[ln_rr[0] % 4], lnmv_l[ln_rr[0] % 4], lnrs_l[ln_rr[0] % 4]
            ln_rr[0] += 1
            for i in range(2):
                P.op("vector", C("bn_stats", out=lnst[:, i, :], in_=src[:, i * 512:(i + 1) * 512]), reads=[src], writes=[lnst])
            yield
            P.op("vector", C("bn_aggr", out=lnmv[:], in_=lnst[:]), reads=[lnst], writes=[lnmv])
            yield
            P.op("scalar", C("activation", out=lnrs[:], in_=lnmv[:, 1:2], func=AF.Sqrt, bias=1e-5), reads=[lnmv], writes=[lnrs])
            yield
            P.op("vector", C("reciprocal", out=lnrs[:], in_=lnrs[:]), reads=[lnrs], writes=[lnrs])
            yield
            P.op("vector", C("tensor_scalar", out=dst[:], in0=src[:], scalar1=lnmv[:, 0:1], scalar2=lnrs[:, 0:1],
                             op0=ALU.subtract, op1=ALU.mult), reads=[src, lnmv, lnrs], writes=[dst])
            yield

        def modulate_g(t, scrow, shrow):
            P.op("gpsimd", C("tensor_tensor", out=t[:], in0=t[:], in1=scrow[:], op=ALU.mult), reads=[t, scrow], writes=[t])
            yield
            P.op("vector", C("tensor_tensor", out=t[:], in0=t[:], in1=shrow[:], op=ALU.add), reads=[t, shrow], writes=[t])
            yield

        def run_interleaved(gens):
            active = list(gens)
            while active:
                nxt = []
                for g_ in active:
                    try:
                        next(g_)
                        nxt.append(g_)
                    except StopIteration:
                        pass
                active = nxt

        def load_row(buf, dram_row_ap, reads=(), plus1=False):
            P.dma("sync", buf[:], dram_row_ap.partition_broadcast(128), reads=reads, writes=[buf])
            if plus1:
                P.op("gpsimd", C("tensor_scalar", out=buf[:], in0=buf[:], scalar1=1.0, scalar2=None, op0=ALU.add), reads=[buf], writes=[buf])

        def modulate(t, scrow, shrow):
            P.op("gpsimd", C("tensor_tensor", out=t[:], in0=t[:], in1=scrow[:], op=ALU.mult), reads=[t, scrow], writes=[t])
            P.op("vector", C("tensor_tensor", out=t[:], in0=t[:], in1=shrow[:], op=ALU.add), reads=[t, shrow], writes=[t])

        def transpose_to(dst_ap, src_bf, pbank, reads, writes):
            pv = pbank.ap.bitcast(BF16)
            for kc in range(KC):
                P.op("tensor", C("transpose", out=pv[:, kc * 128:(kc + 1) * 128], in_=src_bf[:, kc * 128:(kc + 1) * 128],
                                                            identity=ident_b[:]), reads=[src_bf, ident_b], writes=[pbank])
            evac(dst_ap, pv.rearrange("p (k t) -> p k t", k=KC), reads=[pbank] + list(reads), writes=writes)

        def dbg(name, sb_ap, buf, dst=None):
            if name in dbg_d:
                o = P.dma("sync", dbg_d[name] if dst is None else dst, sb_ap, reads=[buf])
                out_ops.append(o)

        c3 = A.alloc("c3", 128, [D], F32)
        sT = A.alloc("sT", 128, [KC, 3], F32)
        bmr = A.alloc("bmr", 128, [6144], F32)
        mrow = A.alloc("mrow", 128, [6144], F32)
        wm = [A.alloc("wm%d" % i, 128, [KC, 512], F32) for i in range(2)]
        P.dma("sync", c3[0:3, :], c3_d[:, :], writes=[c3])
        P.dma("sync", bmr[0:1, :], bmod_d[:, :], writes=[bmr])
        for kc in range(KC):
            P.op("tensor", C("transpose", out=psb[0][:, kc * 4:kc * 4 + 3], in_=c3[0:3, kc * 128:(kc + 1) * 128],
                                                        identity=ident_f[0:3, 0:3]), reads=[c3, ident_f], writes=[psb[0]])
        P.op("scalar", C("activation", out=sT[:], in_=psb[0][:, 0:32].rearrange("p (k j) -> p k j", j=4)[:, :, 0:3], func=AF.Silu),
             reads=[psb[0]], writes=[sT])
        for n in range(12):
            w = wm[n % 2]
            P.dma("sync", w[:], wmod_d[:, n * 512:(n + 1) * 512].rearrange("(k p) n -> p k n", p=128), writes=[w])
            pb = psb[1 + n % 2]
            for kc in range(KC):
                mm(pb[0:3, :], sT[:, kc, :], w[:, kc, :], kc == 0, False, [sT, w], [pb])
            mm(pb[0:3, :], ones_f[0:1, 0:3], bmr[0:1, n * 512:(n + 1) * 512], False, True, [ones_f, bmr], [pb])
            evac(mrow[0:3, n * 512:(n + 1) * 512], pb[0:3, :], [pb], [mrow])
        P.dma("sync", modrows_d[:, :], mrow[0:3, :], reads=[mrow], writes=[modrows])
        dbg("mod", mrow[0:3, :], mrow)
        phase_end(c3, sT, bmr, mrow, wm[0], wm[1])
        if stop_after == 0:
            P.finish(out_ops)
            P.emit()
            return nc

        zT = A.alloc("zT", 128, [T], F32)
        fw1 = A.alloc("fw1", 128, [64], F32)
        fw2 = A.alloc("fw2", 128, [64], F32)
        fw3 = A.alloc("fw3", 128, [2048], F32)
        fb = A.alloc("fb", 128, [2], F32)
        h1T = A.alloc("h1T", 128, [T], F32)
        h2T_f = A.alloc("h2T_f", 128, [T], F32)
        su = A.alloc("su", 128, [512], F32)
        sk = A.alloc("sk", 128, [512], I32)
        skf = A.alloc("skf", 128, [512], F32)
        sfx = A.alloc("sfx", 128, [512], F32)
        P.dma("sync", zT[0:33, :], zT_d[:, :], writes=[zT])
        P.dma("sync", fw1[0:33, :], fw1_d[:, :], writes=[fw1])
        P.dma("sync", fw2[0:64, :], fw2_d[:, :], writes=[fw2])
        P.dma("sync", fw3[0:64, :], fw3_d[:, :], writes=[fw3])
        P.dma("sync", fb[0:64, 0:1], fb1_d[:, :], writes=[fb])
        P.dma("sync", fb[0:64, 1:2], fb2_d[:, :], reads=[], writes=[fb])
        P.op("vector", C("tensor_scalar", out=fb[0:64, :], in0=fb[0:64, :], scalar1=9.0 * PI, scalar2=None, op0=ALU.add), reads=[fb], writes=[fb])

        def sin_layer(wbuf, kdim, src, dst, bcol):
            for tch in range(4):
                pb = psb[tch % 2]
                mm(pb[0:64, :], wbuf[0:kdim, 0:64], src[0:kdim, tch * 512:(tch + 1) * 512], True, True, [wbuf, src], [pb])
                P.op("vector", C("tensor_scalar", out=su[0:64, :], in0=pb[0:64, :], scalar1=fb[0:64, bcol:bcol + 1], scalar2=None, op0=ALU.add),
                     reads=[pb, fb], writes=[su])
                P.op("vector", C("tensor_scalar", out=sk[0:64, :], in0=su[0:64, :], scalar1=1.0 / (2 * PI), scalar2=None, op0=ALU.mult), reads=[su], writes=[sk])
                P.op("vector", C("tensor_copy", out=skf[0:64, :], in_=sk[0:64, :]), reads=[sk], writes=[skf])
                P.op("vector", C("scalar_tensor_tensor", out=su[0:64, :], in0=skf[0:64, :], scalar=-2.0 * PI, in1=su[0:64, :], op0=ALU.mult, op1=ALU.add),
                     reads=[skf, su], writes=[su])
                P.op("vector", C("tensor_scalar", out=sfx[0:64, :], in0=su[0:64, :], scalar1=0.0, scalar2=2.0 * PI, op0=ALU.is_lt, op1=ALU.mult), reads=[su], writes=[sfx])
                P.op("vector", C("tensor_tensor", out=su[0:64, :], in0=su[0:64, :], in1=sfx[0:64, :], op=ALU.add), reads=[su, sfx], writes=[su])
                P.op("vector", C("tensor_scalar", out=su[0:64, :], in0=su[0:64, :], scalar1=-PI, scalar2=PI, op0=ALU.add, op1=ALU.min), reads=[su], writes=[su])
                P.op("vector", C("tensor_scalar", out=su[0:64, :], in0=su[0:64, :], scalar1=-PI, scalar2=None, op0=ALU.max), reads=[su], writes=[su])
                P.op("scalar", C("activation", out=dst[0:64, tch * 512:(tch + 1) * 512], in_=su[0:64, :], func=AF.Sin), reads=[su], writes=[dst])

        sin_layer(fw1, 33, zT, h1T, 0)
        sin_layer(fw2, 64, h1T, h2T_f, 1)
        dbg("h2f", h2T_f[0:64, :], h2T_f)
        Pall = A.alloc("Pall", 128, [2, NT, 512], BF16)
        Mall = A.alloc("Mall", 128, [2, NT, 512], BF16)
        dec = [A.alloc("dec%d" % i, 128, [2, 512], F32) for i in range(2)]
        ta_l = [A.alloc("ta%d" % i, 128, [512], F32) for i in range(2)]
        tb_l = [A.alloc("tb%d" % i, 128, [512], F32) for i in range(2)]
        for tt in range(NT):
            dc = dec[tt % 2]
            P.dma("sync", dc[:, 0, :], decay_d[tt * 128:(tt + 1) * 128, :], writes=[dc])
            P.dma("sync", dc[:, 1, :], decayb_d[tt * 128:(tt + 1) * 128, :], writes=[dc])
            for o in range(2):
                pf = psb[2 + (o * 2) % 4]
                pbk = psb[3 + (o * 2) % 4]
                ta, tb = ta_l[o], tb_l[o]
                mm(pf[:, :], h2T_f[0:64, tt * 128:(tt + 1) * 128], fw3[0:64, (o * 2) * 512:(o * 2 + 1) * 512], True, True, [h2T_f, fw3], [pf])
                mm(pbk[:, :], h2T_f[0:64, tt * 128:(tt + 1) * 128], fw3[0:64, (o * 2 + 1) * 512:(o * 2 + 2) * 512], True, True, [h2T_f, fw3], [pbk])
                P.op("vector", C("tensor_tensor", out=ta[:], in0=pf[:, :], in1=dc[:, 0, :], op=ALU.mult), reads=[pf, dc], writes=[ta])
                P.op("vector", C("tensor_tensor", out=tb[:], in0=pbk[:, :], in1=dc[:, 1, :], op=ALU.mult), reads=[pbk, dc], writes=[tb])
                P.op("gpsimd", C("tensor_tensor", out=Pall[:, o, tt, :], in0=ta[:], in1=tb[:], op=ALU.add), reads=[ta, tb], writes=[Pall])
                P.op("gpsimd", C("tensor_tensor", out=Mall[:, o, tt, :], in0=ta[:], in1=tb[:], op=ALU.subtract), reads=[ta, tb], writes=[Mall])
        ftab = [[A.alloc("ftab%d%d" % (i, j), 128, [NT, 128], BF16) for j in range(2)] for i in range(2)]
        hout = [[A.alloc("hout%d%d" % (i, j), 128, [512], BF16) for j in range(2)] for i in range(2)]
        it = 0
        for fch in range(16):
            ct, stb = ftab[fch % 2]
            P.dma("sync", ct[:], cft_d[fch].rearrange("p (a b) -> p a b", a=NT), writes=[ct])
            P.dma("sync", stb[:], sft_d[fch].rearrange("p (a b) -> p a b", a=NT), writes=[stb])
            for o in range(2):
                pr = psb[(it * 2) % 8]
                pi_ = psb[(it * 2 + 1) % 8]
                hr_t, hs_t = hout[it % 2]
                it += 1
                for tc in range(NT):
                    mm(pr[:, :], ct[:, tc, :], Pall[:, o, tc, :], tc == 0, tc == NT - 1, [ct, Pall], [pr])
                for tc in range(NT):
                    mm(pi_[:, :], stb[:, tc, :], Mall[:, o, tc, :], tc == 0, tc == NT - 1, [stb, Mall], [pi_])
                evac(hr_t[:], pr[:, :], [pr], [hr_t])
                evac(hs_t[:], pi_[:, :], [pi_], [hs_t])
                P.dma("sync", hr_d[o, fch * 128:(fch + 1) * 128, :], hr_t[:], reads=[hr_t])
                P.dma("sync", hs_d[o, fch * 128:(fch + 1) * 128, :], hs_t[:], reads=[hs_t])
        phase_end(zT, fw1, fw2, fw3, fb, h1T, h2T_f, su, sk, skf, sfx, Pall, Mall, dec[0], dec[1], *ta_l, *tb_l,
                  ftab[0][0], ftab[0][1], ftab[1][0], ftab[1][1], hout[0][0], hout[0][1], hout[1][0], hout[1][1])
        if stop_after == 1:
            P.finish(out_ops)
            P.emit()
            return nc

        masks = A.alloc("masks", 128, [2, 128], BF16)
        esink = A.alloc("esink", 128, [8], F32)
        convp = A.alloc("convp", 128, [4, 12], F32)
        skipp = A.alloc("skipp", 128, [2, 4], F32)
        P.dma("sync", masks[:], masks_d.rearrange("p (a b) -> p a b", a=2), writes=[masks])
        P.dma("sync", esink[0:64, :], sink_d[0, :].partition_broadcast(64), writes=[esink])
        P.op("scalar", C("activation", out=esink[0:64, :], in_=esink[0:64, :], func=AF.Exp), reads=[esink], writes=[esink])
        P.dma("sync", convp[:], convp_d.rearrange("p (a b) -> p a b", a=4), writes=[convp])
        P.dma("sync", skipp[:], skip_d.rearrange("p (a b) -> p a b", a=2), writes=[skipp])
        kcT = [A.alloc("kcT%d" % b, 128, [256], BF16) for b in range(2)]
        vc = [A.alloc("vc%d" % b, 128, [2, 128], BF16) for b in range(2)]

        chk_n = [0]

        def chk(tag):
            if "chk" not in dbg_d:
                return
            i = chk_n[0]
            chk_n[0] += 1
            tb_ = A.alloc("chk%d" % i, 128, [256], F32)
            P.op("vector", C("tensor_copy", out=tb_[:], in_=kcT[0][:]), reads=[kcT[0]], writes=[tb_])
            out_ops.append(P.dma("sync", dbg_d["chk"][i], tb_[:], reads=[tb_]))
            print("chk", i, tag)

        def wchunk_load(buf, col0, ncols=128):
            P.dma("gpsimd", buf[:], win_d[:, col0:col0 + ncols].rearrange("(k p) n -> p k n", p=128), writes=[buf])

        rows = [A.alloc("row%d" % i, 128, [D], F32) for i in range(2)]
        xin = [A.alloc("xin%d" % i, 128, [D], F32) for i in range(2)]
        xn = A.alloc("xn", 128, [D], F32)
        xb = A.alloc("xb", 128, [D], BF16)
        hcT = A.alloc("hcT", 128, [KC, 256], BF16)
        wk = A.alloc("wk", 128, [KC, 128], BF16)
        wv = A.alloc("wv", 128, [KC, 128], BF16)
        wchunk_load(wk, 1024)
        wchunk_load(wv, 1280)
        load_row(rows[0], modrows_d[2, D:2 * D], reads=[modrows], plus1=True)
        load_row(rows[1], modrows_d[2, 0:D], reads=[modrows])
        for b in range(2):
            for tl in range(2):
                xi = xin[tl % 2]
                P.dma("sync", xi[:], ctx_d[b, tl * 128:(tl + 1) * 128, :], writes=[xi])
                layer_norm(xi, xn)
                modulate(xn, rows[0], rows[1])
                P.op("vector", C("tensor_copy", out=xb[:], in_=xn[:]), reads=[xn], writes=[xb])
                transpose_to(hcT[:, :, tl * 128:(tl + 1) * 128], xb, psb[tl % 2], [], [hcT])
            pk = psb[2 + b]
            for kc in range(KC):
                mm(pk[:, 0:256], wk[:, kc, :], hcT[:, kc, :], kc == 0, kc == KC - 1, [wk, hcT], [pk])
            evac(kcT[b][:], pk[:, 0:256], [pk], [kcT[b]])
            for tl in range(2):
                pvv = psb[4 + tl]
                for kc in range(KC):
                    mm(pvv[:, 0:128], hcT[:, kc, tl * 128:(tl + 1) * 128], wv[:, kc, :], kc == 0, kc == KC - 1, [hcT, wv], [pvv])
                evac(vc[b][:, tl, :], pvv[:, 0:128], [pvv], [vc[b]])
        if "a_xn" in dbg_d:
            dbg("a_xn", xn[:], xn)
            dbg("a_row0", rows[0][:], rows[0])
            tmpb = A.alloc("dbgA", 128, [2048], F32)
            P.op("vector", C("tensor_copy", out=tmpb[:], in_=hcT[:].rearrange("p a b -> p (a b)")), reads=[hcT], writes=[tmpb])
            dbg("a_hcT", tmpb[:], tmpb)
            tmpb2 = A.alloc("dbgA2", 128, [1024], F32)
            P.op("vector", C("tensor_copy", out=tmpb2[:], in_=wk[:].rearrange("p a b -> p (a b)")), reads=[wk], writes=[tmpb2])
            dbg("a_wk", tmpb2[:], tmpb2)
            tmpb3 = A.alloc("dbgA3", 128, [512], F32)
            P.op("vector", C("tensor_copy", out=tmpb3[:, 0:256], in_=kcT[0][:]), reads=[kcT[0]], writes=[tmpb3])
            P.op("vector", C("tensor_copy", out=tmpb3[:, 256:512], in_=kcT[1][:]), reads=[kcT[1]], writes=[tmpb3])
            dbg("a_kcT", tmpb3[:], tmpb3)
            tmpb4 = A.alloc("dbgA4", 128, [512], F32)
            P.op("vector", C("tensor_copy", out=tmpb4[:], in_=psb[2][:, :]), reads=[psb[2]], writes=[tmpb4])
            dbg("a_pk", tmpb4[:], tmpb4)
        chk("end of A before phase_end")
        phase_end(hcT, wk, xin[0], xin[1], xn, xb, rows[0], rows[1])
        chk("after A phase_end")
        if stop_after == 2:
            P.finish(out_ops)
            P.emit()
            return nc

        gates = A.alloc("gates", 128, [NTT, NE], F32)
        maskall = A.alloc("maskall", 128, [NTT, NE], F32)
        for b in range(2):
            hxT = A.alloc("hxT", 128, [KC, T], BF16)
            rows = [A.alloc("row%d" % i, 128, [D], F32) for i in range(5)]
            xin = [A.alloc("xin%d" % i, 128, [D], F32) for i in range(4)]
            xn_l = [A.alloc("xn%d" % i, 128, [D], F32) for i in range(2)]
            xb_l = [A.alloc("xb%d" % i, 128, [D], BF16) for i in range(2)]
            if b == 0:
                chk("B after allocs")
            load_row(rows[0], modrows_d[b, D:2 * D], reads=[modrows], plus1=True)
            load_row(rows[1], modrows_d[b, 0:D], reads=[modrows])
            if b == 0:
                chk("B after load_rows")
            def tileB(tl):
                xi = xin[tl % 4]
                P.dma("sync", xi[:], x_d[b, tl * 128:(tl + 1) * 128, :], writes=[xi])
                xn, xb = xn_l[tl % 2], xb_l[tl % 2]
                yield from layer_norm_g(xi, xn)
                yield from modulate_g(xn, rows[0], rows[1])
                P.op("vector", C("tensor_copy", out=xb[:], in_=xn[:]), reads=[xn], writes=[xb])
                yield
                transpose_to(hxT[:, :, tl * 128:(tl + 1) * 128], xb, psb[tl % 2], [], [hxT])

            for tl in range(0, NT, 2):
                run_interleaved([tileB(tl), tileB(tl + 1)])
            if b == 0:
                chk("after B loop")
            if stop_after == 2.5:
                P.finish(out_ops)
                P.emit()
                return nc
            phase_end(*xin, *xn_l, *xb_l, *rows)
            if b == 0:
                chk("after B phase_end")
            QT = A.alloc("QT", 128, [4, T], BF16)
            KT = A.alloc("KT", 128, [T], BF16)
            Vt = A.alloc("Vt", 128, [NT, 128], BF16)
            sTm = A.alloc("sTm", 128, [12, T], BF16)
            ropeC = A.alloc("ropeC", 128, [T], F32)
            ropeS = A.alloc("ropeS", 128, [T], F32)
            wq = [A.alloc("wq%d" % i, 128, [KC, 128], BF16) for i in range(4)]
            r1_l = [A.alloc("r1%d" % i, 128, [512], F32) for i in range(2)]
            r2_l = [A.alloc("r2%d" % i, 128, [512], F32) for i in range(2)]
            U = [A.alloc("U%d" % i, 128, [T + 2], F32) for i in range(2)]
            cs = A.alloc("cs", 128, [T], F32)
            P.dma("sync", ropeC[:], ropeC_d[:, :], writes=[ropeC])
            P.dma("sync", ropeS[:], ropeS_d[:, :], writes=[ropeS])
            for i in range(2):
                P.op("gpsimd", C("memset", U[i][:, 0:1], 0.0), writes=[U[i]])
                P.op("gpsimd", C("memset", U[i][:, T + 1:T + 2], 0.0), writes=[U[i]])
            pc = 0
            for c in range(5):
                wa, wb_ = wq[(2 * c) % 4], wq[(2 * c + 1) % 4]
                if c < 4:
                    wchunk_load(wa, c * 128)
                    wchunk_load(wb_, 512 + c * 128)
                else:
                    wchunk_load(wa, 1024)
                    wchunk_load(wb_, 1152)
                for tch in range(4):
                    pa = psb[pc % 8]
                    pb = psb[(pc + 1) % 8]
                    pc += 2
                    r1, r2 = r1_l[(pc // 2) % 2], r2_l[(pc // 2) % 2]
                    ts_ = slice(tch * 512, (tch + 1) * 512)
                    for kc in range(KC):
                        mm(pa[:, :], wa[:, kc, :], hxT[:, kc, ts_], kc == 0, kc == KC - 1, [wa, hxT], [pa])
                    for kc in range(KC):
                        mm(pb[:, :], wb_[:, kc, :], hxT[:, kc, ts_], kc == 0, kc == KC - 1, [wb_, hxT], [pb])
                    P.op("vector", C("tensor_tensor", out=r1[:], in0=pa[:, :], in1=ropeC[:, ts_], op=ALU.mult), reads=[pa, ropeC], writes=[r1])
                    P.op("vector", C("tensor_tensor", out=r2[:], in0=pb[:, :], in1=ropeS[:, ts_], op=ALU.mult), reads=[pb, ropeS], writes=[r2])
                    if c < 4:
                        P.op("gpsimd", C("tensor_tensor", out=QT[:, c, ts_], in0=r1[:], in1=r2[:], op=ALU.add), reads=[r1, r2], writes=[QT])
                    else:
                        P.op("gpsimd", C("tensor_tensor", out=KT[:, ts_], in0=r1[:], in1=r2[:], op=ALU.add), reads=[r1, r2], writes=[KT])
            if b == 0:
                chk("after qk")
            for tl in range(NT):
                pvv = psb[pc % 8]
                pc += 1
                for kc in range(KC):
                    mm(pvv[:, 0:128], hxT[:, kc, tl * 128:(tl + 1) * 128], wv[:, kc, :], kc == 0, kc == KC - 1, [hxT, wv], [pvv])
                evac(Vt[:, tl, :], pvv[:, 0:128], [pvv], [Vt])
            if b == 0:
                chk("after V")
            for j in range(12):
                wj = wq[j % 4]
                wchunk_load(wj, HY_BASE + j * 128)
                Uj = U[j % 2]
                for tch in range(4):
                    pu = psb[pc % 8]
                    pc += 1
                    for kc in range(KC):
                        mm(pu[:, :], wj[:, kc, :], hxT[:, kc, tch * 512:(tch + 1) * 512], kc == 0, kc == KC - 1, [wj, hxT], [pu])
                    evac(Uj[:, 1 + tch * 512:1 + (tch + 1) * 512], pu[:, :], [pu], [Uj])
                P.op("vector", C("tensor_scalar", out=cs[:], in0=Uj[:, 1:T + 1], scalar1=convp[:, 1, j:j + 1], scalar2=convp[:, 3, j:j + 1],
                                                                  op0=ALU.mult, op1=ALU.add), reads=[Uj, convp], writes=[cs])
                P.op("vector", C("scalar_tensor_tensor", out=cs[:], in0=Uj[:, 0:T], scalar=convp[:, 0, j:j + 1], in1=cs[:],
                                                                         op0=ALU.mult, op1=ALU.add), reads=[Uj, convp, cs], writes=[cs])
                P.op("vector", C("scalar_tensor_tensor", out=sTm[:, j, :], in0=Uj[:, 2:T + 2], scalar=convp[:, 2, j:j + 1], in1=cs[:],
                                                                         op0=ALU.mult, op1=ALU.add), reads=[Uj, convp, cs], writes=[sTm])
            if b == 0:
                chk("after hy")
            if "qT" in dbg_d and b == 0:
                for c in range(4):
                    tmpb = cs
                    P.op("vector", C("tensor_copy", out=tmpb[:], in_=QT[:, c, :]), reads=[QT], writes=[tmpb])
                    dbg("qT", tmpb[:], tmpb, dst=dbg_d["qT"][c])
            if "sT" in dbg_d and b == 0:
                for c in range(12):
                    tmpb = cs
                    P.op("vector", C("tensor_copy", out=tmpb[:], in_=sTm[:, c, :]), reads=[sTm], writes=[tmpb])
                    dbg("sT", tmpb[:], tmpb, dst=dbg_d["sT"][c])
            if "kchk" in dbg_d and b == 0:
                P.op("vector", C("tensor_copy", out=cs[:, 0:256], in_=kcT[0][:]), reads=[kcT[0]], writes=[cs])
                P.op("vector", C("tensor_copy", out=cs[:, 256:512], in_=vc[0][:].rearrange("p a b -> p (a b)")), reads=[vc[0]], writes=[cs])
                dbg("kchk", cs[:, 0:512], cs)
            P.dma("sync", hxs_d[:, :], hxT[:].rearrange("p k t -> p (k t)"), reads=[hxT])
            phase_end(ropeC, ropeS, wq[0], wq[1], wq[2], wq[3], *r1_l, *r2_l, U[0], U[1], cs, hxT)
            if stop_after == 3:
                P.finish(out_ops)
                P.emit()
                return nc

            attT = A.alloc("attT", 128, [8, T], BF16)
            PT = [A.alloc("PT%d" % i, 128, [4, 128], BF16) for i in range(3)]
            dn = A.alloc("dn", 128, [4, 128], F32)
            pidx = 0
            for qi in range(NT):
                for g in range(2):
                    gp = slice(g * 64, (g + 1) * 64)
                    keys = []
                    for j in (qi - 1, qi, qi + 1):
                        if 0 <= j < NT:
                            keys.append(("l", j))
                    keys += [("c", 0), ("c", 1)]
                    po = psb[4 + (qi * 2 + g) % 2]
                    pd = psb[6 + (qi * 2 + g) % 2]
                    for ki, (kind, j) in enumerate(keys):
                        pst = psb[pidx % 4]
                        ptb = PT[pidx % 3]
                        pidx += 1
                        if kind == "l":
                            lhs = KT[gp, j * 128:(j + 1) * 128]
                            lr = KT
                            vv = Vt[:, j, g * 64:(g + 1) * 64]
                            vr = Vt
                        else:
                            lhs = kcT[b][gp, j * 128:(j + 1) * 128]
                            lr = kcT[b]
                            vv = vc[b][:, j, g * 64:(g + 1) * 64]
                            vr = vc[b]
                        mm(pst[:, :], lhs, QT[gp, :, qi * 128:(qi + 1) * 128], True, True, [lr, QT], [pst])
                        P.op("scalar", C("activation", out=ptb[:], in_=pst[:, :].rearrange("p (h q) -> p h q", h=4), func=AF.Exp, scale=0.125),
                             reads=[pst], writes=[ptb])
                        if kind == "l" and j != qi:
                            mi = 0 if j < qi else 1
                            P.op("gpsimd", C("tensor_tensor", out=ptb[:], in0=ptb[:], in1=masks[:, mi:mi + 1, :].to_broadcast([128, 4, 128]), op=ALU.mult),
                                 reads=[ptb, masks], writes=[ptb])
                        mm(po[0:64, :], vv, ptb[:].rearrange("p h q -> p (h q)"), ki == 0, ki == len(keys) - 1, [vr, ptb], [po])
                        mm(pd[0:64, :], ones_b[:, 0:64], ptb[:].rearrange("p h q -> p (h q)"), ki == 0, ki == len(keys) - 1, [ones_b, ptb], [pd])
                    P.op("vector", C("tensor_tensor", out=dn[0:64], in0=pd[0:64, :].rearrange("p (h q) -> p h q", h=4),
                                                                          in1=esink[0:64, g * 4:(g + 1) * 4].unsqueeze(2).to_broadcast([64, 4, 128]), op=ALU.add),
                         reads=[pd, esink], writes=[dn])
                    P.op("vector", C("reciprocal", out=dn[0:64], in_=dn[0:64]), reads=[dn], writes=[dn])
                    P.op("vector", C("tensor_tensor", out=attT[0:64, g * 4:(g + 1) * 4, qi * 128:(qi + 1) * 128],
                                                                                 in0=po[0:64, :].rearrange("p (h q) -> p h q", h=4), in1=dn[0:64], op=ALU.mult),
                         reads=[po, dn], writes=[attT])
            if "attT" in dbg_d and b == 0:
                tmpb = A.alloc("dbga", 128, [T], F32)
                dbg("esink", esink[0:64, :], esink)
                P.op("vector", C("tensor_copy", out=tmpb[:, 0:256], in_=kcT[0][:]), reads=[kcT[0]], writes=[tmpb])
                dbg("kcT", tmpb[:, 0:256], tmpb)
                P.op("vector", C("tensor_copy", out=tmpb[:, 0:256], in_=vc[0][:].rearrange("p a b -> p (a b)")), reads=[vc[0]], writes=[tmpb])
                dbg("vc", tmpb[:, 0:256], tmpb)
                P.op("vector", C("tensor_copy", out=tmpb[:, 0:512], in_=PT[0][:].rearrange("p a b -> p (a b)")), reads=[PT[0]], writes=[tmpb])
                dbg("pt", tmpb[:, 0:512], tmpb)
                dbg("dn", dn[0:64].rearrange("p a b -> p (a b)"), dn)
                for c in range(8):
                    P.op("vector", C("tensor_copy", out=tmpb[0:64, :], in_=attT[0:64, c, :]), reads=[attT], writes=[tmpb])
                    dbg("attT", tmpb[0:64, :], tmpb, dst=dbg_d["attT"][c])
            P.dma("sync", atts_d[:, :], attT[0:64].rearrange("p k t -> p (k t)"), reads=[attT])
            phase_end(QT, KT, Vt, PT[0], PT[1], PT[2], dn, attT)
            if stop_after == 4:
                P.finish(out_ops)
                P.emit()
                return nc

            ztok = A.alloc("ztok", 128, [NT, 512], BF16)
            z1T = A.alloc("z1T", 128, [4, T], BF16)
            Yr = A.alloc("Yr", 128, [NT, 512], BF16)
            Ys = A.alloc("Ys", 128, [NT, 512], BF16)
            ftab = [[A.alloc("ftab%d%d" % (i, j), 128, [NT, 128], BF16) for j in range(2)] for i in range(2)]
            hin = [[A.alloc("hin%d%d" % (i, j), 128, [512], BF16) for j in range(2)] for i in range(2)]
            itab = [ztok, A.alloc("itab1", 128, [NT, 512], BF16)]
            hyT = A.alloc("hyT", 128, [4, T], BF16)
            e1_l = [A.alloc("e1%d" % i, 128, [512], F32) for i in range(2)]
            e2_l = [A.alloc("e2%d" % i, 128, [512], F32) for i in range(2)]
            e3_l = [A.alloc("e3%d" % i, 128, [512], F32) for i in range(2)]
            e4_l = [A.alloc("e4%d" % i, 128, [512], F32) for i in range(2)]
            for o in range(2):
                for tt in range(NT):
                    pz = psb[tt % 2]
                    pzv = pz.ap.bitcast(BF16)
                    for cc in range(4):
                        if o == 0:
                            src_ap, src_b = sTm[:, 8 + cc, tt * 128:(tt + 1) * 128], sTm
                        else:
                            src_ap, src_b = z1T[:, cc, tt * 128:(tt + 1) * 128], z1T
                        P.op("tensor", C("transpose", out=pzv[:, cc * 128:(cc + 1) * 128], in_=src_ap, identity=ident_b[:]),
                             reads=[src_b, ident_b], writes=[pz])
                    evac(ztok[:, tt, :], pzv[:, 0:512], [pz], [ztok])
                for fch in range(16):
                    ct, stb = ftab[fch % 2]
                    hr_t, hs_t = hin[fch % 2]
                    e1, e2, e3, e4 = e1_l[fch % 2], e2_l[fch % 2], e3_l[fch % 2], e4_l[fch % 2]
                    P.dma("sync", ct[:], cft_d[fch].rearrange("p (a b) -> p a b", a=NT), writes=[ct])
                    P.dma("sync", stb[:], sft_d[fch].rearrange("p (a b) -> p a b", a=NT), writes=[stb])
                    P.dma("sync", hr_t[:], hr_d[o, fch * 128:(fch + 1) * 128, :], reads=[hspec], writes=[hr_t])
                    P.dma("sync", hs_t[:], hs_d[o, fch * 128:(fch + 1) * 128, :], reads=[hspec], writes=[hs_t])
                    pr = psb[2 + (fch % 2) * 2]
                    pi_ = psb[3 + (fch % 2) * 2]
                    for tc in range(NT):
                        mm(pr[:, :], ct[:, tc, :], ztok[:, tc, :], tc == 0, tc == NT - 1, [ct, ztok], [pr])
                    for tc in range(NT):
                        mm(pi_[:, :], stb[:, tc, :], ztok[:, tc, :], tc == 0, tc == NT - 1, [stb, ztok], [pi_])
                    P.op("vector", C("tensor_tensor", out=e1[:], in0=pr[:, :], in1=hr_t[:], op=ALU.mult), reads=[pr, hr_t], writes=[e1])
                    P.op("vector", C("tensor_tensor", out=e2[:], in0=pi_[:, :], in1=hs_t[:], op=ALU.mult), reads=[pi_, hs_t], writes=[e2])
                    P.op("gpsimd", C("tensor_tensor", out=Yr[:, fch, :], in0=e1[:], in1=e2[:], op=ALU.subtract), reads=[e1, e2], writes=[Yr])
                    P.op("vector", C("tensor_tensor", out=e3[:], in0=pr[:, :], in1=hs_t[:], op=ALU.mult), reads=[pr, hs_t], writes=[e3])
                    P.op("vector", C("tensor_tensor", out=e4[:], in0=pi_[:, :], in1=hr_t[:], op=ALU.mult), reads=[pi_, hr_t], writes=[e4])
                    P.op("gpsimd", C("tensor_tensor", out=Ys[:, fch, :], in0=e3[:], in1=e4[:], op=ALU.add), reads=[e3, e4], writes=[Ys])
                for tch in range(4):
                    ci_, si_ = itab
                    P.dma("sync", ci_[:], cit_d[tch].rearrange("p (a b) -> p a b", a=NT), writes=[ci_])
                    P.dma("gpsimd", si_[:], sit_d[tch].rearrange("p (a b) -> p a b", a=NT), writes=[si_])
                    ts_ = slice(tch * 512, (tch + 1) * 512)
                    for cc in range(4):
                        pcv = psb[6 + cc % 2]
                        e1 = e1_l[cc % 2]
                        for fc in range(NT):
                            mm(pcv[:, :], Yr[:, fc, cc * 128:(cc + 1) * 128], ci_[:, fc, :], fc == 0, False, [Yr, ci_], [pcv])
                        for fc in range(NT):
                            mm(pcv[:, :], Ys[:, fc, cc * 128:(cc + 1) * 128], si_[:, fc, :], False, fc == NT - 1, [Ys, si_], [pcv])
                        if o == 0:
                            zin_ap, zin_b = sTm[:, 8 + cc, ts_], sTm
                            xo_ap = sTm[:, 0 + cc, ts_]
                            zo_ap, zo_b = z1T[:, cc, ts_], z1T
                        else:
                            zin_ap, zin_b = z1T[:, cc, ts_], z1T
                            xo_ap = sTm[:, 4 + cc, ts_]
                            zo_ap, zo_b = hyT[:, cc, ts_], hyT
                        P.op("vector", C("scalar_tensor_tensor", out=e1[:], in0=zin_ap, scalar=skipp[:, o, cc:cc + 1], in1=pcv[:, :],
                                                                                                          op0=ALU.mult, op1=ALU.add), reads=[pcv, zin_b, skipp], writes=[e1])
                        P.op("gpsimd", C("tensor_tensor", out=zo_ap, in0=e1[:], in1=xo_ap, op=ALU.mult), reads=[e1, sTm], writes=[zo_b])
            if "hyT" in dbg_d and b == 0:
                tmpb = A.alloc("dbgh", 128, [T], F32)
                for c in range(4):
                    P.op("vector", C("tensor_copy", out=tmpb[:], in_=hyT[:, c, :]), reads=[hyT], writes=[tmpb])
                    dbg("hyT", tmpb[:], tmpb, dst=dbg_d["hyT"][c])
            phase_end(ztok, z1T, Yr, Ys, ftab[0][0], ftab[0][1], ftab[1][0], ftab[1][1], hin[0][0], hin[0][1], hin[1][0], hin[1][1],
                      itab[1], *e1_l, *e2_l, *e3_l, *e4_l, sTm)
            if stop_after == 5:
                P.finish(out_ops)
                P.emit()
                return nc

            hxT = A.alloc("hxT", 128, [KC, T], BF16)
            attT = A.alloc("attT", 128, [8, T], BF16)
            P.dma("sync", hxT[:].rearrange("p k t -> p (k t)"), hxs_d[:, :], writes=[hxT])
            P.dma("sync", attT[0:64].rearrange("p k t -> p (k t)"), atts_d[:, :], writes=[attT])
            wba = A.alloc("wba", 128, [8, D], BF16)
            wbh = A.alloc("wbh", 128, [4, D], BF16)
            mT = A.alloc("mT", 128, [KC, T], BF16)
            wg = [A.alloc("wg%d" % i, 128, [KC, 128], BF16) for i in range(4)]
            ga_l = [A.alloc("ga%d" % i, 128, [512], F32) for i in range(2)]
            gh_l = [A.alloc("gh%d" % i, 128, [512], F32) for i in range(2)]
            m1_l = [A.alloc("m1%d" % i, 128, [512], F32) for i in range(2)]
            m2_l = [A.alloc("m2%d" % i, 128, [512], F32) for i in range(2)]
            P.dma("gpsimd", wba[0:64, :, :], wba_d.rearrange("(h d) n -> d h n", d=64), writes=[wba])
            P.dma("gpsimd", wbh[:], wbh_d.rearrange("(c p) n -> p c n", p=128), writes=[wbh])
            pc = 0
            for nch in range(8):
                wga, wgh = wg[(2 * nch) % 4], wg[(2 * nch + 1) % 4]
                wchunk_load(wga, GATE_BASE + nch * 128)
                wchunk_load(wgh, GATE_BASE + 1024 + nch * 128)
                ns = slice(nch * 128, (nch + 1) * 128)
                for tch in range(4):
                    ts_ = slice(tch * 512, (tch + 1) * 512)
                    p1, p2, p3, p4 = [psb[(pc + i) % 8] for i in range(4)]
                    pc += 4
                    ga, gh, m1, m2 = ga_l[(pc // 4) % 2], gh_l[(pc // 4) % 2], m1_l[(pc // 4) % 2], m2_l[(pc // 4) % 2]
                    for h in range(8):
                        mm(p1[:, :], wba[0:64, h, ns], attT[0:64, h, ts_], h == 0, h == 7, [wba, attT], [p1])
                    for cc in range(4):
                        mm(p2[:, :], wbh[:, cc, ns], hyT[:, cc, ts_], cc == 0, cc == 3, [wbh, hyT], [p2])
                    for kc in range(KC):
                        mm(p3[:, :], wga[:, kc, :], hxT[:, kc, ts_], kc == 0, kc == KC - 1, [wga, hxT], [p3])
                    for kc in range(KC):
                        mm(p4[:, :], wgh[:, kc, :], hxT[:, kc, ts_], kc == 0, kc == KC - 1, [wgh, hxT], [p4])
                    P.op("scalar", C("activation", out=ga[:], in_=p3[:, :], func=AF.Sigmoid), reads=[p3], writes=[ga])
                    P.op("scalar", C("activation", out=gh[:], in_=p4[:, :], func=AF.Sigmoid), reads=[p4], writes=[gh])
                    P.op("vector", C("tensor_tensor", out=m1[:], in0=p1[:, :], in1=ga[:], op=ALU.mult), reads=[p1, ga], writes=[m1])
                    P.op("vector", C("tensor_tensor", out=m2[:], in0=p2[:, :], in1=gh[:], op=ALU.mult), reads=[p2, gh], writes=[m2])
                    P.op("gpsimd", C("tensor_tensor", out=mT[:, nch, ts_], in0=m1[:], in1=m2[:], op=ALU.add), reads=[m1, m2], writes=[mT])
            phase_end(hxT, attT, hyT, wba, wbh, wg[0], wg[1], wg[2], wg[3], *ga_l, *gh_l, *m1_l, *m2_l)
            h2Tt = [A.alloc("h2Tt%d" % i, 128, [KC, 128], BF16) for i in range(2)]
            wo = A.alloc("wo", 128, [KC, D], BF16)
            P.dma("gpsimd", wo[:], wo_d.rearrange("(c p) n -> p c n", p=128), writes=[wo])
            rows = [A.alloc("row%d" % i, 128, [D], F32) for i in range(5)]
            xin = [A.alloc("xin%d" % i, 128, [D], F32) for i in range(4)]
            xn_l = [A.alloc("xn%d" % i, 128, [D], F32) for i in range(2)]
            xb_l = [A.alloc("xb%d" % i, 128, [D], BF16) for i in range(2)]
            xm_l = [A.alloc("xm%d" % i, 128, [D], F32) for i in range(2)]
            lg_l = [A.alloc("lg%d" % i, 128, [NE], F32) for i in range(2)]
            m8_l = [A.alloc("m8%d" % i, 128, [8], F32) for i in range(2)]
            msk_l = [A.alloc("msk%d" % i, 128, [NE], F32) for i in range(2)]
            ssum_l = [A.alloc("ssum%d" % i, 128, [1], F32) for i in range(2)]
            rw = A.alloc("rw", 128, [KC, NE], BF16)
            rbrow = A.alloc("rbrow", 128, [NE], F32)
            P.dma("gpsimd", rw[:], rw_d.rearrange("(c p) n -> p c n", p=128), writes=[rw])
            P.dma("sync", rbrow[:], rb_d[0, :].partition_broadcast(128), writes=[rbrow])
            load_row(rows[0], modrows_d[b, 2 * D:3 * D], reads=[modrows])
            load_row(rows[1], ln_d[0, :])
            load_row(rows[2], ln_d[1, :])
            load_row(rows[3], modrows_d[b, 4 * D:5 * D], reads=[modrows], plus1=True)
            load_row(rows[4], modrows_d[b, 3 * D:4 * D], reads=[modrows])
            def tileF2(tl):
                tsl = slice(tl * 128, (tl + 1) * 128)
                xn, xb, xm = xn_l[tl % 2], xb_l[tl % 2], xm_l[tl % 2]
                lg, m8, msk, ssum = lg_l[tl % 2], m8_l[tl % 2], msk_l[tl % 2], ssum_l[tl % 2]
                py = [psb[(tl % 2) * 2], psb[(tl % 2) * 2 + 1]]
                for n2 in range(2):
                    for kc in range(KC):
                        mm(py[n2][:, :], mT[:, kc, tsl], wo[:, kc, n2 * 512:(n2 + 1) * 512], kc == 0, kc == KC - 1, [mT, wo], [py[n2]])
                xi = xin[tl % 4]
                P.dma("sync", xi[:], x_d[b, tsl, :], writes=[xi])
                for n2 in range(2):
                    P.op("vector", C("tensor_tensor", out=xn[:, n2 * 512:(n2 + 1) * 512], in0=py[n2][:, :], in1=rows[0][:, n2 * 512:(n2 + 1) * 512], op=ALU.mult),
                         reads=[py[n2], rows[0]], writes=[xn])
                P.op("vector", C("scalar_tensor_tensor", out=xn[:], in0=xi[:], scalar=ALPHA, in1=xn[:], op0=ALU.mult, op1=ALU.add), reads=[xi, xn], writes=[xn])
                yield
                yield from layer_norm_g(xn, xm)
                yield from modulate_g(xm, rows[1], rows[2])
                P.dma("sync", xmid_d[b * T + tl * 128:b * T + (tl + 1) * 128, :], xm[:], reads=[xm])
                yield from layer_norm_g(xm, xn)
                yield from modulate_g(xn, rows[3], rows[4])
                P.op("vector", C("tensor_copy", out=xb[:], in_=xn[:]), reads=[xn], writes=[xb])
                yield
                P.dma("sync", h2d_d[b * T + tl * 128:b * T + (tl + 1) * 128, :], xb[:], reads=[xb])
                h2t = h2Tt[tl % 2]
                transpose_to(h2t[:], xb, psb[4 + tl % 2], [], [h2t])
                pl = psb[6 + tl % 2]
                for kc in range(KC):
                    mm(pl[:, 0:NE], h2t[:, kc, :], rw[:, kc, :], kc == 0, kc == KC - 1, [h2t, rw], [pl])
                P.op("vector", C("tensor_tensor", out=lg[:], in0=pl[:, 0:NE], in1=rbrow[:], op=ALU.add), reads=[pl, rbrow], writes=[lg])
                yield
                P.op("vector", C("max", out=m8[:], in_=lg[:]), reads=[lg], writes=[m8])
                yield
                P.op("vector", C("tensor_scalar", out=msk[:], in0=lg[:], scalar1=m8[:, 3:4], scalar2=None, op0=ALU.is_ge), reads=[lg, m8], writes=[msk])
                yield
                P.op("gpsimd", C("tensor_copy", out=maskall[:, b * NT + tl, :], in_=msk[:]), reads=[msk], writes=[maskall])
                yield
                P.op("vector", C("tensor_scalar", out=lg[:], in0=lg[:], scalar1=m8[:, 0:1], scalar2=None, op0=ALU.subtract), reads=[lg, m8], writes=[lg])
                yield
                P.op("scalar", C("activation", out=lg[:], in_=lg[:], func=AF.Exp), reads=[lg], writes=[lg])
                yield
                P.op("vector", C("tensor_tensor", out=lg[:], in0=lg[:], in1=msk[:], op=ALU.mult), reads=[lg, msk], writes=[lg])
                yield
                P.op("vector", C("reduce_sum", out=ssum[:], in_=lg[:], axis=mybir.AxisListType.X), reads=[lg], writes=[ssum])
                yield
                P.op("vector", C("reciprocal", out=ssum[:], in_=ssum[:]), reads=[ssum], writes=[ssum])
                yield
                P.op("vector", C("tensor_scalar", out=gates[:, b * NT + tl, :], in0=lg[:], scalar1=ssum[:, 0:1], scalar2=None, op0=ALU.mult), reads=[lg, ssum], writes=[gates])
                yield

            for tl in range(0, NT, 2):
                run_interleaved([tileF2(tl), tileF2(tl + 1)])
            if "gates" in dbg_d and b == 0:
                dbg("gates", gates[:], gates)
            phase_end(mT, wo, rw, rbrow, *xm_l, *lg_l, *m8_l, *msk_l, *ssum_l, *xin, *xn_l, *xb_l, *rows, *h2Tt)
            if stop_after == 6:
                P.finish(out_ops)
                P.emit()
                return nc

        ltri = A.alloc("ltri", 128, [128], F32)
        kk = A.alloc("kk", 128, [NBLK, NE], F32)
        tokid = A.alloc("tokid", 128, [NTT], F32)
        j4 = A.alloc("j4", 128, [NTT, 4], F32)
        Srun = A.alloc("Srun", 128, [NE], F32)
        rankall = A.alloc("rankall", 128, [NTT, NE], F32)
        cnt = A.alloc("cnt", 128, [NE], F32)
        ci = A.alloc("ci", 128, [NE], I32)
        cf = A.alloc("cf", 128, [NE], F32)
        cfx = A.alloc("cfx", 128, [NE], F32)
        padded = A.alloc("padded", 128, [NE], F32)
        cs_a = A.alloc("cs_a", 128, [NE], F32)
        cs_b = A.alloc("cs_b", 128, [NE], F32)
        pstart = A.alloc("pstart", 128, [NE], F32)
        destm = A.alloc("destm", 128, [NTT, NE], F32)
        d8 = A.alloc("d8", 128, [NTT, 8], F32)
        didx = A.alloc("didx", 128, [NTT, 4], I32)
        eqt = A.alloc("eqt", 128, [NTT, NE], F32)
        pay = A.alloc("pay", 128, [NTT, 4, 4], F32)
        cmpk = A.alloc("cmpk", 128, [NBLK, NE], F32)
        bexp = A.alloc("bexp", 128, [NBLK], F32)
        bexp1024 = A.alloc("bexp1024", 128, [NBLK], F32)
        P.dma("sync", ltri[:], ltri_d[:, :], writes=[ltri])
        P.dma("sync", kk[:], kk512_d.rearrange("p (k e) -> p k e", k=NBLK), writes=[kk])
        P.dma("sync", tokid[:], tokid_d[:, :], writes=[tokid])
        P.dma("sync", j4[:], j4_d.rearrange("p (a b) -> p a b", a=NTT), writes=[j4])
        P.dma("sync", slot_d[:, :], slotinit_d[:, :], writes=[slottab])
        P.op("vector", C("memset", Srun[:], 0.0), writes=[Srun])
        for tl in range(NTT):
            pr_ = psb[tl % 2]
            mm(pr_[:, 0:NE], ones_sq[:], Srun[:], True, False, [ones_sq, Srun], [pr_])
            mm(pr_[:, 0:NE], ltri[:], maskall[:, tl, :], False, True, [ltri, maskall], [pr_])
            evac(rankall[:, tl, :], pr_[:, 0:NE], [pr_], [rankall], eng="scalar")
            P.op("vector", C("tensor_tensor", out=Srun[:], in0=Srun[:], in1=maskall[:, tl, :], op=ALU.add), reads=[Srun, maskall], writes=[Srun])
        pcn = psb[2]
        mm(pcn[:, 0:NE], ones_sq[:], Srun[:], True, True, [ones_sq, Srun], [pcn])
        P.op("vector", C("tensor_copy", out=cnt[:], in_=pcn[:, 0:NE]), reads=[pcn], writes=[cnt])
        P.op("vector", C("tensor_scalar", out=cf[:], in0=cnt[:], scalar1=float(BLK - 1), scalar2=1.0 / BLK, op0=ALU.add, op1=ALU.mult), reads=[cnt], writes=[cf])
        P.op("vector", C("tensor_copy", out=ci[:], in_=cf[:]), reads=[cf], writes=[ci])
        P.op("vector", C("tensor_copy", out=cfx[:], in_=ci[:]), reads=[ci], writes=[cfx])
        P.op("vector", C("tensor_tensor", out=cf[:], in0=cfx[:], in1=cf[:], op=ALU.is_gt), reads=[cfx, cf], writes=[cf])
        P.op("vector", C("tensor_tensor", out=cfx[:], in0=cfx[:], in1=cf[:], op=ALU.subtract), reads=[cfx, cf], writes=[cfx])
        P.op("vector", C("tensor_scalar", out=padded[:], in0=cfx[:], scalar1=float(BLK), scalar2=None, op0=ALU.mult), reads=[cfx], writes=[padded])
        src_b, dst_b = padded, cs_a
        for sft in (1, 2, 4, 8, 16):
            P.op("vector", C("tensor_copy", out=dst_b[:, 0:sft], in_=src_b[:, 0:sft]), reads=[src_b], writes=[dst_b])
            P.op("vector", C("tensor_tensor", out=dst_b[:, sft:NE], in0=src_b[:, sft:NE], in1=src_b[:, 0:NE - sft], op=ALU.add), reads=[src_b], writes=[dst_b])
            src_b, dst_b = dst_b, (cs_b if dst_b is cs_a else cs_a)
        pend = src_b
        P.op("vector", C("tensor_tensor", out=pstart[:], in0=pend[:], in1=padded[:], op=ALU.subtract), reads=[pend, padded], writes=[pstart])
        P.op("vector", C("tensor_tensor", out=destm[:], in0=rankall[:], in1=pstart[:].unsqueeze(1).to_broadcast([128, NTT, NE]), op=ALU.add),
             reads=[rankall, pstart], writes=[destm])
        P.op("vector", C("scalar_tensor_tensor", out=destm[:], in0=destm[:], scalar=1.0, in1=maskall[:], op0=ALU.add, op1=ALU.mult), reads=[destm, maskall], writes=[destm])
        P.op("vector", C("tensor_scalar", out=destm[:], in0=destm[:], scalar1=-1.0, scalar2=None, op0=ALU.add), reads=[destm], writes=[destm])
        for tl in range(NTT):
            P.op("vector", C("max", out=d8[:, tl, :], in_=destm[:, tl, :]), reads=[destm], writes=[d8])
        P.op("vector", C("tensor_copy", out=didx[:], in_=d8[:, :, 0:4]), reads=[d8], writes=[didx])
        P.op("gpsimd", C("memset", pay[:], 0.0), writes=[pay])
        P.op("vector", C("tensor_copy", out=pay[:, :, :, 0], in_=tokid[:].unsqueeze(2).to_broadcast([128, NTT, 4])), reads=[tokid, pay], writes=[pay])
        P.op("vector", C("scalar_tensor_tensor", out=pay[:, :, :, 1], in0=tokid[:].unsqueeze(2).to_broadcast([128, NTT, 4]), scalar=4.0, in1=j4[:],
                         op0=ALU.mult, op1=ALU.add), reads=[tokid, j4, pay], writes=[pay])
        for j in range(4):
            P.op("vector", C("tensor_tensor", out=eqt[:], in0=destm[:], in1=d8[:, :, j:j + 1].to_broadcast([128, NTT, NE]), op=ALU.is_equal),
                 reads=[destm, d8], writes=[eqt])
            P.op("vector", C("tensor_tensor", out=eqt[:], in0=eqt[:], in1=gates[:], op=ALU.mult), reads=[eqt, gates], writes=[eqt])
            P.op("vector", C("tensor_reduce", out=pay[:, :, j, 2], in_=eqt[:], axis=mybir.AxisListType.X, op=ALU.add), reads=[eqt, pay], writes=[pay])
        P.op("vector", C("tensor_scalar", out=pay[:, :, :, 2], in0=pay[:, :, :, 2], scalar1=1.0 / 1.702, scalar2=None, op0=ALU.mult), reads=[pay], writes=[pay])
        for tl in range(NTT):
            for j in range(4):
                P._mk("gpsimd", C("indirect_dma_start", out=slot_d[:, :], out_offset=bass.IndirectOffsetOnAxis(ap=didx[:, tl, j:j + 1], axis=0),
                                   in_=pay[:, tl, j, :], in_offset=None), [didx, pay, slottab], [], True)
        P.op("vector", C("tensor_tensor", out=cmpk[:], in0=pend[:].unsqueeze(1).to_broadcast([128, NBLK, NE]), in1=kk[:], op=ALU.is_le), reads=[pend, kk], writes=[cmpk])
        P.op("vector", C("tensor_reduce", out=bexp[:], in_=cmpk[:], axis=mybir.AxisListType.X, op=ALU.add), reads=[cmpk], writes=[bexp])
        P.op("vector", C("tensor_scalar", out=bexp1024[:], in0=bexp[:], scalar1=1024.0, scalar2=None, op0=ALU.mult), reads=[bexp], writes=[bexp1024])
        if "slot" in dbg_d and b == 0:
            dbg("bexp", bexp[:], bexp)
            dbg("pend", pend[:], pend)
        phase_end(ltri, kk, tokid, j4, Srun, rankall, cnt, ci, cf, cfx, padded, cs_a, cs_b, pstart, destm, d8, didx, eqt, pay, cmpk, maskall, gates)
        if "slot" in dbg_d and b == 0:
            stmp = A.alloc("stmp", 128, [NSLOT // 128, 4], F32)
            P.dma("sync", stmp[:], slot_d.rearrange("(a p) c -> p a c", p=128), reads=[slottab], writes=[stmp])
            dbg("slot", stmp[:], stmp)
            A.release(stmp)
        if stop_after == 7:
            P.finish(out_ops)
            P.emit()
            return nc

        fcp = A.alloc("fcp", 128, [8], F32)
        ecol = A.alloc("ecol", 128, [1], F32)
        b1all = A.alloc("b1all", 128, [2048], BF16)
        b2all = A.alloc("b2all", 128, [D], BF16)
        b2f = A.alloc("b2f", 128, [D], F32)
        P.dma("sync", fcp[:], fcp_d[:, :], writes=[fcp])
        P.dma("sync", ecol[:], ecol_d[:, :], writes=[ecol])
        P.dma("gpsimd", b1all[0:NE, :], b1n_d[:, :], writes=[b1all])
        P.dma("sync", b2f[0:NE, :], b2_d[:, :], writes=[b2f])
        P.op("vector", C("tensor_scalar", out=b2all[0:NE, :], in0=b2f[0:NE, :], scalar1=1.702, scalar2=None, op0=ALU.mult), reads=[b2f], writes=[b2all])
        NU = 16
        w1u = [A.alloc("w1u%d" % i, 128, [KC, 2, 128], BF16) for i in range(NU)]
        w2b = [A.alloc("w2b%d" % i, 128, [8, D], BF16) for i in range(2)]
        xg = [A.alloc("xg%d" % i, 128, [JT, D], BF16) for i in range(2)]
        xgT = [A.alloc("xgT%d" % i, 128, [KC, BLK], BF16) for i in range(2)]
        actT = [A.alloc("actT%d" % i, 128, [8, BLK], BF16) for i in range(2)]
        ys = [A.alloc("ys%d" % i, 128, [D], F32) for i in range(4)]
        stb = [A.alloc("stb%d" % i, 128, [JT, 4], F32) for i in range(2)]
        gidx = [A.alloc("gidx%d" % i, 128, [JT], I32) for i in range(2)]
        sidx = [A.alloc("sidx%d" % i, 128, [JT], I32) for i in range(2)]
        widf = [A.alloc("widf%d" % i, 128, [8], F32) for i in range(2)]
        widx = [A.alloc("widx%d" % i, 128, [8], I32) for i in range(2)]
        ohb = [A.alloc("ohb%d" % i, 128, [BLK], BF16) for i in range(2)]
        tg = [A.alloc("tg%d" % i, 128, [512], F32) for i in range(2)]
        tsg = [A.alloc("tsg%d" % i, 128, [512], F32) for i in range(2)]
        tlin = [A.alloc("tlin%d" % i, 128, [512], F32) for i in range(2)]
        w1rows = w1_d[:, :]
        w2rows = w2_d[:, :]
        ucount = [0]
        blk_units = {}

        regcache = {}

        def wgather(out_ap, src_ap, idx_ap):
            def fn(e):
                if "bc" not in regcache:
                    regcache["bc"] = e.to_reg(NE * 1024 - 1)
                return e.indirect_dma_start(out=out_ap, out_offset=None, in_=src_ap, in_offset=bass.IndirectOffsetOnAxis(ap=idx_ap, axis=0),
                                            bounds_check=regcache["bc"], oob_is_err=False)
            return fn

        def issue_w2(k):
            pp = k % 2
            for fc in range(8):
                P._mk("gpsimd", wgather(w2b[pp][:, fc, :], w2rows, widx[pp][:, fc:fc + 1]), [widx[pp]], [w2b[pp]], True)

        def issue_unit(k, fc):
            pp = k % 2
            wu = w1u[ucount[0] % NU]
            ucount[0] += 1
            blk_units.setdefault(k, []).append(wu)
            P._mk("gpsimd", wgather(wu[:].rearrange("p k g f -> p (k g f)"), w1rows, widx[pp][:, fc:fc + 1]), [widx[pp], wu], [wu], True)

        ORDER = [0, 1]
        lo_, hi_ = 2, NBLK - 1
        while lo_ <= hi_:
            ORDER.append(hi_)
            hi_ -= 1
            if lo_ <= hi_:
                ORDER.append(lo_)
                lo_ += 1
        assert sorted(ORDER) == list(range(NBLK))

        def moe_loads(k):
            pp = k % 2
            blk = ORDER[k]
            P.dma("sync", stb[pp][:], slot_d[blk * BLK:(blk + 1) * BLK, :].rearrange("(j p) c -> p j c", p=128), reads=[slottab], writes=[stb[pp]])
            P.op("vector", C("tensor_copy", out=gidx[pp][:], in_=stb[pp][:, :, 0]), reads=[stb[pp]], writes=[gidx[pp]])
            P.op("vector", C("tensor_copy", out=sidx[pp][:], in_=stb[pp][:, :, 1]), reads=[stb[pp]], writes=[sidx[pp]])
            P.op("vector", C("tensor_scalar", out=widf[pp][:], in0=fcp[:], scalar1=bexp1024[:, blk:blk + 1], scalar2=None, op0=ALU.add), reads=[fcp, bexp1024], writes=[widf[pp]])
            P.op("vector", C("tensor_copy", out=widx[pp][:], in_=widf[pp][:]), reads=[widf[pp]], writes=[widx[pp]])
            P.op("vector", C("tensor_scalar", out=ohb[pp][0:NE, :], in0=ecol[0:NE, 0:1].to_broadcast([NE, BLK]), scalar1=bexp[0:NE, blk:blk + 1], scalar2=None, op0=ALU.is_equal),
                 reads=[ecol, bexp], writes=[ohb[pp]])
            for j in range(JT):
                P._mk("gpsimd", C("indirect_dma_start", out=xg[pp][:, j, :], out_offset=None, in_=h2d_d[:, :],
                                   in_offset=bass.IndirectOffsetOnAxis(ap=gidx[pp][:, j:j + 1], axis=0)), [gidx[pp], h2rows], [xg[pp]], True)
            for fc in range(8):
                issue_unit(k, fc)
            issue_w2(k)

        pcs = [0]
        stp = [0]

        def moe_transposes(k):
            pp = k % 2
            for j in range(JT):
                pz = psb[j % 2]
                pzv = pz.ap.bitcast(BF16)
                for kc in range(KC):
                    P.op("tensor", C("transpose", out=pzv[:, kc * 128:(kc + 1) * 128], in_=xg[pp][:, j, kc * 128:(kc + 1) * 128], identity=ident_b[:]),
                         reads=[xg[pp], ident_b], writes=[pz])
                evac(xgT[pp][:, :, j * 128:(j + 1) * 128], pzv.rearrange("p (k t) -> p k t", k=KC), [pz], [xgT[pp]])

        def moe_compute(k):
            pp = k % 2
            us = blk_units[k]
            for fc in range(8):
                wu = us[fc]
                pg_, pl_ = psb[2 + (pcs[0] % 2) * 2], psb[3 + (pcs[0] % 2) * 2]
                pcs[0] += 1
                tg_, tsg_, tlin_ = tg[stp[0] % 2], tsg[stp[0] % 2], tlin[stp[0] % 2]
                stp[0] += 1
                for kc in range(KC):
                    mm(pg_[:, 0:BLK], wu[:, kc, 0, :], xgT[pp][:, kc, :], kc == 0, False, [wu, xgT[pp]], [pg_])
                mm(pg_[:, 0:BLK], b1all[0:NE, fc * 128:(fc + 1) * 128], ohb[pp][0:NE, :], False, True, [b1all, ohb[pp]], [pg_])
                for kc in range(KC):
                    mm(pl_[:, 0:BLK], wu[:, kc, 1, :], xgT[pp][:, kc, :], kc == 0, False, [wu, xgT[pp]], [pl_])
                mm(pl_[:, 0:BLK], b1all[0:NE, 1024 + fc * 128:1024 + (fc + 1) * 128], ohb[pp][0:NE, :], False, True, [b1all, ohb[pp]], [pl_])
                P.op("vector", C("tensor_scalar", out=tg_[:, 0:BLK], in0=pg_[:, 0:BLK], scalar1=7.0, scalar2=None, op0=ALU.min), reads=[pg_], writes=[tg_])
                if "g_tg" in dbg_d and b == 0 and k == 0 and fc == 0:
                    dbg("g_tg", tg_[:], tg_)
                    dq = A.alloc("dq", 128, [2048], F32)
                    P.op("vector", C("tensor_copy", out=dq[:], in_=wu[:].rearrange("p k g f -> p (k g f)")), reads=[wu], writes=[dq])
                    dbg("g_wu", dq[:], dq)
                    dq2 = A.alloc("dq2", 128, [512], F32)
                    P.op("vector", C("tensor_copy", out=dq2[:], in_=ohb[pp][:]), reads=[ohb[pp]], writes=[dq2])
                    dbg("g_ohb", dq2[:], dq2)
                    dq3 = A.alloc("dq3", 128, [8], F32)
                    P.op("vector", C("tensor_copy", out=dq3[:], in_=widx[pp][:]), reads=[widx[pp]], writes=[dq3])
                    dbg("g_widx", dq3[:], dq3)
                P.op("scalar", C("activation", out=tsg_[:, 0:BLK], in_=tg_[:, 0:BLK], func=AF.Silu, scale=1.702), reads=[tg_], writes=[tsg_])
                P.op("vector", C("tensor_scalar", out=tlin_[:, 0:BLK], in0=pl_[:, 0:BLK], scalar1=7.0, scalar2=-7.0, op0=ALU.min, op1=ALU.max), reads=[pl_], writes=[tlin_])
                P.op("vector", C("scalar_tensor_tensor", out=actT[pp][:, fc, :], in0=tlin_[:, 0:BLK], scalar=1.0, in1=tsg_[:, 0:BLK], op0=ALU.add, op1=ALU.mult),
                     reads=[tlin_, tsg_], writes=[actT[pp]])
            if "g_xgT" in dbg_d and b == 0 and k == 0:
                dtmp = A.alloc("dtmp", 128, [KC * BLK], F32)
                P.op("vector", C("tensor_copy", out=dtmp[:], in_=xgT[pp][:].rearrange("p a b -> p (a b)")), reads=[xgT[pp]], writes=[dtmp])
                dbg("g_xgT", dtmp[:], dtmp)
                dtmp2 = dtmp
                P.op("vector", C("tensor_copy", out=dtmp2[:], in_=actT[pp][:].rearrange("p a b -> p (a b)")), reads=[actT[pp]], writes=[dtmp2])
                dbg("g_actT", dtmp2[:], dtmp2)
            if k + 1 < NBLK:
                moe_transposes(k + 1)
            for j in range(JT):
                ysj = ys[j % 4]
                for n2 in range(2):
                    py_ = psb[(6, 7, 0, 1)[(j * 2 + n2) % 4]]
                    for fc in range(8):
                        mm(py_[:, :], actT[pp][:, fc, j * 128:(j + 1) * 128], w2b[pp][:, fc, n2 * 512:(n2 + 1) * 512], fc == 0, False, [actT[pp], w2b[pp]], [py_])
                    mm(py_[:, :], ohb[pp][0:NE, 0:128], b2all[0:NE, n2 * 512:(n2 + 1) * 512], False, True, [ohb[pp], b2all], [py_])
                    if n2 == 0:
                        P.op("vector", C("tensor_scalar", out=ysj[:, 0:512], in0=py_[:, :], scalar1=stb[pp][:, j, 2:3], scalar2=None, op0=ALU.mult),
                             reads=[py_, stb[pp]], writes=[ysj])
                    else:
                        P.op("vector", C("tensor_scalar", out=ysj[:, 512:1024], in0=py_[:, :], scalar1=stb[pp][:, j, 2:3], scalar2=None, op0=ALU.mult),
                             reads=[py_, stb[pp]], writes=[ysj])
                if "g_ys" in dbg_d and b == 0 and k == 0 and j == 0:
                    dbg("g_ys", ysj[:], ysj)
                P._mk("gpsimd", C("indirect_dma_start", out=out4_d[:, :], out_offset=bass.IndirectOffsetOnAxis(ap=sidx[pp][:, j:j + 1], axis=0),
                                   in_=ysj[:], in_offset=None), [sidx[pp], ysj], [], True)

        moe_loads(0)
        moe_transposes(0)
        for k in range(NBLK):
            if k + 1 < NBLK:
                moe_loads(k + 1)
            moe_compute(k)
        phase_end(fcp, ecol, b1all, b2all, b2f, *w1u, *w2b, *xg, *xgT, *actT, *ys, *stb, *gidx, *sidx, *widf, *widx, *ohb, *tg, *tsg, *tlin, bexp, bexp1024)
        if stop_after == 8:
            P.finish(out_ops)
            P.emit()
            return nc

        xo = [A.alloc("xo%d" % i, 128, [D], F32) for i in range(4)]
        rows = [A.alloc("row%d" % i, 128, [D], F32) for i in range(4)]
        xin = [A.alloc("xin%d" % i, 128, [D], F32) for i in range(4)]
        xn_l = [A.alloc("xn%d" % i, 128, [D], F32) for i in range(4)]
        o4 = [A.alloc("o4%d" % i, 128, [4, D], F32) for i in range(4)]
        load_row(rows[0], modrows_d[0, 5 * D:6 * D], reads=[modrows])
        load_row(rows[3], modrows_d[1, 5 * D:6 * D], reads=[modrows])
        load_row(rows[1], ln_d[2, :])
        load_row(rows[2], ln_d[3, :])
        def tileH(b, tl):
            g2row = rows[0] if b == 0 else rows[3]
            xi = xin[tl % 4]
            o4t = o4[tl % 4]
            r0_ = (b * T + tl * 128) * 4
            P.dma("sync", xi[:], xmid_d[b * T + tl * 128:b * T + (tl + 1) * 128, :], reads=[xmid[b]], writes=[xi])
            P.dma("sync", o4t[:], out4_d[r0_:r0_ + 512, :].rearrange("(p j) n -> p j n", j=4), writes=[o4t])
            xn = xn_l[tl % 4]
            P.op("vector", C("tensor_tensor", out=o4t[:, 0, :], in0=o4t[:, 0, :], in1=o4t[:, 1, :], op=ALU.add), reads=[o4t], writes=[o4t])
            yield
            P.op("vector", C("tensor_tensor", out=o4t[:, 2, :], in0=o4t[:, 2, :], in1=o4t[:, 3, :], op=ALU.add), reads=[o4t], writes=[o4t])
            yield
            P.op("vector", C("tensor_tensor", out=o4t[:, 0, :], in0=o4t[:, 0, :], in1=o4t[:, 2, :], op=ALU.add), reads=[o4t], writes=[o4t])
            yield
            if "fx" in dbg_d and b == 0:
                out_ops.append(P.dma("sync", dbg_d["fx"][tl * 128:(tl + 1) * 128, :], o4t[:, 0, :], reads=[o4t]))
            P.op("vector", C("tensor_tensor", out=xn[:], in0=o4t[:, 0, :], in1=g2row[:], op=ALU.mult), reads=[o4t, g2row], writes=[xn])
            yield
            P.op("vector", C("scalar_tensor_tensor", out=xn[:], in0=xi[:], scalar=ALPHA, in1=xn[:], op0=ALU.mult, op1=ALU.add), reads=[xi, xn], writes=[xn])
            yield
            xot = xo[tl % 4]
            yield from layer_norm_g(xn, xot)
            P.op("vector", C("tensor_tensor", out=xot[:], in0=xot[:], in1=rows[1][:], op=ALU.mult), reads=[xot, rows[1]], writes=[xot])
            yield
            P.op("vector", C("tensor_tensor", out=xot[:], in0=xot[:], in1=rows[2][:], op=ALU.add), reads=[xot, rows[2]], writes=[xot])
            yield
            o = P.dma("sync", out_d[b, tl * 128:(tl + 1) * 128, :], xot[:], reads=[xot])
            out_ops.append(o)

        for b in range(2):
            for tl in range(0, NT, 4):
                run_interleaved([tileH(b, tl + q) for q in range(4)])
        phase_end(*xo, *xin, *xn_l, *rows, *o4)
        P.finish(out_ops)
        P.emit()
    return nc


def _prep_shared(inp):
    f32 = np.float32
    cst = _consts()
    cols = _ext_cols()
    w_in_ext = np.ascontiguousarray(inp["w_in"][0][:, cols]).astype(f32)
    convw = inp["hy_conv_w"][0]
    convb = inp["hy_conv_b"][0]
    cp = np.concatenate([convw, convb[None]], axis=0).reshape(4, 12, 128).transpose(2, 0, 1)
    skipp = inp["hy_skip"][0].reshape(2, 4, 128).transpose(2, 0, 1)
    w1 = inp["exp_w1"][0]
    w1r = np.ascontiguousarray(w1.reshape(NE, 8, 128, 2, 8, 128).transpose(0, 4, 2, 1, 3, 5)).reshape(NE * 8 * 128, 2048)
    b1 = inp["exp_b1"][0]
    b1r = np.ascontiguousarray(b1.reshape(NE, 16, 128).transpose(2, 0, 1)).reshape(128, NE * 16)
    lnp = np.stack([inp["ln1_g"][0], inp["ln1_b"][0], inp["ln2_g"][0], inp["ln2_b"][0]], axis=0)
    sh = {
        "w_mod": inp["w_mod"][0], "b_mod": inp["b_mod"][0][None, :], "w_in": w_in_ext,
        "attn_sink": inp["attn_sink"][0][None, :],
        "convp": np.ascontiguousarray(cp).reshape(128, 48), "skipp": np.ascontiguousarray(skipp).reshape(128, 8),
        "fw1": inp["hy_filt_w1"][0], "fb1": inp["hy_filt_b1"][0][:, None], "fw2": inp["hy_filt_w2"][0],
        "fb2": inp["hy_filt_b2"][0][:, None], "fw3": inp["hy_filt_w3"][0],
        "w_ba": inp["w_branch_attn"][0], "w_bh": inp["w_branch_hyena"][0], "w_o": inp["w_out"][0],
        "lnp": lnp, "router_w": inp["router_w"][0], "router_b": inp["router_b"][0][None, :],
        "w1r": w1r, "b1r": b1r, "w2": inp["exp_w2"][0].reshape(NE * D, D), "b2": inp["exp_b2"][0],
        "ropeC": cst["ropeC"], "ropeS": cst["ropeS"], "masks": cst["masks"].reshape(128, 256), "zT": cst["zT"],
        "decay": cst["decay"], "decayb": cst["decayb"],
        "cft": cst["cft"], "sft": cst["sft"], "cit": cst["cit"], "sit": cst["sit"],
        "ltri": cst["ltri"], "kk512": cst["kk512"], "fcp": cst["fcp"], "ecol": cst["ecol"], "tokid": cst["tokid"], "j4": cst["j4"],
        "slot_init": cst["slot_init"], "b1n": inp["exp_b1"][0],
    }
    return {k: np.ascontiguousarray(v) for k, v in sh.items()}


def _run(inp, stop_after=None, cores=NCORES):
    inp = {k: np.asarray(v) for k, v in inp.items()}
    shared = _prep_shared(inp)
    nc = build(stop_after)
    in_maps = []
    for i in range(cores):
        m = dict(shared)
        m["x"] = np.ascontiguousarray(inp["x"][2 * i:2 * i + 2])
        m["ctx"] = np.ascontiguousarray(inp["ctx"][2 * i:2 * i + 2])
        m["c3"] = np.ascontiguousarray(np.concatenate([inp["c"][2 * i:2 * i + 2], inp["c_ctx"][None, :]], axis=0))
        in_maps.append(m)
    res = run_bass_kernel_spmd(nc, in_maps, core_ids=list(range(cores)))
    return res


def kernel(**inputs):
    res = _run(inputs)
    out = np.concatenate([r["out"] for r in res.results], axis=0)
    return out.astype(np.float32)
```

```python
import contextlib
import math
import numpy as np
import ml_dtypes
import concourse.bass as bass
import concourse.mybir as mybir
from concourse.bass_utils import run_bass_kernel_spmd

F32 = mybir.dt.float32
BF16 = mybir.dt.bfloat16
I32 = mybir.dt.int32
AF = mybir.ActivationFunctionType
ALU = mybir.AluOpType

ENGS = ["tensor", "vector", "scalar", "gpsimd", "sync"]
NCORES = 8
T = 2048
NT = 16
D = 1024
KC = 8
NE = 32
EXTW = 4992
GATE_BASE = 2944
HY_BASE = 1408
NBLK = 64
BLK = 512
JT = BLK // 128
NTT = 2 * NT
TT = 2 * T
NSLOT = NBLK * BLK
ALPHA = 2 ** 0.25
PI = math.pi
DEBUG = {}


def C(method, *a, **k):
    return lambda e: getattr(e, method)(*a, **k)


class Buf:
    __slots__ = ("ap", "name", "last_w", "readers", "off", "size")

    def __init__(self, ap, name=""):
        self.ap = ap
        self.name = name
        self.last_w = None
        self.readers = []

    def __getitem__(self, k):
        return self.ap[k]


class Op:
    __slots__ = ("id", "eng", "fn", "waits", "is_dma", "dsem", "dval", "needed", "val", "idx")


class Prog:
    NDSEM = 40

    def __init__(self, nc):
        self.nc = nc
        self.ops = []
        self.q = {e: [] for e in ENGS}
        self.dma_rr = 0
        self.dsem_last = [None] * self.NDSEM
        self.dsem_uses = [0] * self.NDSEM
        self.dma_since_barrier = []

    def _mk(self, eng, fn, reads, writes, is_dma):
        op = Op()
        op.id = len(self.ops)
        op.eng = eng
        op.fn = fn
        op.is_dma = is_dma
        op.needed = False
        op.val = None
        op.dsem = None
        op.dval = None
        deps = set()
        for b in reads:
            if b.last_w is not None:
                deps.add(b.last_w)
        for b in writes:
            if b.last_w is not None:
                deps.add(b.last_w)
            for r in b.readers:
                deps.add(r)
        if is_dma:
            k = self.dma_rr % self.NDSEM
            self.dma_rr += 1
            if self.dsem_last[k] is not None:
                deps.add(self.dsem_last[k])
            self.dsem_last[k] = op.id
            self.dsem_uses[k] += 1
            op.dsem = k
            op.dval = 16 * self.dsem_uses[k]
            self.dma_since_barrier.append(op.id)
        op.waits = deps
        self.ops.append(op)
        op.idx = len(self.q[eng])
        self.q[eng].append(op)
        for b in reads:
            b.readers.append(op.id)
        for b in writes:
            b.last_w = op.id
            b.readers = []
        return op

    def op(self, eng, fn, reads=(), writes=()):
        return self._mk(eng, fn, reads, writes, False)

    def dma(self, eng, out, in_, reads=(), writes=(), **kw):
        return self._mk(eng, C("dma_start", out=out, in_=in_, **kw), reads, writes, True)

    def barrier(self):
        last = [self.q[e][-1].id for e in ENGS if self.q[e] and not self.q[e][-1].is_dma]
        last = []
        for e in ENGS:
            for o in reversed(self.q[e]):
                if not o.is_dma and o.fn is not None:
                    last.append(o.id)
                    break
        dmas = list(self.dma_since_barrier)
        self.dma_since_barrier = []
        for e in ENGS:
            o = self._mk(e, None, (), (), False)
            o.waits = set(last) | set(dmas)

    def finish(self, out_ops):
        o = self._mk("sync", None, (), (), False)
        o.waits = set(x.id for x in out_ops)

    def emit(self):
        nc = self.nc
        ops = self.ops
        final_waits = {}
        for e in ENGS:
            seen = {}
            seen_d = {}
            for op in self.q[e]:
                wl = []
                for d in sorted(op.waits):
                    t = ops[d]
                    if t.is_dma:
                        if seen_d.get(t.dsem, 0) >= t.dval:
                            continue
                        seen_d[t.dsem] = t.dval
                        wl.append(d)
                    else:
                        if t.fn is None:
                            continue
                        if t.eng == e and e == "tensor":
                            continue
                        if seen.get(t.eng, -1) >= t.idx:
                            continue
                        seen[t.eng] = t.idx
                        wl.append(d)
                        t.needed = True
                final_waits[op.id] = wl
        for e in ENGS:
            c = 0
            for op in self.q[e]:
                if op.is_dma:
                    continue
                if op.needed:
                    c += 1
                    op.val = c
        with contextlib.ExitStack() as st:
            esem = {e: st.enter_context(nc.semaphore("s_" + e)) for e in ENGS}
            dsem = [st.enter_context(nc.semaphore("d%d" % i)) for i in range(self.NDSEM)]
            block = st.enter_context(nc.Block())

            def run(e, eng):
                for op in self.q[e]:
                    for d in final_waits[op.id]:
                        t = ops[d]
                        if t.is_dma:
                            eng.wait_ge(dsem[t.dsem], t.dval)
                        else:
                            eng.wait_ge(esem[t.eng], t.val)
                    if op.fn is None:
                        continue
                    ins = op.fn(eng)
                    if op.is_dma:
                        ins.then_inc(dsem[op.dsem], 16)
                    elif op.needed:
                        ins.then_inc(esem[e], 1)

            @block.tensor
            def _(eng):
                run("tensor", eng)

            @block.vector
            def _(eng):
                run("vector", eng)

            @block.scalar
            def _(eng):
                run("scalar", eng)

            @block.gpsimd
            def _(eng):
                run("gpsimd", eng)

            @block.sync
            def _(eng):
                run("sync", eng)


class Arena:
    def __init__(self, big_ap, nbytes):
        self.big = big_ap
        self.free = [(0, nbytes)]
        self.pending = []
        self.live = []

    def alloc(self, name, parts, shape, dtype):
        esz = 4 if dtype in (F32, I32) else 2
        n = int(np.prod(shape))
        size = (n * esz + 63) // 64 * 64
        for i, (o, s) in enumerate(self.free):
            if s >= size:
                if s == size:
                    self.free.pop(i)
                else:
                    self.free[i] = (o + size, s - size)
                ap = self.big[0:parts, o // 2:(o + n * esz) // 2]
                if dtype != BF16:
                    ap = ap.bitcast(dtype)
                if len(shape) > 1:
                    names = " ".join("a%d" % i for i in range(len(shape)))
                    kw = {"a%d" % i: shape[i] for i in range(1, len(shape))}
                    ap = ap.rearrange("p (%s) -> p %s" % (names, names), **kw)
                b = Buf(ap, name)
                b.off = o
                b.size = size
                for (lo, ls, ln) in self.live:
                    if o < lo + ls and lo < o + size:
                        raise RuntimeError("OVERLAP %s with %s" % (name, ln))
                self.live.append((o, size, name))
                return b
        raise RuntimeError("arena OOM for %s (%d bytes); free=%s" % (name, size, self.free))

    def release(self, *bufs):
        for b in bufs:
            self.pending.append((b.off, b.size))
            k = [x for x in self.live if x[0] == b.off and x[1] == b.size]
            if len(k) != 1:
                raise RuntimeError("bad release %s %s" % (b.name, k))
            self.live.remove(k[0])

    def commit(self):
        fl = self.free + self.pending
        self.pending = []
        fl.sort()
        out = []
        for o, s in fl:
            if out and out[-1][0] + out[-1][1] == o:
                out[-1] = (out[-1][0], out[-1][1] + s)
            else:
                out.append((o, s))
        self.free = out


_CONST_CACHE = {}


def _consts():
    if _CONST_CACHE:
        return _CONST_CACHE
    L = T
    t = np.arange(L)
    row = (t // 64).astype(np.float32)
    col = (t % 64).astype(np.float32)
    inv = (np.float32(10000.0) ** (-np.arange(16, dtype=np.float32) / np.float32(16))).astype(np.float32)
    d = np.arange(64)
    a = d // 32
    half = (d % 32) // 16
    f = d % 16
    pos = np.where(a[:, None] == 0, row[None, :], col[None, :]).astype(np.float32)
    ang = (pos * inv[f][:, None]).astype(np.float32).astype(np.float64)
    C = np.cos(ang)
    S = np.sin(ang) * np.where(half == 0, -1.0, 1.0)[:, None]
    ropeC = np.tile(C, (2, 1)).astype(np.float32)
    ropeS = np.tile(S, (2, 1)).astype(np.float32)
    s_i = np.arange(128)[:, None]
    q_i = np.arange(128)[None, :]
    maskP = (q_i <= s_i).astype(np.float32)
    maskN = (s_i <= q_i).astype(np.float32)
    masks = np.stack([maskP, maskN], axis=1).astype(ml_dtypes.bfloat16)
    tl = np.linspace(0.0, 1.0, L, dtype=np.float32)
    omega = (np.float32(2.0 * math.pi) * np.arange(L, dtype=np.float32) / np.float32(L)).astype(np.float32)
    fb = np.linspace(1e-4, 15, 16, dtype=np.float32)
    fo = (fb[None, :] * omega[:, None]).astype(np.float32).astype(np.float64)
    z = np.concatenate([tl[:, None].astype(np.float64), np.cos(fo), -np.sin(fo)], axis=-1)
    zT = np.ascontiguousarray(z.T).astype(np.float32)
    min_decay = math.log(1e-2) / 0.3
    max_decay = math.log(1e-2) / 1.5
    deltas = np.abs(np.linspace(min_decay, max_decay, 512, dtype=np.float32)).astype(np.float64)
    decay = np.exp(-tl[:, None].astype(np.float64) * deltas[None, :]).astype(np.float32)
    decayb = decay.copy()
    decayb[0] = 0.0
    N = 2 * L
    tt = np.arange(L, dtype=np.float64)
    ff = np.arange(L, dtype=np.float64) + 0.5
    th = 2.0 * math.pi * np.outer(tt, ff) / N
    Cf = np.cos(th)
    Sf = np.sin(th)
    bf = ml_dtypes.bfloat16

    def tile_fwd(M):
        return np.ascontiguousarray(M.reshape(16, 128, 16, 128).transpose(2, 1, 0, 3)).astype(bf).reshape(16, 128, 2048)

    def tile_inv(M):
        return np.ascontiguousarray(M.reshape(16, 128, 4, 512).transpose(2, 1, 0, 3)).astype(bf).reshape(4, 128, 8192)

    pp_ = np.arange(128)
    ltri = (pp_[:, None] < pp_[None, :]).astype(np.float32)
    kk512 = np.tile(np.repeat(np.arange(NBLK, dtype=np.float32) * BLK, NE)[None, :], (128, 1))
    fcp = (np.arange(8)[None, :] * 128 + pp_[:, None]).astype(np.float32)
    ecol = pp_[:, None].astype(np.float32)
    tokid = (np.arange(NTT)[None, :] * 128 + pp_[:, None]).astype(np.float32)
    j4 = np.tile(np.tile(np.arange(4, dtype=np.float32), NTT)[None, :], (128, 1))
    sl = np.arange(NSLOT)
    slot_init = np.stack([TT + sl % 128, 4 * TT + sl % 128, np.zeros(NSLOT), np.zeros(NSLOT)], axis=1).astype(np.float32)
    _CONST_CACHE.update(dict(
        ltri=ltri, kk512=np.ascontiguousarray(kk512), fcp=fcp, ecol=ecol, tokid=tokid, j4=np.ascontiguousarray(j4), slot_init=slot_init,
        ropeC=ropeC, ropeS=ropeS, masks=masks, zT=zT, decay=decay, decayb=decayb,
        cft=tile_fwd(Cf), sft=tile_fwd(Sf),
        cit=tile_inv(Cf.T * (2.0 / N)), sit=tile_inv(Sf.T * (2.0 / N)),
    ))
    return _CONST_CACHE


def _ext_cols():
    q_cols, qp_cols = [], []
    for c in range(4):
        for hf in range(2):
            h = c + 4 * hf
            for dd in range(64):
                q_cols.append(h * 64 + dd)
                qp_cols.append(h * 64 + (dd ^ 16))
    k_cols = [512 + i for i in range(128)]
    kp_cols = [512 + (i // 64) * 64 + ((i % 64) ^ 16) for i in range(128)]
    v_cols = [640 + i for i in range(128)]
    rest = list(range(768, 4352))
    return np.array(q_cols + qp_cols + k_cols + kp_cols + v_cols + rest)


def build(stop_after=None):
    nc = bass.Bass("TRN2", target_bir_lowering=False)

    def din(name, shape, dt=F32):
        return nc.dram_tensor(name, list(shape), dt, kind="ExternalInput").ap()

    x_d = din("x", [2, T, D])
    c3_d = din("c3", [3, D])
    ctx_d = din("ctx", [2, 256, D])
    wmod_d = din("w_mod", [D, 6144])
    bmod_d = din("b_mod", [1, 6144])
    win_d = din("w_in", [D, EXTW])
    sink_d = din("attn_sink", [1, 8])
    convp_d = din("convp", [128, 48])
    skip_d = din("skipp", [128, 8])
    fw1_d = din("fw1", [33, 64])
    fb1_d = din("fb1", [64, 1])
    fw2_d = din("fw2", [64, 64])
    fb2_d = din("fb2", [64, 1])
    fw3_d = din("fw3", [64, 2048])
    wba_d = din("w_ba", [512, D])
    wbh_d = din("w_bh", [512, D])
    wo_d = din("w_o", [D, D])
    ln_d = din("lnp", [4, D])
    rw_d = din("router_w", [D, NE])
    rb_d = din("router_b", [1, NE])
    w1_d = din("w1r", [NE * 8 * 128, 2048])
    b1_d = din("b1r", [128, NE * 16])
    w2_d = din("w2", [NE * D, D])
    b2_d = din("b2", [NE, D])
    ropeC_d = din("ropeC", [128, T])
    ropeS_d = din("ropeS", [128, T])
    masks_d = din("masks", [128, 256], BF16)
    zT_d = din("zT", [33, T])
    decay_d = din("decay", [T, 512])
    decayb_d = din("decayb", [T, 512])
    cft_d = din("cft", [16, 128, 2048], BF16)
    sft_d = din("sft", [16, 128, 2048], BF16)
    cit_d = din("cit", [4, 128, 8192], BF16)
    sit_d = din("sit", [4, 128, 8192], BF16)
    ltri_d = din("ltri", [128, 128])
    kk512_d = din("kk512", [128, NBLK * NE])
    fcp_d = din("fcp", [128, 8])
    ecol_d = din("ecol", [128, 1])
    tokid_d = din("tokid", [128, NTT])
    j4_d = din("j4", [128, NTT * 4])
    slotinit_d = din("slot_init", [NSLOT, 4])
    b1n_d = din("b1n", [NE, 2048])
    out_d = nc.dram_tensor("out", [2, T, D], F32, kind="ExternalOutput").ap()
    dbg_d = {k: nc.dram_tensor("dbg_" + k, list(s), F32, kind="ExternalOutput").ap() for k, s in DEBUG.items()}

    modrows_d = nc.dram_tensor("modrows", [3, 6144], F32).ap()
    xmid_d = nc.dram_tensor("xmid", [2 * T, D], F32).ap()
    hr_d = nc.dram_tensor("hr_s", [2, T, 512], BF16).ap()
    hs_d = nc.dram_tensor("hs_s", [2, T, 512], BF16).ap()
    hxs_d = nc.dram_tensor("hx_s", [128, KC * T], BF16).ap()
    atts_d = nc.dram_tensor("att_s", [64, 8 * T], BF16).ap()
    h2d_d = nc.dram_tensor("h2_rows", [TT + 128, D], BF16).ap()
    slot_d = nc.dram_tensor("slot_tab", [NSLOT, 4], F32).ap()
    out4_d = nc.dram_tensor("out4", [4 * TT + 128, D], F32).ap()
    h2rows = Buf(h2d_d, "h2rows")
    slottab = Buf(slot_d, "slottab")
    modrows = Buf(modrows_d, "modrows")
    xmid = [Buf(xmid_d, "xmid0"), Buf(xmid_d, "xmid1")]
    hspec = Buf(hr_d, "hspec")

    ARENA_BYTES = 206 * 1024
    st = contextlib.ExitStack()
    with st:
        P = Prog(nc)
        big = st.enter_context(nc.sbuf_tensor("arena", [128, ARENA_BYTES // 2], BF16))
        A = Arena(big, ARENA_BYTES)
        psb = [Buf(st.enter_context(nc.psum_tensor("ps%d" % i, [128, 512], F32)), "ps%d" % i) for i in range(8)]
        out_ops = []

        def phase_end(*bufs):
            A.release(*bufs)
            P.barrier()
            A.commit()

        cpy_rr = [0]

        def evac(out_ap, in_ap, reads, writes, eng=None):
            if eng is None:
                eng = ("scalar", "vector")[cpy_rr[0] % 2]
                cpy_rr[0] += 1
            if eng == "scalar":
                P.op("scalar", C("activation", out=out_ap, in_=in_ap, func=AF.Copy), reads=reads, writes=writes)
            else:
                P.op(eng, C("tensor_copy", out=out_ap, in_=in_ap), reads=reads, writes=writes)

        def mm(out_ap, lhsT, rhs, start, stop, reads, writes):
            P.op("tensor", C("matmul", out_ap, lhsT=lhsT, rhs=rhs, start=start, stop=stop), reads=reads, writes=writes)

        ident_f = A.alloc("ident_f", 128, [128], F32)
        ident_b = A.alloc("ident_b", 128, [128], BF16)
        ones_b = A.alloc("ones_b", 128, [64], BF16)
        ones_f = A.alloc("ones_f", 128, [128], F32)
        lnst_l = [A.alloc("lnst%d" % i, 128, [2, 6], F32) for i in range(4)]
        lnmv_l = [A.alloc("lnmv%d" % i, 128, [2], F32) for i in range(4)]
        lnrs_l = [A.alloc("lnrs%d" % i, 128, [1], F32) for i in range(4)]
        ln_rr = [0]
        P.op("gpsimd", C("memset", ident_f[:], 1.0), writes=[ident_f])
        P.op("gpsimd", C("affine_select", out=ident_f[:], in_=ident_f[:], pattern=[[-1, 128]], compare_op=ALU.is_equal,
                                                  fill=0.0, base=0, channel_multiplier=1), reads=[ident_f], writes=[ident_f])
        P.op("vector", C("tensor_copy", out=ident_b[:], in_=ident_f[:]), reads=[ident_f], writes=[ident_b])
        P.op("gpsimd", C("memset", ones_b[:], 1.0), writes=[ones_b])
        P.op("gpsimd", C("memset", ones_f[:], 1.0), writes=[ones_f])
        ones_sq = ones_f
        zrow = A.alloc("zrow", 128, [D], BF16)
        P.op("gpsimd", C("memset", zrow[:], 0.0), writes=[zrow])
        P.dma("sync", h2d_d[TT:TT + 128, :], zrow[:], reads=[zrow])

        def layer_norm(src, dst, reads_extra=()):
            lnst, lnmv, lnrs = lnst_l[ln_rr[0] % 4], lnmv_l[ln_rr[0] % 4], lnrs_l[ln_rr[0] % 4]
            ln_rr[0] += 1
            for i in range(2):
                P.op("vector", C("bn_stats", out=lnst[:, i, :], in_=src[:, i * 512:(i + 1) * 512]), reads=[src], writes=[lnst])
            P.op("vector", C("bn_aggr", out=lnmv[:], in_=lnst[:]), reads=[lnst], writes=[lnmv])
            P.op("scalar", C("activation", out=lnrs[:], in_=lnmv[:, 1:2], func=AF.Sqrt, bias=1e-5), reads=[lnmv], writes=[lnrs])
            P.op("vector", C("reciprocal", out=lnrs[:], in_=lnrs[:]), reads=[lnrs], writes=[lnrs])
            P.op("vector", C("tensor_scalar", out=dst[:], in0=src[:], scalar1=lnmv[:, 0:1], scalar2=lnrs[:, 0:1],
                                                     op0=ALU.subtract, op1=ALU.mult), reads=[src, lnmv, lnrs], writes=[dst])

        def layer_norm_g(src, dst):
            lnst, lnmv, lnrs = lnst_l[ln_rr[0] % 4], lnmv_l[ln_rr[0] % 4], lnrs_l[ln_rr[0] % 4]
            ln_rr[0] += 1
            for i in range(2):
                P.op("vector", C("bn_stats", out=lnst[:, i, :], in_=src[:, i * 512:(i + 1) * 512]), reads=[src], writes=[lnst])
            yield
            P.op("vector", C("bn_aggr", out=lnmv[:], in_=lnst[:]), reads=[lnst], writes=[lnmv])
            yield
            P.op("scalar", C("activation", out=lnrs[:], in_=lnmv[:, 1:2], func=AF.Sqrt, bias=1e-5), reads=[lnmv], writes=[lnrs])
            yield
            P.op("vector", C("reciprocal", out=lnrs[:], in_=lnrs[:]), reads=[lnrs], writes=[lnrs])
            yield
            P.op("vector", C("tensor_scalar", out=dst[:], in0=src[:], scalar1=lnmv[:, 0:1], scalar2=lnrs[:, 0:1],
                             op0=ALU.subtract, op1=ALU.mult), reads=[src, lnmv, lnrs], writes=[dst])
            yield

        def modulate_g(t, scrow, shrow):
            P.op("gpsimd", C("tensor_tensor", out=t[:], in0=t[:], in1=scrow[:], op=ALU.mult), reads=[t, scrow], writes=[t])
            yield
            P.op("vector", C("tensor_tensor", out=t[:], in0=t[:], in1=shrow[:], op=ALU.add), reads=[t, shrow], writes=[t])
            yield

        def run_interleaved(gens):
            active = list(gens)
            while active:
                nxt = []
                for g_ in active:
                    try:
                        next(g_)
                        nxt.append(g_)
                    except StopIteration:
                        pass
                active = nxt

        def load_row(buf, dram_row_ap, reads=(), plus1=False):
            P.dma("sync", buf[:], dram_row_ap.partition_broadcast(128), reads=reads, writes=[buf])
            if plus1:
                P.op("gpsimd", C("tensor_scalar", out=buf[:], in0=buf[:], scalar1=1.0, scalar2=None, op0=ALU.add), reads=[buf], writes=[buf])

        def modulate(t, scrow, shrow):
            P.op("gpsimd", C("tensor_tensor", out=t[:], in0=t[:], in1=scrow[:], op=ALU.mult), reads=[t, scrow], writes=[t])
            P.op("vector", C("tensor_tensor", out=t[:], in0=t[:], in1=shrow[:], op=ALU.add), reads=[t, shrow], writes=[t])

        def transpose_to(dst_ap, src_bf, pbank, reads, writes):
            pv = pbank.ap.bitcast(BF16)
            for kc in range(KC):
                P.op("tensor", C("transpose", out=pv[:, kc * 128:(kc + 1) * 128], in_=src_bf[:, kc * 128:(kc + 1) * 128],
                                                            identity=ident_b[:]), reads=[src_bf, ident_b], writes=[pbank])
            evac(dst_ap, pv.rearrange("p (k t) -> p k t", k=KC), reads=[pbank] + list(reads), writes=writes)

        def dbg(name, sb_ap, buf, dst=None):
            if name in dbg_d:
                o = P.dma("sync", dbg_d[name] if dst is None else dst, sb_ap, reads=[buf])
                out_ops.append(o)

        c3 = A.alloc("c3", 128, [D], F32)
        sT = A.alloc("sT", 128, [KC, 3], F32)
        bmr = A.alloc("bmr", 128, [6144], F32)
        mrow = A.alloc("mrow", 128, [6144], F32)
        wm = [A.alloc("wm%d" % i, 128, [KC, 512], F32) for i in range(2)]
        P.dma("sync", c3[0:3, :], c3_d[:, :], writes=[c3])
        P.dma("sync", bmr[0:1, :], bmod_d[:, :], writes=[bmr])
        for kc in range(KC):
            P.op("tensor", C("transpose", out=psb[0][:, kc * 4:kc * 4 + 3], in_=c3[0:3, kc * 128:(kc + 1) * 128],
                                                        identity=ident_f[0:3, 0:3]), reads=[c3, ident_f], writes=[psb[0]])
        P.op("scalar", C("activation", out=sT[:], in_=psb[0][:, 0:32].rearrange("p (k j) -> p k j", j=4)[:, :, 0:3], func=AF.Silu),
             reads=[psb[0]], writes=[sT])
        for n in range(12):
            w = wm[n % 2]
            P.dma("sync", w[:], wmod_d[:, n * 512:(n + 1) * 512].rearrange("(k p) n -> p k n", p=128), writes=[w])
            pb = psb[1 + n % 2]
            for kc in range(KC):
                mm(pb[0:3, :], sT[:, kc, :], w[:, kc, :], kc == 0, False, [sT, w], [pb])
            mm(pb[0:3, :], ones_f[0:1, 0:3], bmr[0:1, n * 512:(n + 1) * 512], False, True, [ones_f, bmr], [pb])
            evac(mrow[0:3, n * 512:(n + 1) * 512], pb[0:3, :], [pb], [mrow])
        P.dma("sync", modrows_d[:, :], mrow[0:3, :], reads=[mrow], writes=[modrows])
        dbg("mod", mrow[0:3, :], mrow)
        phase_end(c3, sT, bmr, mrow, wm[0], wm[1])
        if stop_after == 0:
            P.finish(out_ops)
            P.emit()
            return nc

        zT = A.alloc("zT", 128, [T], F32)
        fw1 = A.alloc("fw1", 128, [64], F32)
        fw2 = A.alloc("fw2", 128, [64], F32)
        fw3 = A.alloc("fw3", 128, [2048], F32)
        fb = A.alloc("fb", 128, [2], F32)
        h1T = A.alloc("h1T", 128, [T], F32)
        h2T_f = A.alloc("h2T_f", 128, [T], F32)
        su = A.alloc("su", 128, [512], F32)
        sk = A.alloc("sk", 128, [512], I32)
        skf = A.alloc("skf", 128, [512], F32)
        sfx = A.alloc("sfx", 128, [512], F32)
        P.dma("sync", zT[0:33, :], zT_d[:, :], writes=[zT])
        P.dma("sync", fw1[0:33, :], fw1_d[:, :], writes=[fw1])
        P.dma("sync", fw2[0:64, :], fw2_d[:, :], writes=[fw2])
        P.dma("sync", fw3[0:64, :], fw3_d[:, :], writes=[fw3])
        P.dma("sync", fb[0:64, 0:1], fb1_d[:, :], writes=[fb])
        P.dma("sync", fb[0:64, 1:2], fb2_d[:, :], reads=[], writes=[fb])
        P.op("vector", C("tensor_scalar", out=fb[0:64, :], in0=fb[0:64, :], scalar1=9.0 * PI, scalar2=None, op0=ALU.add), reads=[fb], writes=[fb])

        def sin_layer(wbuf, kdim, src, dst, bcol):
            for tch in range(4):
                pb = psb[tch % 2]
                mm(pb[0:64, :], wbuf[0:kdim, 0:64], src[0:kdim, tch * 512:(tch + 1) * 512], True, True, [wbuf, src], [pb])
                P.op("vector", C("tensor_scalar", out=su[0:64, :], in0=pb[0:64, :], scalar1=fb[0:64, bcol:bcol + 1], scalar2=None, op0=ALU.add),
                     reads=[pb, fb], writes=[su])
                P.op("vector", C("tensor_scalar", out=sk[0:64, :], in0=su[0:64, :], scalar1=1.0 / (2 * PI), scalar2=None, op0=ALU.mult), reads=[su], writes=[sk])
                P.op("vector", C("tensor_copy", out=skf[0:64, :], in_=sk[0:64, :]), reads=[sk], writes=[skf])
                P.op("vector", C("scalar_tensor_tensor", out=su[0:64, :], in0=skf[0:64, :], scalar=-2.0 * PI, in1=su[0:64, :], op0=ALU.mult, op1=ALU.add),
                     reads=[skf, su], writes=[su])
                P.op("vector", C("tensor_scalar", out=sfx[0:64, :], in0=su[0:64, :], scalar1=0.0, scalar2=2.0 * PI, op0=ALU.is_lt, op1=ALU.mult), reads=[su], writes=[sfx])
                P.op("vector", C("tensor_tensor", out=su[0:64, :], in0=su[0:64, :], in1=sfx[0:64, :], op=ALU.add), reads=[su, sfx], writes=[su])
                P.op("vector", C("tensor_scalar", out=su[0:64, :], in0=su[0:64, :], scalar1=-PI, scalar2=PI, op0=ALU.add, op1=ALU.min), reads=[su], writes=[su])
                P.op("vector", C("tensor_scalar", out=su[0:64, :], in0=su[0:64, :], scalar1=-PI, scalar2=None, op0=ALU.max), reads=[su], writes=[su])
                P.op("scalar", C("activation", out=dst[0:64, tch * 512:(tch + 1) * 512], in_=su[0:64, :], func=AF.Sin), reads=[su], writes=[dst])

        sin_layer(fw1, 33, zT, h1T, 0)
        sin_layer(fw2, 64, h1T, h2T_f, 1)
        dbg("h2f", h2T_f[0:64, :], h2T_f)
        Pall = A.alloc("Pall", 128, [2, NT, 512], BF16)
        Mall = A.alloc("Mall", 128, [2, NT, 512], BF16)
        dec = [A.alloc("dec%d" % i, 128, [2, 512], F32) for i in range(2)]
        ta_l = [A.alloc("ta%d" % i, 128, [512], F32) for i in range(2)]
        tb_l = [A.alloc("tb%d" % i, 128, [512], F32) for i in range(2)]
        for tt in range(NT):
            dc = dec[tt % 2]
            P.dma("sync", dc[:, 0, :], decay_d[tt * 128:(tt + 1) * 128, :], writes=[dc])
            P.dma("sync", dc[:, 1, :], decayb_d[tt * 128:(tt + 1) * 128, :], writes=[dc])
            for o in range(2):
                pf = psb[2 + (o * 2) % 4]
                pbk = psb[3 + (o * 2) % 4]
                ta, tb = ta_l[o], tb_l[o]
                mm(pf[:, :], h2T_f[0:64, tt * 128:(tt + 1) * 128], fw3[0:64, (o * 2) * 512:(o * 2 + 1) * 512], True, True, [h2T_f, fw3], [pf])
                mm(pbk[:, :], h2T_f[0:64, tt * 128:(tt + 1) * 128], fw3[0:64, (o * 2 + 1) * 512:(o * 2 + 2) * 512], True, True, [h2T_f, fw3], [pbk])
                P.op("vector", C("tensor_tensor", out=ta[:], in0=pf[:, :], in1=dc[:, 0, :], op=ALU.mult), reads=[pf, dc], writes=[ta])
                P.op("vector", C("tensor_tensor", out=tb[:], in0=pbk[:, :], in1=dc[:, 1, :], op=ALU.mult), reads=[pbk, dc], writes=[tb])
                P.op("gpsimd", C("tensor_tensor", out=Pall[:, o, tt, :], in0=ta[:], in1=tb[:], op=ALU.add), reads=[ta, tb], writes=[Pall])
                P.op("gpsimd", C("tensor_tensor", out=Mall[:, o, tt, :], in0=ta[:], in1=tb[:], op=ALU.subtract), reads=[ta, tb], writes=[Mall])
        ftab = [[A.alloc("ftab%d%d" % (i, j), 128, [NT, 128], BF16) for j in range(2)] for i in range(2)]
        hout = [[A.alloc("hout%d%d" % (i, j), 128, [512], BF16) for j in range(2)] for i in range(2)]
        it = 0
        for fch in range(16):
            ct, stb = ftab[fch % 2]
            P.dma("sync", ct[:], cft_d[fch].rearrange("p (a b) -> p a b", a=NT), writes=[ct])
            P.dma("sync", stb[:], sft_d[fch].rearrange("p (a b) -> p a b", a=NT), writes=[stb])
            for o in range(2):
                pr = psb[(it * 2) % 8]
                pi_ = psb[(it * 2 + 1) % 8]
                hr_t, hs_t = hout[it % 2]
                it += 1
                for tc in range(NT):
                    mm(pr[:, :], ct[:, tc, :], Pall[:, o, tc, :], tc == 0, tc == NT - 1, [ct, Pall], [pr])
                for tc in range(NT):
                    mm(pi_[:, :], stb[:, tc, :], Mall[:, o, tc, :], tc == 0, tc == NT - 1, [stb, Mall], [pi_])
                evac(hr_t[:], pr[:, :], [pr], [hr_t])
                evac(hs_t[:], pi_[:, :], [pi_], [hs_t])
                P.dma("sync", hr_d[o, fch * 128:(fch + 1) * 128, :], hr_t[:], reads=[hr_t])
                P.dma("sync", hs_d[o, fch * 128:(fch + 1) * 128, :], hs_t[:], reads=[hs_t])
        phase_end(zT, fw1, fw2, fw3, fb, h1T, h2T_f, su, sk, skf, sfx, Pall, Mall, dec[0], dec[1], *ta_l, *tb_l,
                  ftab[0][0], ftab[0][1], ftab[1][0], ftab[1][1], hout[0][0], hout[0][1], hout[1][0], hout[1][1])
        if stop_after == 1:
            P.finish(out_ops)
            P.emit()
            return nc

        masks = A.alloc("masks", 128, [2, 128], BF16)
        esink = A.alloc("esink", 128, [8], F32)
        convp = A.alloc("convp", 128, [4, 12], F32)
        skipp = A.alloc("skipp", 128, [2, 4], F32)
        P.dma("sync", masks[:], masks_d.rearrange("p (a b) -> p a b", a=2), writes=[masks])
        P.dma("sync", esink[0:64, :], sink_d[0, :].partition_broadcast(64), writes=[esink])
        P.op("scalar", C("activation", out=esink[0:64, :], in_=esink[0:64, :], func=AF.Exp), reads=[esink], writes=[esink])
        P.dma("sync", convp[:], convp_d.rearrange("p (a b) -> p a b", a=4), writes=[convp])
        P.dma("sync", skipp[:], skip_d.rearrange("p (a b) -> p a b", a=2), writes=[skipp])
        kcT = [A.alloc("kcT%d" % b, 128, [256], BF16) for b in range(2)]
        vc = [A.alloc("vc%d" % b, 128, [2, 128], BF16) for b in range(2)]

        chk_n = [0]

        def chk(tag):
            if "chk" not in dbg_d:
                return
            i = chk_n[0]
            chk_n[0] += 1
            tb_ = A.alloc("chk%d" % i, 128, [256], F32)
            P.op("vector", C("tensor_copy", out=tb_[:], in_=kcT[0][:]), reads=[kcT[0]], writes=[tb_])
            out_ops.append(P.dma("sync", dbg_d["chk"][i], tb_[:], reads=[tb_]))
            print("chk", i, tag)

        def wchunk_load(buf, col0, ncols=128):
            P.dma("gpsimd", buf[:], win_d[:, col0:col0 + ncols].rearrange("(k p) n -> p k n", p=128), writes=[buf])

        rows = [A.alloc("row%d" % i, 128, [D], F32) for i in range(2)]
        xin = [A.alloc("xin%d" % i, 128, [D], F32) for i in range(2)]
        xn = A.alloc("xn", 128, [D], F32)
        xb = A.alloc("xb", 128, [D], BF16)
        hcT = A.alloc("hcT", 128, [KC, 256], BF16)
        wk = A.alloc("wk", 128, [KC, 128], BF16)
        wv = A.alloc("wv", 128, [KC, 128], BF16)
        wchunk_load(wk, 1024)
        wchunk_load(wv, 1280)
        load_row(rows[0], modrows_d[2, D:2 * D], reads=[modrows], plus1=True)
        load_row(rows[1], modrows_d[2, 0:D], reads=[modrows])
        for b in range(2):
            for tl in range(2):
                xi = xin[tl % 2]
                P.dma("sync", xi[:], ctx_d[b, tl * 128:(tl + 1) * 128, :], writes=[xi])
                layer_norm(xi, xn)
                modulate(xn, rows[0], rows[1])
                P.op("vector", C("tensor_copy", out=xb[:], in_=xn[:]), reads=[xn], writes=[xb])
                transpose_to(hcT[:, :, tl * 128:(tl + 1) * 128], xb, psb[tl % 2], [], [hcT])
            pk = psb[2 + b]
            for kc in range(KC):
                mm(pk[:, 0:256], wk[:, kc, :], hcT[:, kc, :], kc == 0, kc == KC - 1, [wk, hcT], [pk])
            evac(kcT[b][:], pk[:, 0:256], [pk], [kcT[b]])
            for tl in range(2):
                pvv = psb[4 + tl]
                for kc in range(KC):
                    mm(pvv[:, 0:128], hcT[:, kc, tl * 128:(tl + 1) * 128], wv[:, kc, :], kc == 0, kc == KC - 1, [hcT, wv], [pvv])
                evac(vc[b][:, tl, :], pvv[:, 0:128], [pvv], [vc[b]])
        if "a_xn" in dbg_d:
            dbg("a_xn", xn[:], xn)
            dbg("a_row0", rows[0][:], rows[0])
            tmpb = A.alloc("dbgA", 128, [2048], F32)
            P.op("vector", C("tensor_copy", out=tmpb[:], in_=hcT[:].rearrange("p a b -> p (a b)")), reads=[hcT], writes=[tmpb])
            dbg("a_hcT", tmpb[:], tmpb)
            tmpb2 = A.alloc("dbgA2", 128, [1024], F32)
            P.op("vector", C("tensor_copy", out=tmpb2[:], in_=wk[:].rearrange("p a b -> p (a b)")), reads=[wk], writes=[tmpb2])
            dbg("a_wk", tmpb2[:], tmpb2)
            tmpb3 = A.alloc("dbgA3", 128, [512], F32)
            P.op("vector", C("tensor_copy", out=tmpb3[:, 0:256], in_=kcT[0][:]), reads=[kcT[0]], writes=[tmpb3])
            P.op("vector", C("tensor_copy", out=tmpb3[:, 256:512], in_=kcT[1][:]), reads=[kcT[1]], writes=[tmpb3])
            dbg("a_kcT", tmpb3[:], tmpb3)
            tmpb4 = A.alloc("dbgA4", 128, [512], F32)
            P.op("vector", C("tensor_copy", out=tmpb4[:], in_=psb[2][:, :]), reads=[psb[2]], writes=[tmpb4])
            dbg("a_pk", tmpb4[:], tmpb4)
        chk("end of A before phase_end")
        phase_end(hcT, wk, xin[0], xin[1], xn, xb, rows[0], rows[1])
        chk("after A phase_end")
        if stop_after == 2:
            P.finish(out_ops)
            P.emit()
            return nc

        gates = A.alloc("gates", 128, [NTT, NE], F32)
        maskall = A.alloc("maskall", 128, [NTT, NE], F32)
        for b in range(2):
            hxT = A.alloc("hxT", 128, [KC, T], BF16)
            rows = [A.alloc("row%d" % i, 128, [D], F32) for i in range(5)]
            xin = [A.alloc("xin%d" % i, 128, [D], F32) for i in range(4)]
            xn_l = [A.alloc("xn%d" % i, 128, [D], F32) for i in range(2)]
            xb_l = [A.alloc("xb%d" % i, 128, [D], BF16) for i in range(2)]
            if b == 0:
                chk("B after allocs")
            load_row(rows[0], modrows_d[b, D:2 * D], reads=[modrows], plus1=True)
            load_row(rows[1], modrows_d[b, 0:D], reads=[modrows])
            if b == 0:
                chk("B after load_rows")
            def tileB(tl):
                xi = xin[tl % 4]
                P.dma("sync", xi[:], x_d[b, tl * 128:(tl + 1) * 128, :], writes=[xi])
                xn, xb = xn_l[tl % 2], xb_l[tl % 2]
                yield from layer_norm_g(xi, xn)
                yield from modulate_g(xn, rows[0], rows[1])
                P.op("vector", C("tensor_copy", out=xb[:], in_=xn[:]), reads=[xn], writes=[xb])
                yield
                transpose_to(hxT[:, :, tl * 128:(tl + 1) * 128], xb, psb[tl % 2], [], [hxT])

            for tl in range(0, NT, 2):
                run_interleaved([tileB(tl), tileB(tl + 1)])
            if b == 0:
                chk("after B loop")
            if stop_after == 2.5:
                P.finish(out_ops)
                P.emit()
                return nc
            phase_end(*xin, *xn_l, *xb_l, *rows)
            if b == 0:
                chk("after B phase_end")
            QT = A.alloc("QT", 128, [4, T], BF16)
            KT = A.alloc("KT", 128, [T], BF16)
            Vt = A.alloc("Vt", 128, [NT, 128], BF16)
            sTm = A.alloc("sTm", 128, [12, T], BF16)
            ropeC = A.alloc("ropeC", 128, [T], F32)
            ropeS = A.alloc("ropeS", 128, [T], F32)
            wq = [A.alloc("wq%d" % i, 128, [KC, 128], BF16) for i in range(4)]
            r1_l = [A.alloc("r1%d" % i, 128, [512], F32) for i in range(2)]
            r2_l = [A.alloc("r2%d" % i, 128, [512], F32) for i in range(2)]
            U = [A.alloc("U%d" % i, 128, [T + 2], F32) for i in range(2)]
            cs = A.alloc("cs", 128, [T], F32)
            P.dma("sync", ropeC[:], ropeC_d[:, :], writes=[ropeC])
            P.dma("sync", ropeS[:], ropeS_d[:, :], writes=[ropeS])
            for i in range(2):
                P.op("gpsimd", C("memset", U[i][:, 0:1], 0.0), writes=[U[i]])
                P.op("gpsimd", C("memset", U[i][:, T + 1:T + 2], 0.0), writes=[U[i]])
            pc = 0
            for c in range(5):
                wa, wb_ = wq[(2 * c) % 4], wq[(2 * c + 1) % 4]
                if c < 4:
                    wchunk_load(wa, c * 128)
                    wchunk_load(wb_, 512 + c * 128)
                else:
                    wchunk_load(wa, 1024)
                    wchunk_load(wb_, 1152)
                for tch in range(4):
                    pa = psb[pc % 8]
                    pb = psb[(pc + 1) % 8]
                    pc += 2
                    r1, r2 = r1_l[(pc // 2) % 2], r2_l[(pc // 2) % 2]
                    ts_ = slice(tch * 512, (tch + 1) * 512)
                    for kc in range(KC):
                        mm(pa[:, :], wa[:, kc, :], hxT[:, kc, ts_], kc == 0, kc == KC - 1, [wa, hxT], [pa])
                    for kc in range(KC):
                        mm(pb[:, :], wb_[:, kc, :], hxT[:, kc, ts_], kc == 0, kc == KC - 1, [wb_, hxT], [pb])
                    P.op("vector", C("tensor_tensor", out=r1[:], in0=pa[:, :], in1=ropeC[:, ts_], op=ALU.mult), reads=[pa, ropeC], writes=[r1])
                    P.op("vector", C("tensor_tensor", out=r2[:], in0=pb[:, :], in1=ropeS[:, ts_], op=ALU.mult), reads=[pb, ropeS], writes=[r2])
                    if c < 4:
                        P.op("vector", C("tensor_tensor", out=QT[:, c, ts_], in0=r1[:], in1=r2[:], op=ALU.add), reads=[r1, r2], writes=[QT])
                    else:
                        P.op("vector", C("tensor_tensor", out=KT[:, ts_], in0=r1[:], in1=r2[:], op=ALU.add), reads=[r1, r2], writes=[KT])
            if b == 0:
                chk("after qk")
            for tl in range(NT):
                pvv = psb[pc % 8]
                pc += 1
                for kc in range(KC):
                    mm(pvv[:, 0:128], hxT[:, kc, tl * 128:(tl + 1) * 128], wv[:, kc, :], kc == 0, kc == KC - 1, [hxT, wv], [pvv])
                evac(Vt[:, tl, :], pvv[:, 0:128], [pvv], [Vt])
            if b == 0:
                chk("after V")
            for j in range(12):
                wj = wq[j % 4]
                wchunk_load(wj, HY_BASE + j * 128)
                Uj = U[j % 2]
                for tch in range(4):
                    pu = psb[pc % 8]
                    pc += 1
                    for kc in range(KC):
                        mm(pu[:, :], wj[:, kc, :], hxT[:, kc, tch * 512:(tch + 1) * 512], kc == 0, kc == KC - 1, [wj, hxT], [pu])
                    evac(Uj[:, 1 + tch * 512:1 + (tch + 1) * 512], pu[:, :], [pu], [Uj])
                P.op("vector", C("tensor_scalar", out=cs[:], in0=Uj[:, 1:T + 1], scalar1=convp[:, 1, j:j + 1], scalar2=convp[:, 3, j:j + 1],
                                                                  op0=ALU.mult, op1=ALU.add), reads=[Uj, convp], writes=[cs])
                P.op("vector", C("scalar_tensor_tensor", out=cs[:], in0=Uj[:, 0:T], scalar=convp[:, 0, j:j + 1], in1=cs[:],
                                                                         op0=ALU.mult, op1=ALU.add), reads=[Uj, convp, cs], writes=[cs])
                P.op("vector", C("scalar_tensor_tensor", out=sTm[:, j, :], in0=Uj[:, 2:T + 2], scalar=convp[:, 2, j:j + 1], in1=cs[:],
                                                                         op0=ALU.mult, op1=ALU.add), reads=[Uj, convp, cs], writes=[sTm])
            if b == 0:
                chk("after hy")
            if "qT" in dbg_d and b == 0:
                for c in range(4):
                    tmpb = cs
                    P.op("vector", C("tensor_copy", out=tmpb[:], in_=QT[:, c, :]), reads=[QT], writes=[tmpb])
                    dbg("qT", tmpb[:], tmpb, dst=dbg_d["qT"][c])
            if "sT" in dbg_d and b == 0:
                for c in range(12):
                    tmpb = cs
                    P.op("vector", C("tensor_copy", out=tmpb[:], in_=sTm[:, c, :]), reads=[sTm], writes=[tmpb])
                    dbg("sT", tmpb[:], tmpb, dst=dbg_d["sT"][c])
            if "kchk" in dbg_d and b == 0:
                P.op("vector", C("tensor_copy", out=cs[:, 0:256], in_=kcT[0][:]), reads=[kcT[0]], writes=[cs])
                P.op("vector", C("tensor_copy", out=cs[:, 256:512], in_=vc[0][:].rearrange("p a b -> p (a b)")), reads=[vc[0]], writes=[cs])
                dbg("kchk", cs[:, 0:512], cs)
            P.dma("sync", hxs_d[:, :], hxT[:].rearrange("p k t -> p (k t)"), reads=[hxT])
            phase_end(ropeC, ropeS, wq[0], wq[1], wq[2], wq[3], *r1_l, *r2_l, U[0], U[1], cs, hxT)
            if stop_after == 3:
                P.finish(out_ops)
                P.emit()
                return nc

            attT = A.alloc("attT", 128, [8, T], BF16)
            PT = [A.alloc("PT%d" % i, 128, [4, 128], BF16) for i in range(3)]
            dn = A.alloc("dn", 128, [4, 128], F32)
            pidx = 0
            for qi in range(NT):
                for g in range(2):
                    gp = slice(g * 64, (g + 1) * 64)
                    keys = []
                    for j in (qi - 1, qi, qi + 1):
                        if 0 <= j < NT:
                            keys.append(("l", j))
                    keys += [("c", 0), ("c", 1)]
                    po = psb[4 + (qi * 2 + g) % 2]
                    pd = psb[6 + (qi * 2 + g) % 2]
                    for ki, (kind, j) in enumerate(keys):
                        pst = psb[pidx % 4]
                        ptb = PT[pidx % 3]
                        pidx += 1
                        if kind == "l":
                            lhs = KT[gp, j * 128:(j + 1) * 128]
                            lr = KT
                            vv = Vt[:, j, g * 64:(g + 1) * 64]
                            vr = Vt
                        else:
                            lhs = kcT[b][gp, j * 128:(j + 1) * 128]
                            lr = kcT[b]
                            vv = vc[b][:, j, g * 64:(g + 1) * 64]
                            vr = vc[b]
                        mm(pst[:, :], lhs, QT[gp, :, qi * 128:(qi + 1) * 128], True, True, [lr, QT], [pst])
                        P.op("scalar", C("activation", out=ptb[:], in_=pst[:, :].rearrange("p (h q) -> p h q", h=4), func=AF.Exp, scale=0.125),
                             reads=[pst], writes=[ptb])
                        if kind == "l" and j != qi:
                            mi = 0 if j < qi else 1
                            P.op("gpsimd", C("tensor_tensor", out=ptb[:], in0=ptb[:], in1=masks[:, mi:mi + 1, :].to_broadcast([128, 4, 128]), op=ALU.mult),
                                 reads=[ptb, masks], writes=[ptb])
                        mm(po[0:64, :], vv, ptb[:].rearrange("p h q -> p (h q)"), ki == 0, ki == len(keys) - 1, [vr, ptb], [po])
                        mm(pd[0:64, :], ones_b[:, 0:64], ptb[:].rearrange("p h q -> p (h q)"), ki == 0, ki == len(keys) - 1, [ones_b, ptb], [pd])
                    P.op("vector", C("tensor_tensor", out=dn[0:64], in0=pd[0:64, :].rearrange("p (h q) -> p h q", h=4),
                                                                          in1=esink[0:64, g * 4:(g + 1) * 4].unsqueeze(2).to_broadcast([64, 4, 128]), op=ALU.add),
                         reads=[pd, esink], writes=[dn])
                    P.op("vector", C("reciprocal", out=dn[0:64], in_=dn[0:64]), reads=[dn], writes=[dn])
                    P.op("vector", C("tensor_tensor", out=attT[0:64, g * 4:(g + 1) * 4, qi * 128:(qi + 1) * 128],
                                                                                 in0=po[0:64, :].rearrange("p (h q) -> p h q", h=4), in1=dn[0:64], op=ALU.mult),
                         reads=[po, dn], writes=[attT])
            if "attT" in dbg_d and b == 0:
                tmpb = A.alloc("dbga", 128, [T], F32)
                dbg("esink", esink[0:64, :], esink)
                P.op("vector", C("tensor_copy", out=tmpb[:, 0:256], in_=kcT[0][:]), reads=[kcT[0]], writes=[tmpb])
                dbg("kcT", tmpb[:, 0:256], tmpb)
                P.op("vector", C("tensor_copy", out=tmpb[:, 0:256], in_=vc[0][:].rearrange("p a b -> p (a b)")), reads=[vc[0]], writes=[tmpb])
                dbg("vc", tmpb[:, 0:256], tmpb)
                P.op("vector", C("tensor_copy", out=tmpb[:, 0:512], in_=PT[0][:].rearrange("p a b -> p (a b)")), reads=[PT[0]], writes=[tmpb])
                dbg("pt", tmpb[:, 0:512], tmpb)
                dbg("dn", dn[0:64].rearrange("p a b -> p (a b)"), dn)
                for c in range(8):
                    P.op("vector", C("tensor_copy", out=tmpb[0:64, :], in_=attT[0:64, c, :]), reads=[attT], writes=[tmpb])
                    dbg("attT", tmpb[0:64, :], tmpb, dst=dbg_d["attT"][c])
            P.dma("sync", atts_d[:, :], attT[0:64].rearrange("p k t -> p (k t)"), reads=[attT])
            phase_end(QT, KT, Vt, PT[0], PT[1], PT[2], dn, attT)
            if stop_after == 4:
                P.finish(out_ops)
                P.emit()
                return nc

            ztok = A.alloc("ztok", 128, [NT, 512], BF16)
            z1T = A.alloc("z1T", 128, [4, T], BF16)
            Yr = A.alloc("Yr", 128, [NT, 512], BF16)
            Ys = A.alloc("Ys", 128, [NT, 512], BF16)
            ftab = [[A.alloc("ftab%d%d" % (i, j), 128, [NT, 128], BF16) for j in range(2)] for i in range(2)]
            hin = [[A.alloc("hin%d%d" % (i, j), 128, [512], BF16) for j in range(2)] for i in range(2)]
            itab = [ztok, A.alloc("itab1", 128, [NT, 512], BF16)]
            hyT = A.alloc("hyT", 128, [4, T], BF16)
            e1_l = [A.alloc("e1%d" % i, 128, [512], F32) for i in range(2)]
            e2_l = [A.alloc("e2%d" % i, 128, [512], F32) for i in range(2)]
            e3_l = [A.alloc("e3%d" % i, 128, [512], F32) for i in range(2)]
            e4_l = [A.alloc("e4%d" % i, 128, [512], F32) for i in range(2)]
            for o in range(2):
                for tt in range(NT):
                    pz = psb[tt % 2]
                    pzv = pz.ap.bitcast(BF16)
                    for cc in range(4):
                        if o == 0:
                            src_ap, src_b = sTm[:, 8 + cc, tt * 128:(tt + 1) * 128], sTm
                        else:
                            src_ap, src_b = z1T[:, cc, tt * 128:(tt + 1) * 128], z1T
                        P.op("tensor", C("transpose", out=pzv[:, cc * 128:(cc + 1) * 128], in_=src_ap, identity=ident_b[:]),
                             reads=[src_b, ident_b], writes=[pz])
                    evac(ztok[:, tt, :], pzv[:, 0:512], [pz], [ztok])
                for fch in range(16):
                    ct, stb = ftab[fch % 2]
                    hr_t, hs_t = hin[fch % 2]
                    e1, e2, e3, e4 = e1_l[fch % 2], e2_l[fch % 2], e3_l[fch % 2], e4_l[fch % 2]
                    P.dma("sync", ct[:], cft_d[fch].rearrange("p (a b) -> p a b", a=NT), writes=[ct])
                    P.dma("sync", stb[:], sft_d[fch].rearrange("p (a b) -> p a b", a=NT), writes=[stb])
                    P.dma("sync", hr_t[:], hr_d[o, fch * 128:(fch + 1) * 128, :], reads=[hspec], writes=[hr_t])
                    P.dma("sync", hs_t[:], hs_d[o, fch * 128:(fch + 1) * 128, :], reads=[hspec], writes=[hs_t])
                    pr = psb[2 + (fch % 2) * 2]
                    pi_ = psb[3 + (fch % 2) * 2]
                    for tc in range(NT):
                        mm(pr[:, :], ct[:, tc, :], ztok[:, tc, :], tc == 0, tc == NT - 1, [ct, ztok], [pr])
                    for tc in range(NT):
                        mm(pi_[:, :], stb[:, tc, :], ztok[:, tc, :], tc == 0, tc == NT - 1, [stb, ztok], [pi_])
                    P.op("vector", C("tensor_tensor", out=e1[:], in0=pr[:, :], in1=hr_t[:], op=ALU.mult), reads=[pr, hr_t], writes=[e1])
                    P.op("vector", C("tensor_tensor", out=e2[:], in0=pi_[:, :], in1=hs_t[:], op=ALU.mult), reads=[pi_, hs_t], writes=[e2])
                    P.op("vector", C("tensor_tensor", out=Yr[:, fch, :], in0=e1[:], in1=e2[:], op=ALU.subtract), reads=[e1, e2], writes=[Yr])
                    P.op("vector", C("tensor_tensor", out=e3[:], in0=pr[:, :], in1=hs_t[:], op=ALU.mult), reads=[pr, hs_t], writes=[e3])
                    P.op("vector", C("tensor_tensor", out=e4[:], in0=pi_[:, :], in1=hr_t[:], op=ALU.mult), reads=[pi_, hr_t], writes=[e4])
                    P.op("vector", C("tensor_tensor", out=Ys[:, fch, :], in0=e3[:], in1=e4[:], op=ALU.add), reads=[e3, e4], writes=[Ys])
                for tch in range(4):
                    ci_, si_ = itab
                    P.dma("sync", ci_[:], cit_d[tch].rearrange("p (a b) -> p a b", a=NT), writes=[ci_])
                    P.dma("gpsimd", si_[:], sit_d[tch].rearrange("p (a b) -> p a b", a=NT), writes=[si_])
                    ts_ = slice(tch * 512, (tch + 1) * 512)
                    for cc in range(4):
                        pcv = psb[6 + cc % 2]
                        e1 = e1_l[cc % 2]
                        for fc in range(NT):
                            mm(pcv[:, :], Yr[:, fc, cc * 128:(cc + 1) * 128], ci_[:, fc, :], fc == 0, False, [Yr, ci_], [pcv])
                        for fc in range(NT):
                            mm(pcv[:, :], Ys[:, fc, cc * 128:(cc + 1) * 128], si_[:, fc, :], False, fc == NT - 1, [Ys, si_], [pcv])
                        if o == 0:
                            zin_ap, zin_b = sTm[:, 8 + cc, ts_], sTm
                            xo_ap = sTm[:, 0 + cc, ts_]
                            zo_ap, zo_b = z1T[:, cc, ts_], z1T
                        else:
                            zin_ap, zin_b = z1T[:, cc, ts_], z1T
                            xo_ap = sTm[:, 4 + cc, ts_]
                            zo_ap, zo_b = hyT[:, cc, ts_], hyT
                        P.op("vector", C("scalar_tensor_tensor", out=e1[:], in0=zin_ap, scalar=skipp[:, o, cc:cc + 1], in1=pcv[:, :],
                                                                                                          op0=ALU.mult, op1=ALU.add), reads=[pcv, zin_b, skipp], writes=[e1])
                        P.op("vector", C("tensor_tensor", out=zo_ap, in0=e1[:], in1=xo_ap, op=ALU.mult), reads=[e1, sTm], writes=[zo_b])
            if "hyT" in dbg_d and b == 0:
                tmpb = A.alloc("dbgh", 128, [T], F32)
                for c in range(4):
                    P.op("vector", C("tensor_copy", out=tmpb[:], in_=hyT[:, c, :]), reads=[hyT], writes=[tmpb])
                    dbg("hyT", tmpb[:], tmpb, dst=dbg_d["hyT"][c])
            phase_end(ztok, z1T, Yr, Ys, ftab[0][0], ftab[0][1], ftab[1][0], ftab[1][1], hin[0][0], hin[0][1], hin[1][0], hin[1][1],
                      itab[1], *e1_l, *e2_l, *e3_l, *e4_l, sTm)
            if stop_after == 5:
                P.finish(out_ops)
                P.emit()
                return nc

            hxT = A.alloc("hxT", 128, [KC, T], BF16)
            attT = A.alloc("attT", 128, [8, T], BF16)
            P.dma("sync", hxT[:].rearrange("p k t -> p (k t)"), hxs_d[:, :], writes=[hxT])
            P.dma("sync", attT[0:64].rearrange("p k t -> p (k t)"), atts_d[:, :], writes=[attT])
            wba = A.alloc("wba", 128, [8, D], BF16)
            wbh = A.alloc("wbh", 128, [4, D], BF16)
            mT = A.alloc("mT", 128, [KC, T], BF16)
            wg = [A.alloc("wg%d" % i, 128, [KC, 128], BF16) for i in range(4)]
            ga_l = [A.alloc("ga%d" % i, 128, [512], F32) for i in range(2)]
            gh_l = [A.alloc("gh%d" % i, 128, [512], F32) for i in range(2)]
            m1_l = [A.alloc("m1%d" % i, 128, [512], F32) for i in range(2)]
            m2_l = [A.alloc("m2%d" % i, 128, [512], F32) for i in range(2)]
            P.dma("gpsimd", wba[0:64, :, :], wba_d.rearrange("(h d) n -> d h n", d=64), writes=[wba])
            P.dma("gpsimd", wbh[:], wbh_d.rearrange("(c p) n -> p c n", p=128), writes=[wbh])
            pc = 0
            for nch in range(8):
                wga, wgh = wg[(2 * nch) % 4], wg[(2 * nch + 1) % 4]
                wchunk_load(wga, GATE_BASE + nch * 128)
                wchunk_load(wgh, GATE_BASE + 1024 + nch * 128)
                ns = slice(nch * 128, (nch + 1) * 128)
                for tch in range(4):
                    ts_ = slice(tch * 512, (tch + 1) * 512)
                    p1, p2, p3, p4 = [psb[(pc + i) % 8] for i in range(4)]
                    pc += 4
                    ga, gh, m1, m2 = ga_l[(pc // 4) % 2], gh_l[(pc // 4) % 2], m1_l[(pc // 4) % 2], m2_l[(pc // 4) % 2]
                    for h in range(8):
                        mm(p1[:, :], wba[0:64, h, ns], attT[0:64, h, ts_], h == 0, h == 7, [wba, attT], [p1])
                    for cc in range(4):
                        mm(p2[:, :], wbh[:, cc, ns], hyT[:, cc, ts_], cc == 0, cc == 3, [wbh, hyT], [p2])
                    for kc in range(KC):
                        mm(p3[:, :], wga[:, kc, :], hxT[:, kc, ts_], kc == 0, kc == KC - 1, [wga, hxT], [p3])
                    for kc in range(KC):
                        mm(p4[:, :], wgh[:, kc, :], hxT[:, kc, ts_], kc == 0, kc == KC - 1, [wgh, hxT], [p4])
                    P.op("scalar", C("activation", out=ga[:], in_=p3[:, :], func=AF.Sigmoid), reads=[p3], writes=[ga])
                    P.op("scalar", C("activation", out=gh[:], in_=p4[:, :], func=AF.Sigmoid), reads=[p4], writes=[gh])
                    P.op("vector", C("tensor_tensor", out=m1[:], in0=p1[:, :], in1=ga[:], op=ALU.mult), reads=[p1, ga], writes=[m1])
                    P.op("vector", C("tensor_tensor", out=m2[:], in0=p2[:, :], in1=gh[:], op=ALU.mult), reads=[p2, gh], writes=[m2])
                    P.op("vector", C("tensor_tensor", out=mT[:, nch, ts_], in0=m1[:], in1=m2[:], op=ALU.add), reads=[m1, m2], writes=[mT])
            phase_end(hxT, attT, hyT, wba, wbh, wg[0], wg[1], wg[2], wg[3], *ga_l, *gh_l, *m1_l, *m2_l)
            h2Tt = [A.alloc("h2Tt%d" % i, 128, [KC, 128], BF16) for i in range(2)]
            wo = A.alloc("wo", 128, [KC, D], BF16)
            P.dma("gpsimd", wo[:], wo_d.rearrange("(c p) n -> p c n", p=128), writes=[wo])
            rows = [A.alloc("row%d" % i, 128, [D], F32) for i in range(5)]
            xin = [A.alloc("xin%d" % i, 128, [D], F32) for i in range(4)]
            xn_l = [A.alloc("xn%d" % i, 128, [D], F32) for i in range(2)]
            xb_l = [A.alloc("xb%d" % i, 128, [D], BF16) for i in range(2)]
            xm_l = [A.alloc("xm%d" % i, 128, [D], F32) for i in range(2)]
            lg_l = [A.alloc("lg%d" % i, 128, [NE], F32) for i in range(2)]
            m8_l = [A.alloc("m8%d" % i, 128, [8], F32) for i in range(2)]
            msk_l = [A.alloc("msk%d" % i, 128, [NE], F32) for i in range(2)]
            ssum_l = [A.alloc("ssum%d" % i, 128, [1], F32) for i in range(2)]
            rw = A.alloc("rw", 128, [KC, NE], BF16)
            rbrow = A.alloc("rbrow", 128, [NE], F32)
            P.dma("gpsimd", rw[:], rw_d.rearrange("(c p) n -> p c n", p=128), writes=[rw])
            P.dma("sync", rbrow[:], rb_d[0, :].partition_broadcast(128), writes=[rbrow])
            load_row(rows[0], modrows_d[b, 2 * D:3 * D], reads=[modrows])
            load_row(rows[1], ln_d[0, :])
            load_row(rows[2], ln_d[1, :])
            load_row(rows[3], modrows_d[b, 4 * D:5 * D], reads=[modrows], plus1=True)
            load_row(rows[4], modrows_d[b, 3 * D:4 * D], reads=[modrows])
            def tileF2(tl):
                tsl = slice(tl * 128, (tl + 1) * 128)
                xn, xb, xm = xn_l[tl % 2], xb_l[tl % 2], xm_l[tl % 2]
                lg, m8, msk, ssum = lg_l[tl % 2], m8_l[tl % 2], msk_l[tl % 2], ssum_l[tl % 2]
                py = [psb[(tl % 2) * 2], psb[(tl % 2) * 2 + 1]]
                for n2 in range(2):
                    for kc in range(KC):
                        mm(py[n2][:, :], mT[:, kc, tsl], wo[:, kc, n2 * 512:(n2 + 1) * 512], kc == 0, kc == KC - 1, [mT, wo], [py[n2]])
                xi = xin[tl % 4]
                P.dma("sync", xi[:], x_d[b, tsl, :], writes=[xi])
                for n2 in range(2):
                    P.op("vector", C("tensor_tensor", out=xn[:, n2 * 512:(n2 + 1) * 512], in0=py[n2][:, :], in1=rows[0][:, n2 * 512:(n2 + 1) * 512], op=ALU.mult),
                         reads=[py[n2], rows[0]], writes=[xn])
                P.op("vector", C("scalar_tensor_tensor", out=xn[:], in0=xi[:], scalar=ALPHA, in1=xn[:], op0=ALU.mult, op1=ALU.add), reads=[xi, xn], writes=[xn])
                yield
                yield from layer_norm_g(xn, xm)
                yield from modulate_g(xm, rows[1], rows[2])
                P.dma("sync", xmid_d[b * T + tl * 128:b * T + (tl + 1) * 128, :], xm[:], reads=[xm])
                yield from layer_norm_g(xm, xn)
                yield from modulate_g(xn, rows[3], rows[4])
                P.op("vector", C("tensor_copy", out=xb[:], in_=xn[:]), reads=[xn], writes=[xb])
                yield
                P.dma("sync", h2d_d[b * T + tl * 128:b * T + (tl + 1) * 128, :], xb[:], reads=[xb])
                h2t = h2Tt[tl % 2]
                transpose_to(h2t[:], xb, psb[4 + tl % 2], [], [h2t])
                pl = psb[6 + tl % 2]
                for kc in range(KC):
                    mm(pl[:, 0:NE], h2t[:, kc, :], rw[:, kc, :], kc == 0, kc == KC - 1, [h2t, rw], [pl])
                P.op("vector", C("tensor_tensor", out=lg[:], in0=pl[:, 0:NE], in1=rbrow[:], op=ALU.add), reads=[pl, rbrow], writes=[lg])
                yield
                P.op("vector", C("max", out=m8[:], in_=lg[:]), reads=[lg], writes=[m8])
                yield
                P.op("vector", C("tensor_scalar", out=msk[:], in0=lg[:], scalar1=m8[:, 3:4], scalar2=None, op0=ALU.is_ge), reads=[lg, m8], writes=[msk])
                yield
                P.op("gpsimd", C("tensor_copy", out=maskall[:, b * NT + tl, :], in_=msk[:]), reads=[msk], writes=[maskall])
                yield
                P.op("vector", C("tensor_scalar", out=lg[:], in0=lg[:], scalar1=m8[:, 0:1], scalar2=None, op0=ALU.subtract), reads=[lg, m8], writes=[lg])
                yield
                P.op("scalar", C("activation", out=lg[:], in_=lg[:], func=AF.Exp), reads=[lg], writes=[lg])
                yield
                P.op("vector", C("tensor_tensor", out=lg[:], in0=lg[:], in1=msk[:], op=ALU.mult), reads=[lg, msk], writes=[lg])
                yield
                P.op("vector", C("reduce_sum", out=ssum[:], in_=lg[:], axis=mybir.AxisListType.X), reads=[lg], writes=[ssum])
                yield
                P.op("vector", C("reciprocal", out=ssum[:], in_=ssum[:]), reads=[ssum], writes=[ssum])
                yield
                P.op("vector", C("tensor_scalar", out=gates[:, b * NT + tl, :], in0=lg[:], scalar1=ssum[:, 0:1], scalar2=None, op0=ALU.mult), reads=[lg, ssum], writes=[gates])
                yield

            for tl in range(0, NT, 2):
                run_interleaved([tileF2(tl), tileF2(tl + 1)])
            if "gates" in dbg_d and b == 0:
                dbg("gates", gates[:], gates)
            phase_end(mT, wo, rw, rbrow, *xm_l, *lg_l, *m8_l, *msk_l, *ssum_l, *xin, *xn_l, *xb_l, *rows, *h2Tt)
            if stop_after == 6:
                P.finish(out_ops)
                P.emit()
                return nc

        ltri = A.alloc("ltri", 128, [128], F32)
        kk = A.alloc("kk", 128, [NBLK, NE], F32)
        tokid = A.alloc("tokid", 128, [NTT], F32)
        j4 = A.alloc("j4", 128, [NTT, 4], F32)
        Srun = A.alloc("Srun", 128, [NE], F32)
        rankall = A.alloc("rankall", 128, [NTT, NE], F32)
        cnt = A.alloc("cnt", 128, [NE], F32)
        ci = A.alloc("ci", 128, [NE], I32)
        cf = A.alloc("cf", 128, [NE], F32)
        cfx = A.alloc("cfx", 128, [NE], F32)
        padded = A.alloc("padded", 128, [NE], F32)
        cs_a = A.alloc("cs_a", 128, [NE], F32)
        cs_b = A.alloc("cs_b", 128, [NE], F32)
        pstart = A.alloc("pstart", 128, [NE], F32)
        destm = A.alloc("destm", 128, [NTT, NE], F32)
        d8 = A.alloc("d8", 128, [NTT, 8], F32)
        didx = A.alloc("didx", 128, [NTT, 4], I32)
        eqt = A.alloc("eqt", 128, [NTT, NE], F32)
        pay = A.alloc("pay", 128, [NTT, 4, 4], F32)
        cmpk = A.alloc("cmpk", 128, [NBLK, NE], F32)
        bexp = A.alloc("bexp", 128, [NBLK], F32)
        bexp1024 = A.alloc("bexp1024", 128, [NBLK], F32)
        P.dma("sync", ltri[:], ltri_d[:, :], writes=[ltri])
        P.dma("sync", kk[:], kk512_d.rearrange("p (k e) -> p k e", k=NBLK), writes=[kk])
        P.dma("sync", tokid[:], tokid_d[:, :], writes=[tokid])
        P.dma("sync", j4[:], j4_d.rearrange("p (a b) -> p a b", a=NTT), writes=[j4])
        P.dma("sync", slot_d[:, :], slotinit_d[:, :], writes=[slottab])
        P.op("vector", C("memset", Srun[:], 0.0), writes=[Srun])
        for tl in range(NTT):
            pr_ = psb[tl % 2]
            mm(pr_[:, 0:NE], ones_sq[:], Srun[:], True, False, [ones_sq, Srun], [pr_])
            mm(pr_[:, 0:NE], ltri[:], maskall[:, tl, :], False, True, [ltri, maskall], [pr_])
            evac(rankall[:, tl, :], pr_[:, 0:NE], [pr_], [rankall], eng="scalar")
            P.op("vector", C("tensor_tensor", out=Srun[:], in0=Srun[:], in1=maskall[:, tl, :], op=ALU.add), reads=[Srun, maskall], writes=[Srun])
        pcn = psb[2]
        mm(pcn[:, 0:NE], ones_sq[:], Srun[:], True, True, [ones_sq, Srun], [pcn])
        P.op("vector", C("tensor_copy", out=cnt[:], in_=pcn[:, 0:NE]), reads=[pcn], writes=[cnt])
        P.op("vector", C("tensor_scalar", out=cf[:], in0=cnt[:], scalar1=float(BLK - 1), scalar2=1.0 / BLK, op0=ALU.add, op1=ALU.mult), reads=[cnt], writes=[cf])
        P.op("vector", C("tensor_copy", out=ci[:], in_=cf[:]), reads=[cf], writes=[ci])
        P.op("vector", C("tensor_copy", out=cfx[:], in_=ci[:]), reads=[ci], writes=[cfx])
        P.op("vector", C("tensor_tensor", out=cf[:], in0=cfx[:], in1=cf[:], op=ALU.is_gt), reads=[cfx, cf], writes=[cf])
        P.op("vector", C("tensor_tensor", out=cfx[:], in0=cfx[:], in1=cf[:], op=ALU.subtract), reads=[cfx, cf], writes=[cfx])
        P.op("vector", C("tensor_scalar", out=padded[:], in0=cfx[:], scalar1=float(BLK), scalar2=None, op0=ALU.mult), reads=[cfx], writes=[padded])
        src_b, dst_b = padded, cs_a
        for sft in (1, 2, 4, 8, 16):
            P.op("vector", C("tensor_copy", out=dst_b[:, 0:sft], in_=src_b[:, 0:sft]), reads=[src_b], writes=[dst_b])
            P.op("vector", C("tensor_tensor", out=dst_b[:, sft:NE], in0=src_b[:, sft:NE], in1=src_b[:, 0:NE - sft], op=ALU.add), reads=[src_b], writes=[dst_b])
            src_b, dst_b = dst_b, (cs_b if dst_b is cs_a else cs_a)
        pend = src_b
        P.op("vector", C("tensor_tensor", out=pstart[:], in0=pend[:], in1=padded[:], op=ALU.subtract), reads=[pend, padded], writes=[pstart])
        P.op("vector", C("tensor_tensor", out=destm[:], in0=rankall[:], in1=pstart[:].unsqueeze(1).to_broadcast([128, NTT, NE]), op=ALU.add),
             reads=[rankall, pstart], writes=[destm])
        P.op("vector", C("scalar_tensor_tensor", out=destm[:], in0=destm[:], scalar=1.0, in1=maskall[:], op0=ALU.add, op1=ALU.mult), reads=[destm, maskall], writes=[destm])
        P.op("vector", C("tensor_scalar", out=destm[:], in0=destm[:], scalar1=-1.0, scalar2=None, op0=ALU.add), reads=[destm], writes=[destm])
        for tl in range(NTT):
            P.op("vector", C("max", out=d8[:, tl, :], in_=destm[:, tl, :]), reads=[destm], writes=[d8])
        P.op("vector", C("tensor_copy", out=didx[:], in_=d8[:, :, 0:4]), reads=[d8], writes=[didx])
        P.op("gpsimd", C("memset", pay[:], 0.0), writes=[pay])
        P.op("vector", C("tensor_copy", out=pay[:, :, :, 0], in_=tokid[:].unsqueeze(2).to_broadcast([128, NTT, 4])), reads=[tokid, pay], writes=[pay])
        P.op("vector", C("scalar_tensor_tensor", out=pay[:, :, :, 1], in0=tokid[:].unsqueeze(2).to_broadcast([128, NTT, 4]), scalar=4.0, in1=j4[:],
                         op0=ALU.mult, op1=ALU.add), reads=[tokid, j4, pay], writes=[pay])
        for j in range(4):
            P.op("vector", C("tensor_tensor", out=eqt[:], in0=destm[:], in1=d8[:, :, j:j + 1].to_broadcast([128, NTT, NE]), op=ALU.is_equal),
                 reads=[destm, d8], writes=[eqt])
            P.op("vector", C("tensor_tensor", out=eqt[:], in0=eqt[:], in1=gates[:], op=ALU.mult), reads=[eqt, gates], writes=[eqt])
            P.op("vector", C("tensor_reduce", out=pay[:, :, j, 2], in_=eqt[:], axis=mybir.AxisListType.X, op=ALU.add), reads=[eqt, pay], writes=[pay])
        P.op("vector", C("tensor_scalar", out=pay[:, :, :, 2], in0=pay[:, :, :, 2], scalar1=1.0 / 1.702, scalar2=None, op0=ALU.mult), reads=[pay], writes=[pay])
        for tl in range(NTT):
            for j in range(4):
                P._mk("gpsimd", C("indirect_dma_start", out=slot_d[:, :], out_offset=bass.IndirectOffsetOnAxis(ap=didx[:, tl, j:j + 1], axis=0),
                                   in_=pay[:, tl, j, :], in_offset=None), [didx, pay, slottab], [], True)
        P.op("vector", C("tensor_tensor", out=cmpk[:], in0=pend[:].unsqueeze(1).to_broadcast([128, NBLK, NE]), in1=kk[:], op=ALU.is_le), reads=[pend, kk], writes=[cmpk])
        P.op("vector", C("tensor_reduce", out=bexp[:], in_=cmpk[:], axis=mybir.AxisListType.X, op=ALU.add), reads=[cmpk], writes=[bexp])
        P.op("vector", C("tensor_scalar", out=bexp1024[:], in0=bexp[:], scalar1=1024.0, scalar2=None, op0=ALU.mult), reads=[bexp], writes=[bexp1024])
        if "slot" in dbg_d and b == 0:
            dbg("bexp", bexp[:], bexp)
            dbg("pend", pend[:], pend)
        phase_end(ltri, kk, tokid, j4, Srun, rankall, cnt, ci, cf, cfx, padded, cs_a, cs_b, pstart, destm, d8, didx, eqt, pay, cmpk, maskall, gates)
        if "slot" in dbg_d and b == 0:
            stmp = A.alloc("stmp", 128, [NSLOT // 128, 4], F32)
            P.dma("sync", stmp[:], slot_d.rearrange("(a p) c -> p a c", p=128), reads=[slottab], writes=[stmp])
            dbg("slot", stmp[:], stmp)
            A.release(stmp)
        if stop_after == 7:
            P.finish(out_ops)
            P.emit()
            return nc

        fcp = A.alloc("fcp", 128, [8], F32)
        ecol = A.alloc("ecol", 128, [1], F32)
        b1all = A.alloc("b1all", 128, [2048], BF16)
        b2all = A.alloc("b2all", 128, [D], BF16)
        b2f = A.alloc("b2f", 128, [D], F32)
        P.dma("sync", fcp[:], fcp_d[:, :], writes=[fcp])
        P.dma("sync", ecol[:], ecol_d[:, :], writes=[ecol])
        P.dma("gpsimd", b1all[0:NE, :], b1n_d[:, :], writes=[b1all])
        P.dma("sync", b2f[0:NE, :], b2_d[:, :], writes=[b2f])
        P.op("vector", C("tensor_scalar", out=b2all[0:NE, :], in0=b2f[0:NE, :], scalar1=1.702, scalar2=None, op0=ALU.mult), reads=[b2f], writes=[b2all])
        NU = 16
        w1u = [A.alloc("w1u%d" % i, 128, [KC, 2, 128], BF16) for i in range(NU)]
        w2b = [A.alloc("w2b%d" % i, 128, [8, D], BF16) for i in range(2)]
        xg = [A.alloc("xg%d" % i, 128, [JT, D], BF16) for i in range(2)]
        xgT = [A.alloc("xgT%d" % i, 128, [KC, BLK], BF16) for i in range(2)]
        actT = [A.alloc("actT%d" % i, 128, [8, BLK], BF16) for i in range(2)]
        ys = [A.alloc("ys%d" % i, 128, [D], F32) for i in range(4)]
        stb = [A.alloc("stb%d" % i, 128, [JT, 4], F32) for i in range(2)]
        gidx = [A.alloc("gidx%d" % i, 128, [JT], I32) for i in range(2)]
        sidx = [A.alloc("sidx%d" % i, 128, [JT], I32) for i in range(2)]
        widf = [A.alloc("widf%d" % i, 128, [8], F32) for i in range(2)]
        widx = [A.alloc("widx%d" % i, 128, [8], I32) for i in range(2)]
        ohb = [A.alloc("ohb%d" % i, 128, [BLK], BF16) for i in range(2)]
        tg = [A.alloc("tg%d" % i, 128, [512], F32) for i in range(2)]
        tsg = [A.alloc("tsg%d" % i, 128, [512], F32) for i in range(2)]
        tlin = [A.alloc("tlin%d" % i, 128, [512], F32) for i in range(2)]
        w1rows = w1_d[:, :]
        w2rows = w2_d[:, :]
        ucount = [0]
        blk_units = {}

        regcache = {}

        def wgather(out_ap, src_ap, idx_ap):
            def fn(e):
                if "bc" not in regcache:
                    regcache["bc"] = e.to_reg(NE * 1024 - 1)
                return e.indirect_dma_start(out=out_ap, out_offset=None, in_=src_ap, in_offset=bass.IndirectOffsetOnAxis(ap=idx_ap, axis=0),
                                            bounds_check=regcache["bc"], oob_is_err=False)
            return fn

        def issue_w2(k):
            pp = k % 2
            for fc in range(8):
                P._mk("gpsimd", wgather(w2b[pp][:, fc, :], w2rows, widx[pp][:, fc:fc + 1]), [widx[pp]], [w2b[pp]], True)

        def issue_unit(k, fc):
            pp = k % 2
            wu = w1u[ucount[0] % NU]
            ucount[0] += 1
            blk_units.setdefault(k, []).append(wu)
            P._mk("gpsimd", wgather(wu[:].rearrange("p k g f -> p (k g f)"), w1rows, widx[pp][:, fc:fc + 1]), [widx[pp], wu], [wu], True)

        ORDER = [0, 1]
        lo_, hi_ = 2, NBLK - 1
        while lo_ <= hi_:
            ORDER.append(hi_)
            hi_ -= 1
            if lo_ <= hi_:
                ORDER.append(lo_)
                lo_ += 1
        assert sorted(ORDER) == list(range(NBLK))

        def moe_loads(k):
            pp = k % 2
            blk = ORDER[k]
            P.dma("sync", stb[pp][:], slot_d[blk * BLK:(blk + 1) * BLK, :].rearrange("(j p) c -> p j c", p=128), reads=[slottab], writes=[stb[pp]])
            P.op("vector", C("tensor_copy", out=gidx[pp][:], in_=stb[pp][:, :, 0]), reads=[stb[pp]], writes=[gidx[pp]])
            P.op("vector", C("tensor_copy", out=sidx[pp][:], in_=stb[pp][:, :, 1]), reads=[stb[pp]], writes=[sidx[pp]])
            P.op("vector", C("tensor_scalar", out=widf[pp][:], in0=fcp[:], scalar1=bexp1024[:, blk:blk + 1], scalar2=None, op0=ALU.add), reads=[fcp, bexp1024], writes=[widf[pp]])
            P.op("vector", C("tensor_copy", out=widx[pp][:], in_=widf[pp][:]), reads=[widf[pp]], writes=[widx[pp]])
            P.op("vector", C("tensor_scalar", out=ohb[pp][0:NE, :], in0=ecol[0:NE, 0:1].to_broadcast([NE, BLK]), scalar1=bexp[0:NE, blk:blk + 1], scalar2=None, op0=ALU.is_equal),
                 reads=[ecol, bexp], writes=[ohb[pp]])
            for j in range(JT):
                P._mk("gpsimd", C("indirect_dma_start", out=xg[pp][:, j, :], out_offset=None, in_=h2d_d[:, :],
                                   in_offset=bass.IndirectOffsetOnAxis(ap=gidx[pp][:, j:j + 1], axis=0)), [gidx[pp], h2rows], [xg[pp]], True)
            for fc in range(8):
                issue_unit(k, fc)
            issue_w2(k)

        pcs = [0]
        stp = [0]

        def moe_transposes(k):
            pp = k % 2
            for j in range(JT):
                pz = psb[j % 2]
                pzv = pz.ap.bitcast(BF16)
                for kc in range(KC):
                    P.op("tensor", C("transpose", out=pzv[:, kc * 128:(kc + 1) * 128], in_=xg[pp][:, j, kc * 128:(kc + 1) * 128], identity=ident_b[:]),
                         reads=[xg[pp], ident_b], writes=[pz])
                evac(xgT[pp][:, :, j * 128:(j + 1) * 128], pzv.rearrange("p (k t) -> p k t", k=KC), [pz], [xgT[pp]])

        def moe_compute(k):
            pp = k % 2
            us = blk_units[k]
            for fc in range(8):
                wu = us[fc]
                pg_, pl_ = psb[2 + (pcs[0] % 2) * 2], psb[3 + (pcs[0] % 2) * 2]
                pcs[0] += 1
                tg_, tsg_, tlin_ = tg[stp[0] % 2], tsg[stp[0] % 2], tlin[stp[0] % 2]
                stp[0] += 1
                for kc in range(KC):
                    mm(pg_[:, 0:BLK], wu[:, kc, 0, :], xgT[pp][:, kc, :], kc == 0, False, [wu, xgT[pp]], [pg_])
                mm(pg_[:, 0:BLK], b1all[0:NE, fc * 128:(fc + 1) * 128], ohb[pp][0:NE, :], False, True, [b1all, ohb[pp]], [pg_])
                for kc in range(KC):
                    mm(pl_[:, 0:BLK], wu[:, kc, 1, :], xgT[pp][:, kc, :], kc == 0, False, [wu, xgT[pp]], [pl_])
                mm(pl_[:, 0:BLK], b1all[0:NE, 1024 + fc * 128:1024 + (fc + 1) * 128], ohb[pp][0:NE, :], False, True, [b1all, ohb[pp]], [pl_])
                P.op("vector", C("tensor_scalar", out=tg_[:, 0:BLK], in0=pg_[:, 0:BLK], scalar1=7.0, scalar2=None, op0=ALU.min), reads=[pg_], writes=[tg_])
                if "g_tg" in dbg_d and b == 0 and k == 0 and fc == 0:
                    dbg("g_tg", tg_[:], tg_)
                    dq = A.alloc("dq", 128, [2048], F32)
                    P.op("vector", C("tensor_copy", out=dq[:], in_=wu[:].rearrange("p k g f -> p (k g f)")), reads=[wu], writes=[dq])
                    dbg("g_wu", dq[:], dq)
                    dq2 = A.alloc("dq2", 128, [512], F32)
                    P.op("vector", C("tensor_copy", out=dq2[:], in_=ohb[pp][:]), reads=[ohb[pp]], writes=[dq2])
                    dbg("g_ohb", dq2[:], dq2)
                    dq3 = A.alloc("dq3", 128, [8], F32)
                    P.op("vector", C("tensor_copy", out=dq3[:], in_=widx[pp][:]), reads=[widx[pp]], writes=[dq3])
                    dbg("g_widx", dq3[:], dq3)
                P.op("scalar", C("activation", out=tsg_[:, 0:BLK], in_=tg_[:, 0:BLK], func=AF.Silu, scale=1.702), reads=[tg_], writes=[tsg_])
                P.op("vector", C("tensor_scalar", out=tlin_[:, 0:BLK], in0=pl_[:, 0:BLK], scalar1=7.0, scalar2=-7.0, op0=ALU.min, op1=ALU.max), reads=[pl_], writes=[tlin_])
                P.op("vector", C("scalar_tensor_tensor", out=actT[pp][:, fc, :], in0=tlin_[:, 0:BLK], scalar=1.0, in1=tsg_[:, 0:BLK], op0=ALU.add, op1=ALU.mult),
                     reads=[tlin_, tsg_], writes=[actT[pp]])
            if "g_xgT" in dbg_d and b == 0 and k == 0:
                dtmp = A.alloc("dtmp", 128, [KC * BLK], F32)
                P.op("vector", C("tensor_copy", out=dtmp[:], in_=xgT[pp][:].rearrange("p a b -> p (a b)")), reads=[xgT[pp]], writes=[dtmp])
                dbg("g_xgT", dtmp[:], dtmp)
                dtmp2 = dtmp
                P.op("vector", C("tensor_copy", out=dtmp2[:], in_=actT[pp][:].rearrange("p a b -> p (a b)")), reads=[actT[pp]], writes=[dtmp2])
                dbg("g_actT", dtmp2[:], dtmp2)
            if k + 1 < NBLK:
                moe_transposes(k + 1)
            for j in range(JT):
                ysj = ys[j % 4]
                for n2 in range(2):
                    py_ = psb[(6, 7, 0, 1)[(j * 2 + n2) % 4]]
                    for fc in range(8):
                        mm(py_[:, :], actT[pp][:, fc, j * 128:(j + 1) * 128], w2b[pp][:, fc, n2 * 512:(n2 + 1) * 512], fc == 0, False, [actT[pp], w2b[pp]], [py_])
                    mm(py_[:, :], ohb[pp][0:NE, 0:128], b2all[0:NE, n2 * 512:(n2 + 1) * 512], False, True, [ohb[pp], b2all], [py_])
                    if n2 == 0:
                        P.op("vector", C("tensor_scalar", out=ysj[:, 0:512], in0=py_[:, :], scalar1=stb[pp][:, j, 2:3], scalar2=None, op0=ALU.mult),
                             reads=[py_, stb[pp]], writes=[ysj])
                    else:
                        P.op("vector", C("tensor_scalar", out=ysj[:, 512:1024], in0=py_[:, :], scalar1=stb[pp][:, j, 2:3], scalar2=None, op0=ALU.mult),
                             reads=[py_, stb[pp]], writes=[ysj])
                if "g_ys" in dbg_d and b == 0 and k == 0 and j == 0:
                    dbg("g_ys", ysj[:], ysj)
                P._mk("gpsimd", C("indirect_dma_start", out=out4_d[:, :], out_offset=bass.IndirectOffsetOnAxis(ap=sidx[pp][:, j:j + 1], axis=0),
                                   in_=ysj[:], in_offset=None), [sidx[pp], ysj], [], True)

        moe_loads(0)
        moe_transposes(0)
        for k in range(NBLK):
            if k + 1 < NBLK:
                moe_loads(k + 1)
            moe_compute(k)
        phase_end(fcp, ecol, b1all, b2all, b2f, *w1u, *w2b, *xg, *xgT, *actT, *ys, *stb, *gidx, *sidx, *widf, *widx, *ohb, *tg, *tsg, *tlin, bexp, bexp1024)
        if stop_after == 8:
            P.finish(out_ops)
            P.emit()
            return nc

        xo = [A.alloc("xo%d" % i, 128, [D], F32) for i in range(4)]
        rows = [A.alloc("row%d" % i, 128, [D], F32) for i in range(4)]
        xin = [A.alloc("xin%d" % i, 128, [D], F32) for i in range(4)]
        xn_l = [A.alloc("xn%d" % i, 128, [D], F32) for i in range(4)]
        o4 = [A.alloc("o4%d" % i, 128, [4, D], F32) for i in range(4)]
        load_row(rows[0], modrows_d[0, 5 * D:6 * D], reads=[modrows])
        load_row(rows[3], modrows_d[1, 5 * D:6 * D], reads=[modrows])
        load_row(rows[1], ln_d[2, :])
        load_row(rows[2], ln_d[3, :])
        def tileH(b, tl):
            g2row = rows[0] if b == 0 else rows[3]
            xi = xin[tl % 4]
            o4t = o4[tl % 4]
            r0_ = (b * T + tl * 128) * 4
            P.dma("sync", xi[:], xmid_d[b * T + tl * 128:b * T + (tl + 1) * 128, :], reads=[xmid[b]], writes=[xi])
            P.dma("sync", o4t[:], out4_d[r0_:r0_ + 512, :].rearrange("(p j) n -> p j n", j=4), writes=[o4t])
            xn = xn_l[tl % 4]
            P.op("vector", C("tensor_tensor", out=o4t[:, 0, :], in0=o4t[:, 0, :], in1=o4t[:, 1, :], op=ALU.add), reads=[o4t], writes=[o4t])
            yield
            P.op("vector", C("tensor_tensor", out=o4t[:, 2, :], in0=o4t[:, 2, :], in1=o4t[:, 3, :], op=ALU.add), reads=[o4t], writes=[o4t])
            yield
            P.op("vector", C("tensor_tensor", out=o4t[:, 0, :], in0=o4t[:, 0, :], in1=o4t[:, 2, :], op=ALU.add), reads=[o4t], writes=[o4t])
            yield
            if "fx" in dbg_d and b == 0:
                out_ops.append(P.dma("sync", dbg_d["fx"][tl * 128:(tl + 1) * 128, :], o4t[:, 0, :], reads=[o4t]))
            P.op("vector", C("tensor_tensor", out=xn[:], in0=o4t[:, 0, :], in1=g2row[:], op=ALU.mult), reads=[o4t, g2row], writes=[xn])
            yield
            P.op("vector", C("scalar_tensor_tensor", out=xn[:], in0=xi[:], scalar=ALPHA, in1=xn[:], op0=ALU.mult, op1=ALU.add), reads=[xi, xn], writes=[xn])
            yield
            xot = xo[tl % 4]
            yield from layer_norm_g(xn, xot)
            P.op("vector", C("tensor_tensor", out=xot[:], in0=xot[:], in1=rows[1][:], op=ALU.mult), reads=[xot, rows[1]], writes=[xot])
            yield
            P.op("vector", C("tensor_tensor", out=xot[:], in0=xot[:], in1=rows[2][:], op=ALU.add), reads=[xot, rows[2]], writes=[xot])
            yield
            o = P.dma("sync", out_d[b, tl * 128:(tl + 1) * 128, :], xot[:], reads=[xot])
            out_ops.append(o)

        for b in range(2):
            for tl in range(0, NT, 4):
                run_interleaved([tileH(b, tl + q) for q in range(4)])
        phase_end(*xo, *xin, *xn_l, *rows, *o4)
        P.finish(out_ops)
        P.emit()
    return nc


def _prep_shared(inp):
    f32 = np.float32
    cst = _consts()
    cols = _ext_cols()
    w_in_ext = np.ascontiguousarray(inp["w_in"][0][:, cols]).astype(f32)
    convw = inp["hy_conv_w"][0]
    convb = inp["hy_conv_b"][0]
    cp = np.concatenate([convw, convb[None]], axis=0).reshape(4, 12, 128).transpose(2, 0, 1)
    skipp = inp["hy_skip"][0].reshape(2, 4, 128).transpose(2, 0, 1)
    w1 = inp["exp_w1"][0]
    w1r = np.ascontiguousarray(w1.reshape(NE, 8, 128, 2, 8, 128).transpose(0, 4, 2, 1, 3, 5)).reshape(NE * 8 * 128, 2048)
    b1 = inp["exp_b1"][0]
    b1r = np.ascontiguousarray(b1.reshape(NE, 16, 128).transpose(2, 0, 1)).reshape(128, NE * 16)
    lnp = np.stack([inp["ln1_g"][0], inp["ln1_b"][0], inp["ln2_g"][0], inp["ln2_b"][0]], axis=0)
    sh = {
        "w_mod": inp["w_mod"][0], "b_mod": inp["b_mod"][0][None, :], "w_in": w_in_ext,
        "attn_sink": inp["attn_sink"][0][None, :],
        "convp": np.ascontiguousarray(cp).reshape(128, 48), "skipp": np.ascontiguousarray(skipp).reshape(128, 8),
        "fw1": inp["hy_filt_w1"][0], "fb1": inp["hy_filt_b1"][0][:, None], "fw2": inp["hy_filt_w2"][0],
        "fb2": inp["hy_filt_b2"][0][:, None], "fw3": inp["hy_filt_w3"][0],
        "w_ba": inp["w_branch_attn"][0], "w_bh": inp["w_branch_hyena"][0], "w_o": inp["w_out"][0],
        "lnp": lnp, "router_w": inp["router_w"][0], "router_b": inp["router_b"][0][None, :],
        "w1r": w1r, "b1r": b1r, "w2": inp["exp_w2"][0].reshape(NE * D, D), "b2": inp["exp_b2"][0],
        "ropeC": cst["ropeC"], "ropeS": cst["ropeS"], "masks": cst["masks"].reshape(128, 256), "zT": cst["zT"],
        "decay": cst["decay"], "decayb": cst["decayb"],
        "cft": cst["cft"], "sft": cst["sft"], "cit": cst["cit"], "sit": cst["sit"],
        "ltri": cst["ltri"], "kk512": cst["kk512"], "fcp": cst["fcp"], "ecol": cst["ecol"], "tokid": cst["tokid"], "j4": cst["j4"],
        "slot_init": cst["slot_init"], "b1n": inp["exp_b1"][0],
    }
    return {k: np.ascontiguousarray(v) for k, v in sh.items()}


def _run(inp, stop_after=None, cores=NCORES):
    inp = {k: np.asarray(v) for k, v in inp.items()}
    shared = _prep_shared(inp)
    nc = build(stop_after)
    in_maps = []
    for i in range(cores):
        m = dict(shared)
        m["x"] = np.ascontiguousarray(inp["x"][2 * i:2 * i + 2])
        m["ctx"] = np.ascontiguousarray(inp["ctx"][2 * i:2 * i + 2])
        m["c3"] = np.ascontiguousarray(np.concatenate([inp["c"][2 * i:2 * i + 2], inp["c_ctx"][None, :]], axis=0))
        in_maps.append(m)
    res = run_bass_kernel_spmd(nc, in_maps, core_ids=list(range(cores)))
    return res


def kernel(**inputs):
    res = _run(inputs)
    out = np.concatenate([r["out"] for r in res.results], axis=0)
    return out.astype(np.float32)
```
